# Optimizing a Trainium2 kernel written in Bass

```python
import jax, jax.numpy as jnp
from jax import lax
import numpy as np

D_MODEL = 1024
BATCH = 8
SEQ = 4096
DEPTH = 2

GRID_W = 64
CTX_LEN = 256
N_MIXERS = 2
D_FF = 4 * D_MODEL
CONV_W = 3
ML_HEADS = 8
ML_DV = D_MODEL // ML_HEADS
ML_DQK = ML_DV // 2
ML_QK_COLS = 2 * ML_HEADS * ML_DQK
ML_QKV_COLS = ML_QK_COLS + ML_HEADS * ML_DV
ML_PROJ_COLS = ML_QKV_COLS + ML_HEADS * ML_DV
ML_CHUNK = 128
EPS = 1e-6
N_CONV_LAYERS = (DEPTH + N_MIXERS - 1) // N_MIXERS
N_MLSTM_LAYERS = DEPTH // N_MIXERS

kernel_name = 'hybrid_shortconv_mlstm_dit'


def _rmsnorm(x, w):
    x32 = x.astype(jnp.float32)
    y = x32 * lax.rsqrt(jnp.mean(x32 * x32, axis=-1, keepdims=True) + EPS)
    return y.astype(x.dtype) * w


def _modulate(h, shift, scale):
    return h * (1 + scale) + shift


def _dwconv_centred(u, w, axis):
    n = u.shape[axis]
    half = CONV_W // 2
    pad = [(0, 0)] * u.ndim
    pad[axis] = (half, CONV_W - 1 - half)
    up = jnp.pad(u, pad)
    out = w[0] * lax.slice_in_dim(up, 0, n, axis=axis)
    for j in range(1, CONV_W):
        out = out + w[j] * lax.slice_in_dim(up, j, j + n, axis=axis)
    return out


def _short_conv_mixer(h, w_in, conv_w, w_out, rows):
    bsz, t_len, d = h.shape
    b_gate, c_gate, u = jnp.split(h @ w_in, 3, axis=-1)
    u = c_gate * u
    if rows is None:
        y = _dwconv_centred(u, conv_w, axis=1)
    else:
        y = _dwconv_centred(u.reshape(bsz, rows, GRID_W, d), conv_w, axis=2).reshape(bsz, t_len, d)
    return (b_gate * y) @ w_out


def _sqrelu_mlp(h, w1, w2):
    return jnp.square(jax.nn.relu(h @ w1)) @ w2


def _mlstm_inputs(h, w_qkvo, w_if, b_if, with_o):
    bsz, t_len, _ = h.shape
    p = h @ (w_qkvo if with_o else w_qkvo[:, :ML_QKV_COLS])
    q = p[..., :ML_HEADS * ML_DQK]
    k = p[..., ML_HEADS * ML_DQK:ML_QK_COLS]
    v = p[..., ML_QK_COLS:ML_QKV_COLS]
    o = p[..., ML_QKV_COLS:] if with_o else None

    def heads(a, dh):
        return a.reshape(bsz, t_len, ML_HEADS, dh).transpose(0, 2, 1, 3).astype(jnp.float32)

    q = heads(q, ML_DQK) * (ML_DQK ** -0.5)
    k = heads(k, ML_DQK)
    v = heads(v, ML_DV)
    g = (jnp.einsum('btd,rdg->rbgt', h, w_if) + b_if[:, None, :, None]).astype(jnp.float32)
    ig = g[:, :, :ML_HEADS]
    lf = jax.nn.log_sigmoid(g[:, :, ML_HEADS:])
    return q, k, v, o, ig, lf


def _mlstm_scan(q, k, v, ig, lf, state, with_output):
    bsz, nh, t_len, _ = q.shape
    dv = v.shape[-1]
    nc = t_len // ML_CHUNK

    def chunks(a):
        a = a.reshape(a.shape[:2] + (nc, ML_CHUNK) + a.shape[3:])
        return jnp.moveaxis(a, 2, 0)

    xs = (chunks(q), chunks(k), chunks(v), chunks(ig), chunks(lf))
    tri = jnp.tril(jnp.ones((ML_CHUNK, ML_CHUNK), dtype=bool))

    def body(carry, inp):
        c_mat, n_vec, m = carry
        qc, kc, vc, ic, fc = inp
        b = jnp.cumsum(fc, axis=-1)
        out = None
        if with_output:
            logd = b[..., :, None] - b[..., None, :] + ic[..., None, :]
            logd = jnp.where(tri, logd, -jnp.inf)
            inter = b + m[..., None]
            m_t = jnp.maximum(inter, jnp.max(logd, axis=-1))
            s = jnp.einsum('bhtd,bhsd->bhts', qc, kc) * jnp.exp(logd - m_t[..., None])
            w_inter = jnp.exp(inter - m_t)
            num = jnp.einsum('bhts,bhsv->bhtv', s, vc) + w_inter[..., None] * jnp.einsum('bhtd,bhdv->bhtv', qc, c_mat)
            den = jnp.sum(s, axis=-1) + w_inter * jnp.einsum('bhtd,bhd->bht', qc, n_vec)
            out = num / jnp.maximum(jnp.abs(den), jnp.exp(-m_t))[..., None]
        b_last = b[..., -1]
        dec = b_last[..., None] - b + ic
        m_new = jnp.maximum(b_last + m, jnp.max(dec, axis=-1))
        wk = jnp.exp(dec - m_new[..., None])
        a = jnp.exp(b_last + m - m_new)
        c_new = a[..., None, None] * c_mat + jnp.einsum('bhsd,bhsv->bhdv', kc * wk[..., None], vc)
        n_new = a[..., None] * n_vec + jnp.einsum('bhs,bhsd->bhd', wk, kc)
        return (c_new, n_new, m_new), out

    state, hs = lax.scan(body, state, xs)
    if with_output:
        hs = jnp.moveaxis(hs, 0, 2).reshape(bsz, nh, t_len, dv)
    return hs, state


def _mlstm_readout(h, o, norm_w, w_out):
    bsz, nh, t_len, dv = h.shape
    h = h.transpose(0, 2, 1, 3)
    h = h * lax.rsqrt(jnp.mean(h * h, axis=-1, keepdims=True) + EPS)
    h = h.reshape(bsz, t_len, nh * dv).astype(o.dtype) * norm_w
    return (jax.nn.sigmoid(o) * h) @ w_out


def _mlstm_mixer(hx, hc, w_qkvo, w_if, b_if, norm_w, w_out, ctx_out):
    qx, kx, vx, ox, igx, lfx = _mlstm_inputs(hx, w_qkvo, w_if, b_if, True)
    qc, kc, vc, oc, igc, lfc = _mlstm_inputs(hc, w_qkvo, w_if, b_if, ctx_out)
    bsz = hx.shape[0]
    zero = (jnp.zeros((bsz, ML_HEADS, ML_DQK, ML_DV), jnp.float32),
            jnp.zeros((bsz, ML_HEADS, ML_DQK), jnp.float32),
            jnp.zeros((bsz, ML_HEADS), jnp.float32))
    hx_sum = 0.0
    hc_sum = 0.0
    for r in range(2):
        if r == 0:
            flip = lambda a: a
        else:
            flip = lambda a: jnp.flip(a, axis=2)
        hc_r, st = _mlstm_scan(flip(qc), flip(kc), flip(vc), flip(igc[r]), flip(lfc[r]), zero, ctx_out)
        hx_r, _ = _mlstm_scan(flip(qx), flip(kx), flip(vx), flip(igx[r]), flip(lfx[r]), st, True)
        hx_sum = hx_sum + flip(hx_r)
        if ctx_out:
            hc_sum = hc_sum + flip(hc_r)
    yx = _mlstm_readout(hx_sum, ox, norm_w, w_out)
    yc = _mlstm_readout(hc_sum, oc, norm_w, w_out) if ctx_out else None
    return yx, yc


def setup_inputs(seed: int = 0) -> dict:
    key = jax.random.key(seed)
    ks = jax.random.split(key, 20)
    f32 = jnp.float32

    def nrm(k, shape, fan_in):
        return jax.random.normal(k, shape, f32) * (fan_in ** -0.5)

    x = jax.random.normal(ks[0], (BATCH, SEQ, D_MODEL), f32)
    c = jax.random.normal(ks[1], (BATCH, D_MODEL), f32)
    ctx = jax.random.normal(ks[2], (BATCH, CTX_LEN, D_MODEL), f32)
    c_ctx = jax.random.normal(ks[3], (D_MODEL,), f32)
    norm_w = 1.0 + 0.02 * jax.random.normal(ks[4], (DEPTH, 4, D_MODEL), f32)
    mod_w = nrm(ks[5], (DEPTH, D_MODEL, 6 * D_MODEL), D_MODEL)
    mod_b = 0.01 * jax.random.normal(ks[6], (DEPTH, 6 * D_MODEL), f32)
    mlp_w1 = nrm(ks[7], (DEPTH, D_MODEL, D_FF), D_MODEL)
    mlp_w2 = nrm(ks[8], (DEPTH, D_FF, D_MODEL), D_FF)
    conv_w_in = nrm(ks[9], (N_CONV_LAYERS, D_MODEL, 3 * D_MODEL), D_MODEL)
    conv_w = nrm(ks[10], (N_CONV_LAYERS, CONV_W, D_MODEL), CONV_W)
    conv_w_out = nrm(ks[11], (N_CONV_LAYERS, D_MODEL, D_MODEL), D_MODEL)
    ml_w_qkvo = nrm(ks[12], (N_MLSTM_LAYERS, D_MODEL, ML_PROJ_COLS), D_MODEL)
    ml_w_if = nrm(ks[13], (N_MLSTM_LAYERS, 2, D_MODEL, 2 * ML_HEADS), D_MODEL)
    ig_b = 0.1 * jax.random.normal(ks[14], (N_MLSTM_LAYERS, 2, ML_HEADS), f32)
    fg_b = jnp.linspace(3.0, 6.0, ML_HEADS, dtype=f32) + 0.1 * jax.random.normal(ks[15], (N_MLSTM_LAYERS, 2, ML_HEADS), f32)
    ml_b_if = jnp.concatenate([ig_b, fg_b], axis=-1)
    ml_norm_w = 1.0 + 0.02 * jax.random.normal(ks[16], (N_MLSTM_LAYERS, ML_HEADS * ML_DV), f32)
    ml_w_out = nrm(ks[17], (N_MLSTM_LAYERS, ML_HEADS * ML_DV, D_MODEL), ML_HEADS * ML_DV)
    return {'x': x, 'c': c, 'ctx': ctx, 'c_ctx': c_ctx, 'norm_w': norm_w, 'mod_w': mod_w, 'mod_b': mod_b,
            'mlp_w1': mlp_w1, 'mlp_w2': mlp_w2, 'conv_w_in': conv_w_in, 'conv_w': conv_w,
            'conv_w_out': conv_w_out, 'ml_w_qkvo': ml_w_qkvo, 'ml_w_if': ml_w_if, 'ml_b_if': ml_b_if,
            'ml_norm_w': ml_norm_w, 'ml_w_out': ml_w_out}


def reference(x, c, ctx, c_ctx, norm_w, mod_w, mod_b, mlp_w1, mlp_w2, conv_w_in, conv_w, conv_w_out,
              ml_w_qkvo, ml_w_if, ml_b_if, ml_norm_w, ml_w_out):
    rows = x.shape[1] // GRID_W
    silu_c = jax.nn.silu(c)[:, None, :]
    silu_cc = jax.nn.silu(c_ctx)
    cx = ctx
    for i in range(DEPTH):
        last = i == DEPTH - 1
        kind = i % N_MIXERS
        j = i // N_MIXERS
        nw = norm_w[i]
        mx = jnp.split(silu_c @ mod_w[i] + mod_b[i], 6, axis=-1)
        hx = _modulate(_rmsnorm(x, nw[0]), mx[0], mx[1])
        need_ctx = (not last) or kind == 1
        if need_ctx:
            mc = jnp.split(silu_cc @ mod_w[i] + mod_b[i], 6, axis=-1)
            hc = _modulate(_rmsnorm(cx, nw[0]), mc[0], mc[1])
        if kind == 0:
            yx = _short_conv_mixer(hx, conv_w_in[j], conv_w[j], conv_w_out[j], rows)
            yc = _short_conv_mixer(hc, conv_w_in[j], conv_w[j], conv_w_out[j], None) if not last else None
        else:
            yx, yc = _mlstm_mixer(hx, hc, ml_w_qkvo[j], ml_w_if[j], ml_b_if[j], ml_norm_w[j], ml_w_out[j],
                                  not last)
        x = x + mx[2] * _rmsnorm(yx, nw[1])
        x = x + mx[5] * _rmsnorm(_sqrelu_mlp(_modulate(_rmsnorm(x, nw[2]), mx[3], mx[4]), mlp_w1[i], mlp_w2[i]), nw[3])
        if not last:
            cx = cx + mc[2] * _rmsnorm(yc, nw[1])
            cx = cx + mc[5] * _rmsnorm(_sqrelu_mlp(_modulate(_rmsnorm(cx, nw[2]), mc[3], mc[4]), mlp_w1[i], mlp_w2[i]), nw[3])
    return x
```

```python
import contextlib
import numpy as np
import concourse.bass as bass
import concourse.mybir as mybir
from concourse.bass_utils import run_bass_kernel_spmd

F32 = mybir.dt.float32
BF16 = mybir.dt.bfloat16
AF = mybir.ActivationFunctionType
ALU = mybir.AluOpType
AX = mybir.AxisListType

D = 1024
FC = 8
SEQ = 4096
CTX = 256
TT = 512
NT = SEQ // TT
DFF = 4096
NH = 8
DQK = 64
DV = 128
VS = 130
EPS = 1e-6
DEBUG = False
STOP = None
NCORES = 8
GG_ENG = "dve"
NCX_RUN = 16


class _Stop(Exception):
    pass

SEM_EPOCH = 12000
N_DMA_SEMS = 10


class Op:
    __slots__ = ("eng", "fn", "reads", "writes", "dma", "deps", "sig", "signo", "dsem", "dval")

    def __init__(self, eng, fn, reads, writes, dma):
        self.eng = eng
        self.fn = fn
        self.reads = reads
        self.writes = writes
        self.dma = dma
        self.deps = None
        self.sig = False
        self.signo = 0
        self.dsem = None
        self.dval = 0


class Prog:
    ENGS = ("pe", "act", "dve", "pool", "sp")

    def __init__(self, nc):
        self.nc = nc
        self.ops = []

    def op(self, eng, fn, reads=(), writes=()):
        self.ops.append(Op(eng, fn, tuple(reads), tuple(writes), False))

    def dma(self, eng, fn, reads=(), writes=()):
        self.ops.append(Op(eng, fn, tuple(reads), tuple(writes), True))

    def emit(self):
        nc = self.nc
        ops = self.ops
        last_w = {}
        readers = {}
        for i, o in enumerate(ops):
            deps = {}
            for b in o.reads:
                j = last_w.get(b)
                if j is not None:
                    deps[j] = True
                if b[0] == "ps":
                    for j in readers.get(b, ()):
                        if ops[j].eng != o.eng:
                            deps.setdefault(j, False)
            for b in o.writes:
                j = last_w.get(b)
                if j is not None:
                    deps.setdefault(j, False)
                for j in readers.get(b, ()):
                    deps.setdefault(j, False)
            deps.pop(i, None)
            o.deps = deps
            for b in o.reads:
                readers.setdefault(b, []).append(i)
            for b in o.writes:
                last_w[b] = i
                readers[b] = []
        for o in ops:
            for j, raw in o.deps.items():
                p = ops[j]
                if p.dma:
                    continue
                if o.dma or p.eng != o.eng:
                    p.sig = True
                elif raw and o.eng in ("act", "dve", "pool"):
                    p.sig = True
        cnt = {e: 0 for e in self.ENGS}
        for o in ops:
            if o.sig and not o.dma:
                cnt[o.eng] += 1
                o.signo = cnt[o.eng]
        engobj = {"pe": nc.tensor, "act": nc.scalar, "dve": nc.vector, "pool": nc.gpsimd, "sp": nc.sync}
        with contextlib.ExitStack() as st:
            esems = {}
            for e in self.ENGS:
                n_ep = (cnt[e] + SEM_EPOCH - 1) // SEM_EPOCH
                esems[e] = [st.enter_context(nc.semaphore(f"s_{e}_{k}")) for k in range(n_ep)]
            dsems = {}
            for e in ("sp", "pool", "act"):
                if any(o.dma and o.eng == e for o in ops):
                    dsems[e] = [st.enter_context(nc.semaphore(f"d_{e}_{k}")) for k in range(N_DMA_SEMS)]
            duse = {e: [0] * N_DMA_SEMS for e in dsems}
            dlast = {e: [None] * N_DMA_SEMS for e in dsems}
            dnext = {e: 0 for e in dsems}
            seen = {e: {} for e in self.ENGS}

            def wait(e, sem, key, val):
                s = seen[e]
                if s.get(key, 0) >= val:
                    return
                s[key] = val
                engobj[e].wait_ge(sem, val)

            def wait_op(e, p):
                if p.dma:
                    wait(e, p.dsem, ("d", id(p.dsem)), p.dval)
                else:
                    k = (p.signo - 1) // SEM_EPOCH
                    v = (p.signo - 1) % SEM_EPOCH + 1
                    wait(e, esems[p.eng][k], (p.eng, k), v)

            for o in ops:
                e = o.eng
                for j, raw in o.deps.items():
                    p = ops[j]
                    if p.dma or o.dma or p.eng != e:
                        wait_op(e, p)
                    elif raw and e in ("act", "dve", "pool"):
                        wait_op(e, p)
                if o.dma:
                    k = dnext[e]
                    dnext[e] = (k + 1) % N_DMA_SEMS
                    prev = dlast[e][k]
                    if prev is not None:
                        wait_op(e, prev)
                    duse[e][k] += 1
                    o.dsem = dsems[e][k]
                    o.dval = 16 * duse[e][k]
                    dlast[e][k] = o
                    ins = o.fn(engobj[e])
                    ins.then_inc(o.dsem, 16)
                else:
                    ins = o.fn(engobj[e])
                    if o.sig:
                        k = (o.signo - 1) // SEM_EPOCH
                        ins.then_inc(esems[e][k], 1)
            for e in dsems:
                for k in range(N_DMA_SEMS):
                    p = dlast[e][k]
                    if p is not None:
                        wait_op(e, p)


C_IDENT, C_MINC0, C_MINC1, C_MSTR0, C_MSTR1, C_NEG1, C_NBIG0, C_NBIG1, C_ONES = range(9)
NBIG = -30000.0


def _consts():
    u = np.arange(128)[:, None]
    t = np.arange(128)[None, :]
    mats = [
        (u == t).astype(np.float32),
        -(u <= t).astype(np.float32),
        -(u >= t).astype(np.float32),
        -(u > t).astype(np.float32),
        -(u < t).astype(np.float32),
        -np.ones((128, 128), np.float32),
        NBIG * (t < u).astype(np.float32),
        NBIG * (t > u).astype(np.float32),
        np.ones((128, 128), np.float32),
    ]
    return np.ascontiguousarray(np.concatenate(mats, axis=1))


S_CC = 0
S_NW = 16
S_MB = 80
S_CW = 176
S_MLNW = 200
S_BIF = 208
S_WIF = 240
S_MISC = 496
NSMALL = 500


def _fm(v):
    v = np.asarray(v, np.float32)
    lead = v.shape[:-1]
    a = v.reshape(lead + (FC, 128))
    a = np.moveaxis(a, -1, 0)
    return a


def _small(c_b, c_ctx, norm_w, mod_b, conv_w, ml_norm_w, ml_b_if, ml_w_if):
    s = np.zeros((128, NSMALL), np.float32)
    cc = np.stack([_fm(c_b), _fm(c_ctx)], axis=-1)
    s[:, S_CC:S_CC + 16] = cc.reshape(128, 16)
    s[:, S_NW:S_NW + 64] = _fm(norm_w).reshape(128, 64)
    mb = np.asarray(mod_b, np.float32).reshape(2, 48, 128)
    s[:, S_MB:S_MB + 96] = np.moveaxis(mb, -1, 0).reshape(128, 96)
    s[:, S_CW:S_CW + 24] = _fm(conv_w[0]).reshape(128, 24)
    s[:, S_MLNW:S_MLNW + 8] = _fm(ml_norm_w[0]).reshape(128, 8)
    s[:, S_BIF:S_BIF + 32] = np.asarray(ml_b_if[0], np.float32).reshape(1, 32)
    wif = np.concatenate([ml_w_if[0, 0], ml_w_if[0, 1]], axis=1)
    wif = wif.reshape(FC, 128, 32).transpose(1, 0, 2)
    s[:, S_WIF:S_WIF + 256] = wif.reshape(128, 256)
    s[:, S_MISC + 0] = 1.0
    s[:, S_MISC + 1] = EPS
    return s


CT = 256
NCX = SEQ // CT
NCH = CT // 128
SKEW = 40


class _BankAlloc:
    def __init__(self):
        self.free = [0, 1, 2, 3]

    def try_acq(self, n):
        if len(self.free) < n:
            return None
        r = self.free[:n]
        self.free = self.free[n:]
        return r

    def rel(self, pairs):
        self.free = self.free + list(pairs)


def build_nc():
    sched = _build(None)
    return _build(sched)


def _build(sched_in):
    dry = sched_in is None
    nc = bass.Bass("TRN2", target_bir_lowering=False)
    dt_in = lambda n, shp: nc.dram_tensor(n, list(shp), F32, kind="ExternalInput").ap()
    x_d = dt_in("x", [SEQ, D])
    ctx_d = dt_in("ctx", [CTX, D])
    small_d = dt_in("small", [128, NSMALL])
    consts_d = dt_in("consts", [128, 9 * 128])
    mod_w_d = dt_in("mod_w", [2, D, 6 * D])
    mlp_w1_d = dt_in("mlp_w1", [2, D, DFF])
    mlp_w2_d = dt_in("mlp_w2", [2, DFF, D])
    conv_w_in_d = dt_in("conv_w_in", [D, 3 * D])
    conv_w_out_d = dt_in("conv_w_out", [D, D])
    ml_w_qkvo_d = dt_in("ml_w_qkvo", [D, 3 * D])
    ml_w_out_d = dt_in("ml_w_out", [D, D])
    out_d = nc.dram_tensor("out", [SEQ, D], F32, kind="ExternalOutput").ap()

    def scratch(n, shp, dt):
        return nc.dram_tensor(n, list(shp), dt).ap()

    wsrc = {
        "win0": conv_w_in_d, "wout0": conv_w_out_d, "w1_0": mlp_w1_d[0], "w2_0": mlp_w2_d[0],
        "qkvo": ml_w_qkvo_d, "mlout": ml_w_out_d, "w1_1": mlp_w1_d[1], "w2_1": mlp_w2_d[1],
    }
    wsc = {n: scratch("sc_" + n, a.shape, BF16) for n, a in wsrc.items()}
    x1_s = scratch("x1_s", [NCX, 128, FC * CT], F32)
    qt_s = scratch("qt_s", [NCX, 64, NH * CT], BF16)
    kt_s = scratch("kt_s", [NCX, 64, NH * CT], BF16)
    ktok_s = scratch("ktok_s", [NCX, 128, NCH * 512], BF16)
    vaug_s = scratch("vaug_s", [NCX, 128, NCH * NH * VS], BF16)
    so_s = scratch("so_s", [NCX, 128, NH * CT], BF16)
    hf_s = scratch("hf_s", [NCX, 128, NCH * D], F32)
    dbg_d = None
    if DEBUG:
        dbg_d = nc.dram_tensor("dbg", [NCX, 128, FC * CT], F32, kind="ExternalOutput").ap()

    with contextlib.ExitStack() as st:
        def sb(name, shape, dt):
            return st.enter_context(nc.sbuf_tensor(name, list(shape), dt))

        P = Prog(nc)
        ps = [st.enter_context(nc.psum_tensor(f"ps{i}", [128, 512], F32)) for i in range(8)]
        banks = _BankAlloc()

        SM = sb("SM", [128, NSMALL], F32)
        CST = sb("CST", [128, 9 * 128], F32)
        ONESB = sb("ONESB", [128, 128], BF16)
        WIFB = sb("WIFB", [128, FC, 32], BF16)
        SIL = sb("SIL", [128, FC, 2], F32)
        MOD = sb("MOD", [128, 2, 2, 48], F32)
        PAR = sb("PAR", [128, 2, 2, 4, 8], F32)
        GT = sb("GT", [128, 2 * NCX + 2, 32], F32)
        WRING = [sb(f"WR{i}", [128, FC, 512], BF16) for i in range(4)]
        LFN = sb("LFN", [128, 16], F32)
        G3S = sb("G3S", [128, 40], F32)
        LFB = sb("LFB", [128, NH, 128], F32)
        DTT = sb("DTT", [128, 2, 128], F32)
        PTT = sb("PTT", [128, 2, 128], BF16)
        KW = sb("KW", [128, NH, 64], BF16)
        T0 = sb("T0", [128, 2, VS], F32)
        T1 = sb("T1", [128, NH, VS], F32)
        RD = sb("RD", [128, 16], F32)
        CST_C = [sb(f"CS{d}", [64, NH, VS], F32) for d in range(2)]
        CST_B = [sb(f"CB{d}", [64, NH, VS], BF16) for d in range(2)]
        HN = sb("HN", [128, 2, 128], F32)
        SSQ = sb("SSQ", [128, 16], F32)

        class Cx:
            def __init__(self, i):
                self.i = i
                self.F0 = sb(f"F0_{i}", [128, FC, CT], F32)
                self.F1 = sb(f"F1_{i}", [128, FC, CT], F32)
                self.F2 = sb(f"F2_{i}", [128, FC, CT], F32)
                self.B0 = sb(f"B0_{i}", [128, FC, CT], BF16)
                self.B1 = sb(f"B1_{i}", [128, FC, CT], BF16)
                self.HID = sb(f"HID_{i}", [128, 32, CT], BF16)
                self.RS = sb(f"RS_{i}", [128, CT], F32)
                self.TMPA = sb(f"TMPA_{i}", [128, 2, CT], F32)
                self.KTOK = sb(f"KTOK_{i}", [128, NCH, 512], BF16)
                self.VAUG = sb(f"VAUG_{i}", [128, NCH, NH, VS], BF16)
                self.SSQ = sb(f"SSQ_{i}", [128, 16], F32)
                self.HN = sb(f"HN_{i}", [128, 2, 128], F32)

            def k(self, name, idx=None):
                return (name + str(self.i), idx)

            def ks(self, name, n):
                return [(name + str(self.i), j) for j in range(n)]

        CXS = [Cx(0), Cx(1)]
        MODBUF = [sb(f"MODBUF{i}", [128, FC, CT], F32) for i in range(2)]

        IDENT = CST[:, C_IDENT * 128:(C_IDENT + 1) * 128]
        MINC = [CST[:, C_MINC0 * 128:(C_MINC0 + 1) * 128], CST[:, C_MINC1 * 128:(C_MINC1 + 1) * 128]]
        MSTR = [CST[:, C_MSTR0 * 128:(C_MSTR0 + 1) * 128], CST[:, C_MSTR1 * 128:(C_MSTR1 + 1) * 128]]
        NEG1 = CST[:, C_NEG1 * 128:(C_NEG1 + 1) * 128]
        NBIGM = [CST[:, C_NBIG0 * 128:(C_NBIG0 + 1) * 128], CST[:, C_NBIG1 * 128:(C_NBIG1 + 1) * 128]]
        ONES32 = CST[:, C_ONES * 128:(C_ONES + 1) * 128]
        ONE_COL = SM[:, S_MISC:S_MISC + 1]
        EPS_COL = SM[:, S_MISC + 1:S_MISC + 2]

        def psk(b):
            return [("ps", b)]

        def acq(n=1):
            while True:
                r = banks.try_acq(n)
                if r is not None:
                    return r
                yield "blocked"

        def reg(pair, mi, ntok):
            b = pair * 2 + mi // 2
            c0 = (mi % 2) * 256
            return b, c0

        P.dma("sp", lambda e: e.dma_start(out=SM[:], in_=small_d), writes=["SM"])
        P.dma("sp", lambda e: e.dma_start(out=CST[:], in_=consts_d), writes=["CST"])
        P.op("act", lambda e: e.activation(out=ONESB[:], in_=ONES32, func=AF.Copy), reads=["CST"], writes=["ONESB"])
        P.op("act", lambda e: e.activation(
            out=WIFB[:], in_=SM[:, S_WIF:S_WIF + 256].rearrange("p (k j) -> p k j", k=FC), func=AF.Copy),
            reads=["SM"], writes=["WIFB"])
        P.op("act", lambda e: e.activation(
            out=SIL[:], in_=SM[:, S_CC:S_CC + 16].rearrange("p (k j) -> p k j", k=FC), func=AF.Silu),
            reads=["SM"], writes=["SIL"])
        for d in range(2):
            P.op("pool", lambda e, d=d: e.memset(CST_C[d][:].rearrange("p a b -> p (a b)"), 0.0),
                 writes=[("CS", d, h) for h in range(NH)])
            P.op("pool", lambda e, d=d: e.memset(CST_B[d][:].rearrange("p a b -> p (a b)"), 0.0),
                 writes=[("CB", d, h) for h in range(NH)])
        for cx in CXS:
            P.op("pool", lambda e, cx=cx: e.memset(cx.VAUG[:].rearrange("p a b c -> p (a b c)"), 1.0),
                 writes=cx.ks("VAUG", NCH))

        PIECE = 1 << 20
        wpieces = {}
        for n, src in wsrc.items():
            R, C = src.shape
            npc = 1
            while R * C // npc > PIECE:
                npc *= 2
            rp = R // npc
            keys = []
            for r0 in range(0, R, rp):
                key = ("wsc", n, r0)
                keys.append(key)
                P.dma("pool", lambda e, n=n, src=src, r0=r0, rp=rp: e.dma_start(
                    out=wsc[n][r0:r0 + rp, :], in_=src[r0:r0 + rp, :]), writes=[key])
            wpieces[n] = keys

        flags = {}

        def mod_layer(l):
            pair = (yield from acq(1))[0]
            pb = pair * 2
            for nb in range(24):
                buf = MODBUF[nb % 2]
                bkey = [("MODBUF", nb % 2)]
                P.dma("sp", lambda e, l=l, nb=nb, buf=buf: e.dma_start(
                    out=buf[:], in_=mod_w_d[l, :, nb * 256:(nb + 1) * 256].rearrange("(k p) n -> p k n", p=128)),
                    writes=bkey)

                def mm_mod(e, nb=nb, buf=buf):
                    ins = None
                    for mi in range(2):
                        col = (nb * 2 + mi) * 2
                        for kc in range(FC):
                            ins = e.matmul(ps[pb][:, col:col + 2], lhsT=buf[:, kc, mi * 128:(mi + 1) * 128],
                                           rhs=SIL[:, kc, :], start=(kc == 0), stop=(kc == FC - 1))
                    return ins
                P.op("pe", mm_mod, reads=bkey + ["SIL"], writes=psk(pb))
                yield (1.6, 6.0)
            for s in range(2):
                P.op("dve", lambda e, l=l, s=s: e.tensor_tensor(
                    out=MOD[:, l, s, :], in0=ps[pb][:, 0:96].rearrange("p (m s) -> p m s", s=2)[:, :, s],
                    in1=SM[:, S_MB + l * 48:S_MB + (l + 1) * 48], op=ALU.add),
                    reads=psk(pb) + ["SM"], writes=[("MOD", l, s)])
                nw = lambda j, l=l: SM[:, S_NW + (l * 4 + j) * 8:S_NW + (l * 4 + j) * 8 + 8]
                md = lambda j, l=l, s=s: MOD[:, l, s, j * 8:(j + 1) * 8]
                P.op("dve", lambda e, l=l, s=s, nw=nw, md=md: e.scalar_tensor_tensor(
                    out=PAR[:, l, s, 0, :], in0=md(1), scalar=1.0, in1=nw(0), op0=ALU.add, op1=ALU.mult),
                    reads=[("MOD", l, s), "SM"], writes=[("PAR", l, s, 0)])
                P.op("dve", lambda e, l=l, s=s, nw=nw, md=md: e.tensor_tensor(
                    out=PAR[:, l, s, 1, :], in0=md(2), in1=nw(1), op=ALU.mult),
                    reads=[("MOD", l, s), "SM"], writes=[("PAR", l, s, 1)])
                P.op("dve", lambda e, l=l, s=s, nw=nw, md=md: e.scalar_tensor_tensor(
                    out=PAR[:, l, s, 2, :], in0=md(4), scalar=1.0, in1=nw(2), op0=ALU.add, op1=ALU.mult),
                    reads=[("MOD", l, s), "SM"], writes=[("PAR", l, s, 2)])
                P.op("dve", lambda e, l=l, s=s, nw=nw, md=md: e.tensor_tensor(
                    out=PAR[:, l, s, 3, :], in0=md(5), in1=nw(3), op=ALU.mult),
                    reads=[("MOD", l, s), "SM"], writes=[("PAR", l, s, 3)])
            banks.rel([pair])
            flags[("mod", l)] = True

        for _r in mod_layer(0):
            pass

        def par(l, s, which):
            if which == "a1":
                return PAR[:, l, s, 0, :], [("PAR", l, s, 0)]
            if which == "g1":
                return PAR[:, l, s, 1, :], [("PAR", l, s, 1)]
            if which == "a2":
                return PAR[:, l, s, 2, :], [("PAR", l, s, 2)]
            if which == "g2":
                return PAR[:, l, s, 3, :], [("PAR", l, s, 3)]
            if which == "sh1":
                return MOD[:, l, s, 0:8], [("MOD", l, s)]
            if which == "sh2":
                return MOD[:, l, s, 24:32], [("MOD", l, s)]
            raise KeyError(which)

        sched = [] if dry else list(sched_in)
        wstate = {"issued": 0, "used": 0}
        NSLOT = len(WRING)

        def w_issue():
            n = wstate["issued"]
            name, r0, c0 = sched[n]
            slot = n % NSLOT
            P.dma("sp", lambda e, name=name, r0=r0, c0=c0, slot=slot: e.dma_start(
                out=WRING[slot][:], in_=wsc[name][r0:r0 + 1024, c0:c0 + 512].rearrange("(k p) n -> p k n", p=128)),
                reads=wpieces[name], writes=[("WR", slot)])
            wstate["issued"] = n + 1

        def w_next(expect):
            n = wstate["used"]
            if dry:
                sched.append(expect)
            else:
                assert sched[n] == expect, (n, sched[n], expect)
                while wstate["issued"] < min(len(sched), n + NSLOT):
                    w_issue()
            wstate["used"] = n + 1
            slot = n % NSLOT
            return WRING[slot], ("WR", slot)

        def fm_mm(cx, slot, wkey, src, skey, koff, pair, first, last, ncol=4, msize=128, col0=0):
            for mi in range(ncol):
                b, c0 = reg(pair, mi, CT)

                def f(e, mi=mi, b=b, c0=c0):
                    ins = None
                    for kc in range(FC):
                        ins = e.matmul(ps[b][0:msize, c0:c0 + CT],
                                       lhsT=slot[:, kc, col0 + mi * msize:col0 + (mi + 1) * msize],
                                       rhs=src[:, koff + kc, 0:CT],
                                       start=(first and kc == 0 and mi % 2 == 0), stop=(last and kc == FC - 1),
                                       skip_group_check=True)
                    return ins
                P.op("pe", f, reads=[wkey] + [cx.k(skey, koff + kc) for kc in range(FC)], writes=psk(b))

        def rstd_from_sq(cx):
            pair = (yield from acq(1))[0]
            b = pair * 2

            def f(e):
                ins = None
                for fc in range(FC):
                    ins = e.matmul(ps[b][:, 0:CT], lhsT=ONESB[:], rhs=cx.B1[:, fc, :],
                                   start=(fc == 0), stop=(fc == FC - 1))
                return ins
            P.op("pe", f, reads=["ONESB"] + cx.ks("B1", FC), writes=psk(b))
            yield (1.0, 1.0)
            P.op("act", lambda e: e.activation(out=cx.RS[:], in_=ps[b][:, 0:CT], func=AF.Sqrt, bias=EPS_COL, scale=1.0 / D),
                 reads=psk(b) + ["SM"], writes=[cx.k("RS")])
            P.op("dve", lambda e: e.reciprocal(out=cx.RS[:], in_=cx.RS[:]), reads=[cx.k("RS")], writes=[cx.k("RS")])
            banks.rel([pair])

        def norm_mod(cx, l, s, which_a, which_sh):
            a_ap, a_k = par(l, s, which_a)
            sh_ap, sh_k = par(l, s, which_sh)
            for fc in range(FC):
                eng = ("act", "pool", "dve")[fc % 3]
                if eng == "act":
                    P.op("act", lambda e, fc=fc: e.activation(out=cx.B1[:, fc, :], in_=cx.F0[:, fc, :], func=AF.Square),
                         reads=[cx.k("F0", fc)], writes=[cx.k("B1", fc)])
                else:
                    P.op(eng, lambda e, fc=fc: e.tensor_tensor(out=cx.B1[:, fc, :], in0=cx.F0[:, fc, :], in1=cx.F0[:, fc, :],
                                                               op=ALU.mult),
                         reads=[cx.k("F0", fc)], writes=[cx.k("B1", fc)])
            yield (0.0, 4.0)
            yield from rstd_from_sq(cx)
            for fc in range(FC):
                P.op("dve", lambda e, fc=fc: e.tensor_tensor(out=cx.TMPA[:, fc % 2, :], in0=cx.F0[:, fc, :],
                                                             in1=cx.RS[:], op=ALU.mult),
                     reads=[cx.k("F0", fc), cx.k("RS")], writes=[cx.k("TMPA", fc % 2)])
                P.op("act", lambda e, fc=fc: e.activation(out=cx.B0[:, fc, :], in_=cx.TMPA[:, fc % 2, :],
                                                          func=AF.Identity, bias=sh_ap[:, fc:fc + 1],
                                                          scale=a_ap[:, fc:fc + 1]),
                     reads=[cx.k("TMPA", fc % 2)] + a_k + sh_k, writes=[cx.k("B0", fc)])
            yield (0.0, 9.0)

        def evac_branch(cx, pair, fc0):
            for mi in range(4):
                fc = fc0 + mi
                b, c0 = reg(pair, mi, CT)
                if mi % 2 == 0:
                    P.op("dve", lambda e, fc=fc, b=b, c0=c0: e.tensor_copy(out=cx.F1[:, fc, :], in_=ps[b][:, c0:c0 + CT]),
                         reads=psk(b), writes=[cx.k("F1", fc)])
                else:
                    P.op("act", lambda e, fc=fc, b=b, c0=c0: e.activation(out=cx.F1[:, fc, :], in_=ps[b][:, c0:c0 + CT], func=AF.Copy),
                         reads=psk(b), writes=[cx.k("F1", fc)])
                P.op("pool", lambda e, fc=fc: e.tensor_tensor(out=cx.B1[:, fc, :], in0=cx.F1[:, fc, :], in1=cx.F1[:, fc, :],
                                                              op=ALU.mult),
                     reads=[cx.k("F1", fc)], writes=[cx.k("B1", fc)])

        def residual(cx, l, s, which_g):
            g_ap, g_k = par(l, s, which_g)
            yield from rstd_from_sq(cx)
            for fc in range(FC):
                P.op("dve", lambda e, fc=fc: e.scalar_tensor_tensor(
                    out=cx.TMPA[:, fc % 2, :], in0=cx.F1[:, fc, :], scalar=g_ap[:, fc:fc + 1],
                    in1=cx.RS[:], op0=ALU.mult, op1=ALU.mult),
                    reads=[cx.k("F1", fc), cx.k("RS")] + g_k, writes=[cx.k("TMPA", fc % 2)])
                P.op("pool", lambda e, fc=fc: e.tensor_tensor(out=cx.F0[:, fc, :], in0=cx.F0[:, fc, :],
                                                              in1=cx.TMPA[:, fc % 2, :], op=ALU.add),
                     reads=[cx.k("F0", fc), cx.k("TMPA", fc % 2)], writes=[cx.k("F0", fc)])
            yield (0.0, 8.0)

        def mlp(cx, l, s):
            yield from norm_mod(cx, l, s, "a2", "sh2")
            w1n, w2n = f"w1_{l}", f"w2_{l}"
            for j in range(8):
                pair = (yield from acq(1))[0]
                slot, wkey = w_next((w1n, 0, 512 * j))
                fm_mm(cx, slot, wkey, cx.B0, "B0", 0, pair, True, True)
                for mi in range(4):
                    hc = 4 * j + mi
                    b, c0 = reg(pair, mi, CT)
                    P.op("act", lambda e, b=b, c0=c0, hc=hc: e.activation(out=cx.TMPA[:, hc % 2, :], in_=ps[b][:, c0:c0 + CT],
                                                                          func=AF.Relu),
                         reads=psk(b), writes=[cx.k("TMPA", hc % 2)])
                    eng = "dve" if (hc % 2 == 0) else "pool"
                    P.op(eng, lambda e, hc=hc: e.tensor_tensor(out=cx.HID[:, hc, :], in0=cx.TMPA[:, hc % 2, :],
                                                               in1=cx.TMPA[:, hc % 2, :], op=ALU.mult),
                         reads=[cx.k("TMPA", hc % 2)], writes=[cx.k("HID", hc)])
                banks.rel([pair])
                yield (3.7, 1.5)
            for ch in range(2):
                pair = (yield from acq(1))[0]
                for kg in range(4):
                    slot, wkey = w_next((w2n, 1024 * kg, 512 * ch))
                    fm_mm(cx, slot, wkey, cx.HID, "HID", 8 * kg, pair, kg == 0, kg == 3)
                    if kg < 3:
                        yield (3.7, 0.0)
                evac_branch(cx, pair, 4 * ch)
                banks.rel([pair])
                yield (3.7, 4.0)
            yield from residual(cx, l, s, "g2")

        def load_tokens(cx, src_rows):
            xtok = cx.F2[:].rearrange("p a b -> p (a b)").rearrange("p (t f) -> p t f", f=D)
            P.dma("sp", lambda e: e.dma_start(out=xtok, in_=src_rows.rearrange("(t p) f -> p t f", p=128)),
                  writes=cx.ks("F2", FC))
            yield (0.0, 2.0)
            for g in range(2):
                pair = (yield from acq(1))[0]
                for q in range(4):
                    fc = g * 4 + q
                    b, c0 = reg(pair, q, CT)

                    def f(e, fc=fc, b=b, c0=c0):
                        ins = None
                        for tt in range(NCH):
                            ins = e.transpose(out=ps[b][:, c0 + tt * 128:c0 + (tt + 1) * 128],
                                              in_=xtok[:, tt, fc * 128:(fc + 1) * 128], identity=IDENT)
                        return ins
                    P.op("pe", f, reads=cx.ks("F2", FC) + ["CST"], writes=psk(b))
                    if fc % 2 == 0:
                        P.op("act", lambda e, fc=fc, b=b, c0=c0: e.activation(out=cx.F0[:, fc, :], in_=ps[b][:, c0:c0 + CT], func=AF.Copy),
                             reads=psk(b), writes=[cx.k("F0", fc)])
                    else:
                        P.op("dve", lambda e, fc=fc, b=b, c0=c0: e.tensor_copy(out=cx.F0[:, fc, :], in_=ps[b][:, c0:c0 + CT]),
                             reads=psk(b), writes=[cx.k("F0", fc)])
                banks.rel([pair])
                yield (1.8, 1.5)

        def layer0(cx, s, rowlen):
            yield from norm_mod(cx, 0, s, "a1", "sh1")
            BG = lambda fc: cx.HID[:, fc, :]
            CG = lambda fc: cx.HID[:, 8 + fc, :]
            GG = lambda fc: cx.HID[:, 16 + fc, :]
            for j in range(6):
                pair = (yield from acq(1))[0]
                slot, wkey = w_next(("win0", 0, 512 * j))
                fm_mm(cx, slot, wkey, cx.B0, "B0", 0, pair, True, True)
                for mi in range(4):
                    b, c0 = reg(pair, mi, CT)
                    m = 4 * j + mi
                    src = ps[b][:, c0:c0 + CT]
                    if m < 8:
                        P.op("act", lambda e, m=m, src=src: e.activation(out=BG(m), in_=src, func=AF.Copy),
                             reads=psk(b), writes=[cx.k("HID", m)])
                    elif m < 16:
                        P.op("act", lambda e, m=m, src=src: e.activation(out=CG(m - 8), in_=src, func=AF.Copy),
                             reads=psk(b), writes=[cx.k("HID", m)])
                    else:
                        fc = m - 16
                        P.op("dve", lambda e, fc=fc, src=src: e.tensor_tensor(out=cx.F2[:, fc, :], in0=src, in1=CG(fc), op=ALU.mult),
                             reads=psk(b) + [cx.k("HID", 8 + fc)], writes=[cx.k("F2", fc)])
                banks.rel([pair])
                yield (3.7, 2.0)
            cw = lambda k, fc: SM[:, S_CW + k * 8 + fc:S_CW + k * 8 + fc + 1]
            for fc in range(FC):
                yv = cx.TMPA[:, fc % 2, :]
                y3 = yv.rearrange("p (r w) -> p r w", w=rowlen)
                u3 = cx.F2[:, fc, :].rearrange("p (r w) -> p r w", w=rowlen)
                P.op("act", lambda e, fc=fc, yv=yv: e.activation(out=yv, in_=cx.F2[:, fc, :], func=AF.Identity,
                                                                 bias=0.0, scale=cw(1, fc)),
                     reads=[cx.k("F2", fc), "SM"], writes=[cx.k("TMPA", fc % 2)])
                P.op("dve", lambda e, fc=fc, y3=y3, u3=u3: e.scalar_tensor_tensor(
                    out=y3[:, :, 1:rowlen], in0=u3[:, :, 0:rowlen - 1], scalar=cw(0, fc), in1=y3[:, :, 1:rowlen],
                    op0=ALU.mult, op1=ALU.add),
                    reads=[cx.k("F2", fc), cx.k("TMPA", fc % 2), "SM"], writes=[cx.k("TMPA", fc % 2)])
                P.op("dve", lambda e, fc=fc, y3=y3, u3=u3: e.scalar_tensor_tensor(
                    out=y3[:, :, 0:rowlen - 1], in0=u3[:, :, 1:rowlen], scalar=cw(2, fc), in1=y3[:, :, 0:rowlen - 1],
                    op0=ALU.mult, op1=ALU.add),
                    reads=[cx.k("F2", fc), cx.k("TMPA", fc % 2), "SM"], writes=[cx.k("TMPA", fc % 2)])
                P.op("pool", lambda e, fc=fc, yv=yv: e.tensor_tensor(out=GG(fc), in0=yv, in1=BG(fc), op=ALU.mult),
                     reads=[cx.k("TMPA", fc % 2), cx.k("HID", fc)], writes=[cx.k("HID", 16 + fc)])
                if fc % 4 == 3:
                    yield (0.0, 6.0)
            for j in range(2):
                pair = (yield from acq(1))[0]
                slot, wkey = w_next(("wout0", 0, 512 * j))
                fm_mm(cx, slot, wkey, cx.HID, "HID", 16, pair, True, True)
                evac_branch(cx, pair, 4 * j)
                banks.rel([pair])
                yield (3.7, 4.0)
            yield from residual(cx, 0, s, "g1")
            yield from mlp(cx, 0, s)

        def gates_mm(cx, gi0):
            pair = (yield from acq(1))[0]
            bank = pair * 2

            def f(e):
                ins = None
                for c in range(NCH):
                    for kc in range(FC):
                        ins = e.matmul(ps[bank][:, c * 32:(c + 1) * 32], lhsT=cx.B0[:, kc, c * 128:(c + 1) * 128],
                                       rhs=WIFB[:, kc, :], start=(kc == 0), stop=(kc == FC - 1))
                return ins
            P.op("pe", f, reads=["WIFB"] + cx.ks("B0", FC), writes=psk(bank))
            for c in range(NCH):
                P.op("dve", lambda e, c=c: e.tensor_tensor(out=GT[:, gi0 + c, :], in0=ps[bank][:, c * 32:(c + 1) * 32],
                                                           in1=SM[:, S_BIF:S_BIF + 32], op=ALU.add),
                     reads=psk(bank) + ["SM"], writes=[("GT", gi0 + c)])
            banks.rel([pair])
            yield (1.0, 1.5)

        def scan_chunk(cx, d, gi, c, with_out, first_dir=True):
            pairs = yield from acq(2)
            bY = [pairs[0] * 2, pairs[0] * 2 + 1]
            bX = pairs[1] * 2
            gb = pairs[1] * 2 + 1
            QT = cx.HID[:, 0:8, :]
            KT = cx.HID[:, 8:16, :]
            igs = GT[:, gi, d * 16:d * 16 + 8]
            fps = GT[:, gi, d * 16 + 8:d * 16 + 16]
            gk = [("GT", gi)]
            P.op("act", lambda e: e.activation(out=LFN[:, 0:8], in_=fps, func=AF.Exp, scale=-1.0),
                 reads=gk, writes=[("LFN", 0)])
            P.op("act", lambda e: e.activation(out=LFN[:, 8:16], in_=LFN[:, 0:8], func=AF.Ln, bias=ONE_COL, scale=1.0),
                 reads=[("LFN", 0), "SM"], writes=[("LFN", 1)])
            lfn = LFN[:, 8:16]
            yield (0.0, 1.5)

            def g3(e):
                e.matmul(ps[gb][:, 0:8], lhsT=MINC[d], rhs=lfn, start=True, stop=True)
                e.matmul(ps[gb][:, 8:16], lhsT=MSTR[d], rhs=lfn, start=True, stop=True)
                return e.matmul(ps[gb][:, 16:24], lhsT=NEG1, rhs=lfn, start=True, stop=True)
            P.op("pe", g3, reads=[("LFN", 1), "CST"], writes=psk(gb))
            C1 = G3S[:, 0:8]
            EB = G3S[:, 8:16]
            WKP = G3S[:, 16:24]
            WK = G3S[:, 24:32]
            AT = G3S[:, 32:40]
            if with_out:
                for h in range(NH):
                    P.op("pool", lambda e, h=h: e.tensor_scalar(out=LFB[:, h, :], in0=ONES32, scalar1=LFN[:, 8 + h:9 + h],
                                                                scalar2=None, op0=ALU.mult),
                         reads=[("LFN", 1), "CST"], writes=[("LFB", h)])
            yield (0.3, 1.5)
            if with_out:
                P.op("dve", lambda e: e.tensor_tensor(out=C1, in0=igs, in1=ps[gb][:, 0:8], op=ALU.subtract),
                     reads=gk + psk(gb), writes=[("G3S", 0)])
            P.op("dve", lambda e: e.tensor_tensor(out=WKP, in0=igs, in1=ps[gb][:, 8:16], op=ALU.add),
                 reads=gk + psk(gb), writes=[("G3S", 2)])
            if with_out:
                P.op("act", lambda e: e.activation(out=EB, in_=ps[gb][:, 0:8], func=AF.Exp),
                     reads=psk(gb), writes=[("G3S", 1)])
            P.op("act", lambda e: e.activation(out=AT, in_=ps[gb][:, 16:24], func=AF.Exp),
                 reads=psk(gb), writes=[("G3S", 4)])
            P.op("act", lambda e: e.activation(out=WK, in_=WKP, func=AF.Exp), reads=[("G3S", 2)], writes=[("G3S", 3)])
            for h in range(NH):
                P.op("pool", lambda e, h=h: e.tensor_scalar(out=KW[:, h, :], in0=cx.KTOK[:, c, h * 64:(h + 1) * 64],
                                                            scalar1=G3S[:, 24 + h:25 + h], scalar2=None, op0=ALU.mult),
                     reads=[cx.k("KTOK", c), ("G3S", 3)], writes=[("KW", h)])
            yield (0.0, 3.0)
            cs = slice(c * 128, (c + 1) * 128)
            vk = cx.k("VAUG", c)

            def head_front(h):
                by = bY[h % 2]
                rot = h % 2

                def bbm(e):
                    e.matmul(ps[by][:, 0:128], lhsT=LFB[:, h, :], rhs=MINC[d], start=True, stop=False)
                    e.matmul(ps[by][:, 0:128], lhsT=IDENT, rhs=NBIGM[d], start=False, stop=True)
                    return e.matmul(ps[by][:, 128:256], lhsT=KT[0:64, h, cs], rhs=QT[0:64, h, cs], start=True, stop=True)
                P.op("pe", bbm, reads=[("LFB", h), "CST", cx.k("HID", 8 + h), cx.k("HID", h)], writes=psk(by))
                P.op("act", lambda e: e.activation(out=DTT[:, rot, :], in_=ps[by][:, 0:128], func=AF.Exp,
                                                   bias=G3S[:, h:h + 1], scale=1.0),
                     reads=psk(by) + [("G3S", 0)], writes=[("DTT", rot)])
                P.op("dve", lambda e: e.tensor_tensor(out=PTT[:, rot, :], in0=ps[by][:, 128:256], in1=DTT[:, rot, :], op=ALU.mult),
                     reads=psk(by) + [("DTT", rot)], writes=[("PTT", rot)])

            def head_back(h):
                rot = h % 2
                vrhs = cx.VAUG[:, c, h, 0:DV + 1]

                def mm(e):
                    ins = None
                    if with_out:
                        e.matmul(ps[bX][:, 0:DV + 1], lhsT=PTT[:, rot, :], rhs=vrhs, start=True, stop=True)
                        e.matmul(ps[bX][:, 130:130 + DV + 1], lhsT=QT[0:64, h, cs], rhs=CST_B[d][:, h, 0:DV + 1],
                                 start=True, stop=True)
                    return e.matmul(ps[bX][0:64, 260:260 + DV + 1], lhsT=KW[:, h, :], rhs=vrhs, start=True, stop=True)
                rd = [("KW", h), vk]
                if with_out:
                    rd += [("PTT", rot), cx.k("HID", h), ("CB", d, h)]
                P.op("pe", mm, reads=rd, writes=psk(bX))
                if with_out:
                    P.op("act", lambda e: e.activation(out=T0[:, rot, 0:DV + 1], in_=ps[bX][:, 0:DV + 1], func=AF.Copy),
                         reads=psk(bX), writes=[("T0", rot)])
                    P.op("dve", lambda e: e.scalar_tensor_tensor(
                        out=T1[:, h, 0:DV + 1], in0=ps[bX][:, 130:130 + DV + 1], scalar=G3S[:, 8 + h:9 + h],
                        in1=T0[:, rot, 0:DV + 1], op0=ALU.mult, op1=ALU.add),
                        reads=psk(bX) + [("G3S", 1), ("T0", rot)], writes=[("T1", h)])
                P.op("dve", lambda e: e.scalar_tensor_tensor(
                    out=CST_C[d][:, h, 0:DV + 1], in0=CST_C[d][:, h, 0:DV + 1], scalar=G3S[0:64, 32 + h:33 + h],
                    in1=ps[bX][0:64, 260:260 + DV + 1], op0=ALU.mult, op1=ALU.add),
                    reads=[("CS", d, h), ("G3S", 4)] + psk(bX), writes=[("CS", d, h)])
                P.op("act", lambda e: e.activation(out=CST_B[d][:, h, 0:DV + 1], in_=CST_C[d][:, h, 0:DV + 1], func=AF.Copy),
                     reads=[("CS", d, h)], writes=[("CB", d, h)])

            if with_out:
                head_front(0)
                yield (0.7, 1.5)
            for h in range(NH):
                if with_out and h + 1 < NH:
                    head_front(h + 1)
                head_back(h)
                yield (1.0, 1.5)
            if with_out:
                den = T1[:, :, DV]
                P.op("act", lambda e: e.activation(out=RD[:, 0:8], in_=den, func=AF.Abs),
                     reads=[("T1", h) for h in range(NH)], writes=[("RD", 0)])
                P.op("dve", lambda e: e.tensor_scalar_max(out=RD[:, 0:8], in0=RD[:, 0:8], scalar1=1.0),
                     reads=[("RD", 0)], writes=[("RD", 0)])
                P.op("dve", lambda e: e.reciprocal(out=RD[:, 8:16], in_=RD[:, 0:8]), reads=[("RD", 0)], writes=[("RD", 1)])
                hs = cx.F1[:].rearrange("p a b -> p (a b)").rearrange("p (c f) -> p c f", f=D)
                for h in range(NH):
                    dst = hs[:, c, h * DV:(h + 1) * DV]
                    key = cx.k("F1", 4 * c + h // 2)
                    if first_dir:
                        eng = "act" if h % 2 == 0 else "pool"
                        if eng == "act":
                            P.op("act", lambda e, h=h, dst=dst: e.activation(out=dst, in_=T1[:, h, 0:DV], func=AF.Identity,
                                                                             bias=0.0, scale=RD[:, 8 + h:9 + h]),
                                 reads=[("T1", h), ("RD", 1)], writes=[key])
                        else:
                            P.op("pool", lambda e, h=h, dst=dst: e.tensor_scalar(out=dst, in0=T1[:, h, 0:DV],
                                                                                 scalar1=RD[:, 8 + h:9 + h], scalar2=None,
                                                                                 op0=ALU.mult),
                                 reads=[("T1", h), ("RD", 1)], writes=[key])
                    else:
                        P.op("dve", lambda e, h=h, dst=dst: e.scalar_tensor_tensor(
                            out=dst, in0=T1[:, h, 0:DV], scalar=RD[:, 8 + h:9 + h], in1=dst, op0=ALU.mult, op1=ALU.add),
                            reads=[("T1", h), ("RD", 1), key], writes=[key])
            banks.rel(pairs)
            yield (0.0, 2.0)

        def l1_proj(cx, full):
            if full:
                pairs = yield from acq(2)
                slot, wkey = w_next(("qkvo", 0, 0))
                for g in range(2):
                    pair = pairs[g]
                    fm_mm(cx, slot, wkey, cx.B0, "B0", 0, pair, True, True, ncol=4, msize=64, col0=g * 256)
                    for q in range(4):
                        h = g * 4 + q
                        b, c0 = reg(pair, q, CT)
                        P.op("act", lambda e, h=h, b=b, c0=c0: e.activation(out=cx.HID[0:64, h, :], in_=ps[b][0:64, c0:c0 + CT],
                                                                            func=AF.Copy, scale=DQK ** -0.5),
                             reads=psk(b), writes=[cx.k("HID", h)])
                banks.rel(pairs)
                yield (3.7, 2.0)
            pairs = yield from acq(2)
            slot, wkey = w_next(("qkvo", 0, 512))
            if full:
                for g in range(2):
                    pair = pairs[g]
                    fm_mm(cx, slot, wkey, cx.B0, "B0", 0, pair, True, True, ncol=4, msize=64, col0=g * 256)
                    for q in range(4):
                        h = g * 4 + q
                        b, c0 = reg(pair, q, CT)
                        P.op("dve", lambda e, h=h, b=b, c0=c0: e.tensor_copy(out=cx.HID[0:64, 8 + h, :], in_=ps[b][0:64, c0:c0 + CT]),
                             reads=psk(b), writes=[cx.k("HID", 8 + h)])
            pair = pairs[0]
            for c in range(NCH):
                b = pair * 2 + c

                def f(e, c=c, b=b, slot=slot):
                    ins = None
                    for kc in range(FC):
                        ins = e.matmul(ps[b][:, 0:512], lhsT=cx.B0[:, kc, c * 128:(c + 1) * 128], rhs=slot[:, kc, :],
                                       start=(kc == 0), stop=(kc == FC - 1))
                    return ins
                P.op("pe", f, reads=[wkey] + cx.ks("B0", FC), writes=psk(b))
                P.op("act", lambda e, c=c, b=b: e.activation(out=cx.KTOK[:, c, :], in_=ps[b][:, 0:512], func=AF.Copy),
                     reads=psk(b), writes=[cx.k("KTOK", c)])
            banks.rel(pairs)
            yield (5.5, 2.0)
            for j in range(2):
                pair = (yield from acq(1))[0]
                slot, wkey = w_next(("qkvo", 0, 1024 + 512 * j))
                for c in range(NCH):
                    b = pair * 2 + c

                    def f(e, c=c, b=b, slot=slot):
                        ins = None
                        for kc in range(FC):
                            ins = e.matmul(ps[b][:, 0:512], lhsT=cx.B0[:, kc, c * 128:(c + 1) * 128], rhs=slot[:, kc, :],
                                           start=(kc == 0), stop=(kc == FC - 1))
                        return ins
                    P.op("pe", f, reads=[wkey] + cx.ks("B0", FC), writes=psk(b))
                    dst = cx.VAUG[:, c, 4 * j:4 * j + 4, 0:DV]
                    src = ps[b][:, 0:512].rearrange("p (h v) -> p h v", v=DV)
                    if c % 2 == 0:
                        P.op("dve", lambda e, dst=dst, src=src: e.tensor_copy(out=dst, in_=src),
                             reads=psk(b), writes=[cx.k("VAUG", c)])
                    else:
                        P.op("act", lambda e, dst=dst, src=src: e.activation(out=dst, in_=src, func=AF.Copy),
                             reads=psk(b), writes=[cx.k("VAUG", c)])
                banks.rel([pair])
                yield (3.6, 2.0)
            if full:
                for j in range(2):
                    pair = (yield from acq(1))[0]
                    slot, wkey = w_next(("qkvo", 0, 2048 + 512 * j))
                    fm_mm(cx, slot, wkey, cx.B0, "B0", 0, pair, True, True)
                    for mi in range(4):
                        h = 4 * j + mi
                        b, c0 = reg(pair, mi, CT)
                        P.op("act", lambda e, h=h, b=b, c0=c0: e.activation(out=cx.HID[:, 16 + h, :], in_=ps[b][:, c0:c0 + CT], func=AF.Sigmoid),
                             reads=psk(b), writes=[cx.k("HID", 16 + h)])
                    banks.rel([pair])
                    yield (3.7, 2.0)


        def wait_flag(name):
            while not flags.get(name):
                yield "blocked"

        flat3 = lambda t: t[:].rearrange("p a b -> p (a b)")

        def ctx_context(cx):
            yield from load_tokens(cx, ctx_d)
            yield from layer0(cx, 1, CTX)
            yield from wait_flag(("mod", 1))
            yield from norm_mod(cx, 1, 1, "a1", "sh1")
            yield from l1_proj(cx, False)
            yield from gates_mm(cx, 2 * NCX)
            for d in range(2):
                order = [0, 1] if d == 0 else [1, 0]
                for c in order:
                    yield from scan_chunk(cx, d, 2 * NCX + c, c, False)
            flags[("fscan", -1)] = True
            flags[("bscan", NCX)] = True

        def sweep1_context(cx, i):
            yield from load_tokens(cx, x_d[i * CT:(i + 1) * CT, :])
            yield from layer0(cx, 0, 64)
            yield from wait_flag(("mod", 1))
            P.dma("sp", lambda e: e.dma_start(out=x1_s[i], in_=flat3(cx.F0)), reads=cx.ks("F0", FC), writes=[("x1_s", i)])
            if DEBUG:
                P.dma("sp", lambda e: e.dma_start(out=dbg_d[i], in_=flat3(cx.F0)), reads=cx.ks("F0", FC), writes=[("dbg", i)])
            yield from norm_mod(cx, 1, 0, "a1", "sh1")
            yield from l1_proj(cx, True)
            yield from gates_mm(cx, 2 * i)
            P.dma("sp", lambda e: e.dma_start(out=qt_s[i], in_=cx.HID[0:64, 0:8, :].rearrange("p a b -> p (a b)")),
                  reads=[cx.k("HID", h) for h in range(8)], writes=[("qt_s", i)])
            P.dma("sp", lambda e: e.dma_start(out=kt_s[i], in_=cx.HID[0:64, 8:16, :].rearrange("p a b -> p (a b)")),
                  reads=[cx.k("HID", 8 + h) for h in range(8)], writes=[("kt_s", i)])
            P.dma("sp", lambda e: e.dma_start(out=so_s[i], in_=cx.HID[:, 16:24, :].rearrange("p a b -> p (a b)")),
                  reads=[cx.k("HID", 16 + h) for h in range(8)], writes=[("so_s", i)])
            P.dma("sp", lambda e: e.dma_start(out=ktok_s[i], in_=flat3(cx.KTOK)), reads=cx.ks("KTOK", NCH), writes=[("ktok_s", i)])
            P.dma("sp", lambda e: e.dma_start(out=vaug_s[i], in_=cx.VAUG[:].rearrange("p a b c -> p (a b c)")),
                  reads=cx.ks("VAUG", NCH), writes=[("vaug_s", i)])
            yield from wait_flag(("fscan", i - 1))
            for c in range(NCH):
                yield from scan_chunk(cx, 0, 2 * i + c, c, True, first_dir=True)
            flags[("fscan", i)] = True
            P.dma("sp", lambda e: e.dma_start(out=hf_s[i], in_=flat3(cx.F1)), reads=cx.ks("F1", FC), writes=[("hf_s", i)])
            flags[("s1done", i)] = True
            yield (0.0, 0.0)

        def sweep2_context(cx, i):
            yield from wait_flag(("s1done", i))
            P.dma("sp", lambda e: e.dma_start(out=cx.HID[0:64, 0:8, :].rearrange("p a b -> p (a b)"), in_=qt_s[i]),
                  reads=[("qt_s", i)], writes=[cx.k("HID", h) for h in range(8)])
            P.dma("sp", lambda e: e.dma_start(out=cx.HID[0:64, 8:16, :].rearrange("p a b -> p (a b)"), in_=kt_s[i]),
                  reads=[("kt_s", i)], writes=[cx.k("HID", 8 + h) for h in range(8)])
            P.dma("sp", lambda e: e.dma_start(out=flat3(cx.KTOK), in_=ktok_s[i]), reads=[("ktok_s", i)], writes=cx.ks("KTOK", NCH))
            P.dma("sp", lambda e: e.dma_start(out=cx.VAUG[:].rearrange("p a b c -> p (a b c)"), in_=vaug_s[i]),
                  reads=[("vaug_s", i)], writes=cx.ks("VAUG", NCH))
            P.dma("sp", lambda e: e.dma_start(out=flat3(cx.F1), in_=hf_s[i]), reads=[("hf_s", i)], writes=cx.ks("F1", FC))
            P.dma("sp", lambda e: e.dma_start(out=cx.HID[:, 16:24, :].rearrange("p a b -> p (a b)"), in_=so_s[i]),
                  reads=[("so_s", i)], writes=[cx.k("HID", 16 + h) for h in range(8)])
            P.dma("sp", lambda e: e.dma_start(out=flat3(cx.F0), in_=x1_s[i]), reads=[("x1_s", i)], writes=cx.ks("F0", FC))
            yield (0.0, 3.0)
            yield from wait_flag(("bscan", i + 1))
            hs = cx.F1[:].rearrange("p a b -> p (a b)").rearrange("p (c f) -> p c f", f=D)
            for c in reversed(range(NCH)):
                yield from scan_chunk(cx, 1, 2 * i + c, c, True, first_dir=False)
                if c == 0:
                    flags[("bscan", i)] = True
                hkeys = [cx.k("F1", 4 * c + q) for q in range(4)]
                sq = cx.F2[:].rearrange("p a b -> p (a b)")[:, 0:D]
                sqk = [cx.k("F2", q) for q in range(4)]
                P.op("pool", lambda e, c=c, sq=sq: e.tensor_tensor(out=sq, in0=hs[:, c, :], in1=hs[:, c, :], op=ALU.mult),
                     reads=hkeys, writes=sqk)
                P.op("dve", lambda e, sq=sq: e.tensor_reduce(out=cx.SSQ[:, 0:8], in_=sq.rearrange("p (h v) -> p h v", v=DV),
                                                             axis=AX.X, op=ALU.add),
                     reads=sqk, writes=[cx.k("SSQ", 0)])
                P.op("act", lambda e: e.activation(out=cx.SSQ[:, 8:16], in_=cx.SSQ[:, 0:8], func=AF.Sqrt, bias=EPS_COL, scale=1.0 / DV),
                     reads=[cx.k("SSQ", 0), "SM"], writes=[cx.k("SSQ", 1)])
                P.op("dve", lambda e: e.reciprocal(out=cx.SSQ[:, 8:16], in_=cx.SSQ[:, 8:16]), reads=[cx.k("SSQ", 1)], writes=[cx.k("SSQ", 1)])
                yield (0.0, 3.0)
                pair = (yield from acq(1))[0]
                for h in range(NH):
                    rot = h % 2
                    b = pair * 2 + rot
                    P.op("act", lambda e, c=c, h=h, rot=rot: e.activation(out=cx.HN[:, rot, :], in_=hs[:, c, h * DV:(h + 1) * DV],
                                                                          func=AF.Identity, bias=0.0, scale=cx.SSQ[:, 8 + h:9 + h]),
                         reads=hkeys + [cx.k("SSQ", 1)], writes=[cx.k("HN", rot)])
                    P.op("pe", lambda e, rot=rot, b=b: e.transpose(out=ps[b][:, 0:128], in_=cx.HN[:, rot, :], identity=IDENT),
                         reads=[cx.k("HN", rot), "CST"], writes=psk(b))
                    P.op("dve", lambda e, c=c, h=h, b=b: e.scalar_tensor_tensor(
                        out=cx.HID[:, 24 + h, c * 128:(c + 1) * 128], in0=ps[b][:, 0:128],
                        scalar=SM[:, S_MLNW + h:S_MLNW + h + 1], in1=cx.HID[:, 16 + h, c * 128:(c + 1) * 128],
                        op0=ALU.mult, op1=ALU.mult),
                        reads=psk(b) + ["SM", cx.k("HID", 16 + h)], writes=[cx.k("HID", 24 + h)])
                    if h % 2 == 1:
                        yield (0.5, 1.5)
                banks.rel([pair])
            for j in range(2):
                pair = (yield from acq(1))[0]
                slot, wkey = w_next(("mlout", 0, 512 * j))
                fm_mm(cx, slot, wkey, cx.HID, "HID", 24, pair, True, True)
                evac_branch(cx, pair, 4 * j)
                banks.rel([pair])
                yield (3.7, 4.0)
            yield from residual(cx, 1, 0, "g1")
            yield from mlp(cx, 1, 0)
            xt = cx.F2[:].rearrange("p a b -> p (a b)").rearrange("p (t f) -> p t f", f=D)
            for tt in range(NCH):
                pair = (yield from acq(1))[0]
                for half in range(2):
                    b = pair * 2 + half

                    def f(e, tt=tt, half=half, b=b):
                        ins = None
                        for q in range(4):
                            fc = half * 4 + q
                            ins = e.transpose(out=ps[b][:, q * 128:(q + 1) * 128], in_=cx.F0[:, fc, tt * 128:(tt + 1) * 128],
                                              identity=IDENT)
                        return ins
                    P.op("pe", f, reads=[cx.k("F0", half * 4 + q) for q in range(4)] + ["CST"], writes=psk(b))
                    wk = [cx.k("F2", tt * 4 + half * 2), cx.k("F2", tt * 4 + half * 2 + 1)]
                    if half == 0:
                        P.op("act", lambda e, tt=tt, half=half, b=b: e.activation(out=xt[:, tt, half * 512:(half + 1) * 512],
                                                                                  in_=ps[b][:, 0:512], func=AF.Copy),
                             reads=psk(b), writes=wk)
                    else:
                        P.op("dve", lambda e, tt=tt, half=half, b=b: e.tensor_copy(out=xt[:, tt, half * 512:(half + 1) * 512],
                                                                                   in_=ps[b][:, 0:512]),
                             reads=psk(b), writes=wk)
                banks.rel([pair])
                yield (1.8, 2.0)
            P.dma("sp", lambda e: e.dma_start(out=out_d[i * CT:(i + 1) * CT, :].rearrange("(t p) f -> p t f", p=128), in_=xt),
                  reads=cx.ks("F2", FC), writes=[("out", i)])
            yield (0.0, 0.0)

        jobs = [("ctx", None)] + [("s1", i) for i in range(NCX_RUN)] + [("s2", i) for i in reversed(range(NCX_RUN))]
        if STOP == "ctx":
            jobs = jobs[:1]
        elif STOP == "sweep1":
            jobs = jobs[:1 + NCX_RUN]

        def make(job, cx):
            kind, i = job
            if kind == "ctx":
                return ctx_context(cx)
            if kind == "s1":
                return sweep1_context(cx, i)
            return sweep2_context(cx, i)

        if NCX_RUN < NCX:
            for i in range(NCX_RUN, NCX):
                flags[("s1done", i)] = True
            flags[("bscan", NCX_RUN)] = True
        active = [[mod_layer(1), None, 0.0]]
        free_cx = [CXS[0], CXS[1]]
        pending = list(jobs)

        pe_clock = 0.0
        guard = 0
        while active or pending:
            guard += 1
            assert guard < 2000000, "scheduler livelock"
            while pending and free_cx:
                cx_ = free_cx.pop(0)
                active.append([make(pending.pop(0), cx_), cx_, pe_clock])
            order = sorted(active, key=lambda a_: max(a_[2], pe_clock))
            progressed = False
            for ent in order:
                try:
                    r = next(ent[0])
                except StopIteration:
                    active.remove(ent)
                    if ent[1] is not None:
                        free_cx.append(ent[1])
                    progressed = True
                    break
                if r == "blocked":
                    continue
                pe_us, lat_us = r if r is not None else (0.0, 0.0)
                start = max(pe_clock, ent[2])
                pe_clock = start + pe_us
                ent[2] = pe_clock + lat_us
                progressed = True
                break
            assert progressed, "all pipeline contexts blocked"
        assert not pending
        assert dry or wstate["used"] == len(sched), (wstate, len(sched))
        if dry:
            return sched
        P.emit()
    return nc


_NC_CACHE = {}


def kernel(x, c, ctx, c_ctx, norm_w, mod_w, mod_b, mlp_w1, mlp_w2, conv_w_in, conv_w, conv_w_out,
           ml_w_qkvo, ml_w_if, ml_b_if, ml_norm_w, ml_w_out):
    f32 = lambda a: np.ascontiguousarray(np.asarray(a, dtype=np.float32))
    x = f32(x); c = f32(c); ctx = f32(ctx); c_ctx = f32(c_ctx)
    if "nc" not in _NC_CACHE:
        _NC_CACHE["nc"] = build_nc()
    nc = _NC_CACHE["nc"]
    consts = _consts()
    shared = {
        "consts": consts, "mod_w": f32(mod_w), "mlp_w1": f32(mlp_w1), "mlp_w2": f32(mlp_w2),
        "conv_w_in": f32(conv_w_in[0]), "conv_w_out": f32(conv_w_out[0]),
        "ml_w_qkvo": f32(ml_w_qkvo[0]), "ml_w_out": f32(ml_w_out[0]),
    }
    in_maps = []
    for b in range(NCORES):
        m = dict(shared)
        m["x"] = x[b]
        m["ctx"] = ctx[b]
        m["small"] = _small(c[b], c_ctx, f32(norm_w), f32(mod_b), f32(conv_w), f32(ml_norm_w), f32(ml_b_if), f32(ml_w_if))
        in_maps.append(m)
    res = run_bass_kernel_spmd(nc, in_maps, core_ids=list(range(NCORES)))
    _NC_CACHE["last"] = res
    return np.stack([np.asarray(r["out"], dtype=np.float32) for r in res.results], axis=0)
```

```python
import contextlib
import numpy as np
import concourse.bass as bass
import concourse.mybir as mybir
from concourse.bass_utils import run_bass_kernel_spmd

F32 = mybir.dt.float32
BF16 = mybir.dt.bfloat16
AF = mybir.ActivationFunctionType
ALU = mybir.AluOpType
AX = mybir.AxisListType

D = 1024
FC = 8
SEQ = 4096
CTX = 256
TT = 512
NT = SEQ // TT
DFF = 4096
NH = 8
DQK = 64
DV = 128
VS = 130
EPS = 1e-6
DEBUG = False
STOP = None
NCORES = 8
GG_ENG = "dve"
NCX_RUN = 16


class _Stop(Exception):
    pass

SEM_EPOCH = 12000
N_DMA_SEMS = 10


class Op:
    __slots__ = ("eng", "fn", "reads", "writes", "dma", "deps", "sig", "signo", "dsem", "dval")

    def __init__(self, eng, fn, reads, writes, dma):
        self.eng = eng
        self.fn = fn
        self.reads = reads
        self.writes = writes
        self.dma = dma
        self.deps = None
        self.sig = False
        self.signo = 0
        self.dsem = None
        self.dval = 0


class Prog:
    ENGS = ("pe", "act", "dve", "pool", "sp")

    def __init__(self, nc):
        self.nc = nc
        self.ops = []

    def op(self, eng, fn, reads=(), writes=()):
        self.ops.append(Op(eng, fn, tuple(reads), tuple(writes), False))

    def dma(self, eng, fn, reads=(), writes=()):
        self.ops.append(Op(eng, fn, tuple(reads), tuple(writes), True))

    def emit(self):
        nc = self.nc
        ops = self.ops
        last_w = {}
        readers = {}
        for i, o in enumerate(ops):
            deps = {}
            for b in o.reads:
                j = last_w.get(b)
                if j is not None:
                    deps[j] = True
                if b[0] == "ps":
                    for j in readers.get(b, ()):
                        if ops[j].eng != o.eng:
                            deps.setdefault(j, False)
            for b in o.writes:
                j = last_w.get(b)
                if j is not None:
                    deps.setdefault(j, False)
                for j in readers.get(b, ()):
                    deps.setdefault(j, False)
            deps.pop(i, None)
            o.deps = deps
            for b in o.reads:
                readers.setdefault(b, []).append(i)
            for b in o.writes:
                last_w[b] = i
                readers[b] = []
        for o in ops:
            for j, raw in o.deps.items():
                p = ops[j]
                if p.dma:
                    continue
                if o.dma or p.eng != o.eng:
                    p.sig = True
                elif raw and o.eng in ("act", "dve", "pool"):
                    p.sig = True
        cnt = {e: 0 for e in self.ENGS}
        for o in ops:
            if o.sig and not o.dma:
                cnt[o.eng] += 1
                o.signo = cnt[o.eng]
        engobj = {"pe": nc.tensor, "act": nc.scalar, "dve": nc.vector, "pool": nc.gpsimd, "sp": nc.sync}
        with contextlib.ExitStack() as st:
            esems = {}
            for e in self.ENGS:
                n_ep = (cnt[e] + SEM_EPOCH - 1) // SEM_EPOCH
                esems[e] = [st.enter_context(nc.semaphore(f"s_{e}_{k}")) for k in range(n_ep)]
            dsems = {}
            for e in ("sp", "pool", "act"):
                if any(o.dma and o.eng == e for o in ops):
                    dsems[e] = [st.enter_context(nc.semaphore(f"d_{e}_{k}")) for k in range(N_DMA_SEMS)]
            duse = {e: [0] * N_DMA_SEMS for e in dsems}
            dlast = {e: [None] * N_DMA_SEMS for e in dsems}
            dnext = {e: 0 for e in dsems}
            seen = {e: {} for e in self.ENGS}

            def wait(e, sem, key, val):
                s = seen[e]
                if s.get(key, 0) >= val:
                    return
                s[key] = val
                engobj[e].wait_ge(sem, val)

            def wait_op(e, p):
                if p.dma:
                    wait(e, p.dsem, ("d", id(p.dsem)), p.dval)
                else:
                    k = (p.signo - 1) // SEM_EPOCH
                    v = (p.signo - 1) % SEM_EPOCH + 1
                    wait(e, esems[p.eng][k], (p.eng, k), v)

            for o in ops:
                e = o.eng
                for j, raw in o.deps.items():
                    p = ops[j]
                    if p.dma or o.dma or p.eng != e:
                        wait_op(e, p)
                    elif raw and e in ("act", "dve", "pool"):
                        wait_op(e, p)
                if o.dma:
                    k = dnext[e]
                    dnext[e] = (k + 1) % N_DMA_SEMS
                    prev = dlast[e][k]
                    if prev is not None:
                        wait_op(e, prev)
                    duse[e][k] += 1
                    o.dsem = dsems[e][k]
                    o.dval = 16 * duse[e][k]
                    dlast[e][k] = o
                    ins = o.fn(engobj[e])
                    ins.then_inc(o.dsem, 16)
                else:
                    ins = o.fn(engobj[e])
                    if o.sig:
                        k = (o.signo - 1) // SEM_EPOCH
                        ins.then_inc(esems[e][k], 1)
            for e in dsems:
                for k in range(N_DMA_SEMS):
                    p = dlast[e][k]
                    if p is not None:
                        wait_op(e, p)


C_IDENT, C_MINC0, C_MINC1, C_MSTR0, C_MSTR1, C_NEG1, C_NBIG0, C_NBIG1, C_ONES = range(9)
NBIG = -30000.0


def _consts():
    u = np.arange(128)[:, None]
    t = np.arange(128)[None, :]
    mats = [
        (u == t).astype(np.float32),
        -(u <= t).astype(np.float32),
        -(u >= t).astype(np.float32),
        -(u > t).astype(np.float32),
        -(u < t).astype(np.float32),
        -np.ones((128, 128), np.float32),
        NBIG * (t < u).astype(np.float32),
        NBIG * (t > u).astype(np.float32),
        np.ones((128, 128), np.float32),
    ]
    return np.ascontiguousarray(np.concatenate(mats, axis=1))


S_CC = 0
S_NW = 16
S_MB = 80
S_CW = 176
S_MLNW = 200
S_BIF = 208
S_WIF = 240
S_MISC = 496
NSMALL = 500


def _fm(v):
    v = np.asarray(v, np.float32)
    lead = v.shape[:-1]
    a = v.reshape(lead + (FC, 128))
    a = np.moveaxis(a, -1, 0)
    return a


def _small(c_b, c_ctx, norm_w, mod_b, conv_w, ml_norm_w, ml_b_if, ml_w_if):
    s = np.zeros((128, NSMALL), np.float32)
    cc = np.stack([_fm(c_b), _fm(c_ctx)], axis=-1)
    s[:, S_CC:S_CC + 16] = cc.reshape(128, 16)
    s[:, S_NW:S_NW + 64] = _fm(norm_w).reshape(128, 64)
    mb = np.asarray(mod_b, np.float32).reshape(2, 48, 128)
    s[:, S_MB:S_MB + 96] = np.moveaxis(mb, -1, 0).reshape(128, 96)
    s[:, S_CW:S_CW + 24] = _fm(conv_w[0]).reshape(128, 24)
    s[:, S_MLNW:S_MLNW + 8] = _fm(ml_norm_w[0]).reshape(128, 8)
    s[:, S_BIF:S_BIF + 32] = np.asarray(ml_b_if[0], np.float32).reshape(1, 32)
    wif = np.concatenate([ml_w_if[0, 0], ml_w_if[0, 1]], axis=1)
    wif = wif.reshape(FC, 128, 32).transpose(1, 0, 2)
    s[:, S_WIF:S_WIF + 256] = wif.reshape(128, 256)
    s[:, S_MISC + 0] = 1.0
    s[:, S_MISC + 1] = EPS
    return s


CT = 256
NCX = SEQ // CT
NCH = CT // 128
SKEW = 40


class _BankAlloc:
    def __init__(self):
        self.free = [0, 1, 2, 3]

    def try_acq(self, n):
        if len(self.free) < n:
            return None
        r = self.free[:n]
        self.free = self.free[n:]
        return r

    def rel(self, pairs):
        self.free = self.free + list(pairs)


def build_nc():
    sched = _build(None)
    return _build(sched)


def _build(sched_in):
    dry = sched_in is None
    nc = bass.Bass("TRN2", target_bir_lowering=False)
    dt_in = lambda n, shp: nc.dram_tensor(n, list(shp), F32, kind="ExternalInput").ap()
    x_d = dt_in("x", [SEQ, D])
    ctx_d = dt_in("ctx", [CTX, D])
    small_d = dt_in("small", [128, NSMALL])
    consts_d = dt_in("consts", [128, 9 * 128])
    mod_w_d = dt_in("mod_w", [2, D, 6 * D])
    mlp_w1_d = dt_in("mlp_w1", [2, D, DFF])
    mlp_w2_d = dt_in("mlp_w2", [2, DFF, D])
    conv_w_in_d = dt_in("conv_w_in", [D, 3 * D])
    conv_w_out_d = dt_in("conv_w_out", [D, D])
    ml_w_qkvo_d = dt_in("ml_w_qkvo", [D, 3 * D])
    ml_w_out_d = dt_in("ml_w_out", [D, D])
    out_d = nc.dram_tensor("out", [SEQ, D], F32, kind="ExternalOutput").ap()

    def scratch(n, shp, dt):
        return nc.dram_tensor(n, list(shp), dt).ap()

    wsrc = {
        "win0": conv_w_in_d, "wout0": conv_w_out_d, "w1_0": mlp_w1_d[0], "w2_0": mlp_w2_d[0],
        "qkvo": ml_w_qkvo_d, "mlout": ml_w_out_d, "w1_1": mlp_w1_d[1], "w2_1": mlp_w2_d[1],
    }
    wsc = {n: scratch("sc_" + n, a.shape, BF16) for n, a in wsrc.items()}
    x1_s = scratch("x1_s", [NCX, 128, FC * CT], F32)
    qt_s = scratch("qt_s", [NCX, 64, NH * CT], BF16)
    kt_s = scratch("kt_s", [NCX, 64, NH * CT], BF16)
    ktok_s = scratch("ktok_s", [NCX, 128, NCH * 512], BF16)
    vaug_s = scratch("vaug_s", [NCX, 128, NCH * NH * VS], BF16)
    so_s = scratch("so_s", [NCX, 128, NH * CT], BF16)
    hf_s = scratch("hf_s", [NCX, 128, NCH * D], F32)
    dbg_d = None
    if DEBUG:
        dbg_d = nc.dram_tensor("dbg", [NCX, 128, FC * CT], F32, kind="ExternalOutput").ap()

    with contextlib.ExitStack() as st:
        def sb(name, shape, dt):
            return st.enter_context(nc.sbuf_tensor(name, list(shape), dt))

        P = Prog(nc)
        ps = [st.enter_context(nc.psum_tensor(f"ps{i}", [128, 512], F32)) for i in range(8)]
        banks = _BankAlloc()

        SM = sb("SM", [128, NSMALL], F32)
        CST = sb("CST", [128, 9 * 128], F32)
        ONESB = sb("ONESB", [128, 128], BF16)
        WIFB = sb("WIFB", [128, FC, 32], BF16)
        SIL = sb("SIL", [128, FC, 2], F32)
        MOD = sb("MOD", [128, 2, 2, 48], F32)
        PAR = sb("PAR", [128, 2, 2, 4, 8], F32)
        GT = sb("GT", [128, 2 * NCX + 2, 32], F32)
        WRING = [sb(f"WR{i}", [128, FC, 512], BF16) for i in range(4)]
        LFN = sb("LFN", [128, 16], F32)
        G3S = sb("G3S", [128, 40], F32)
        LFB = sb("LFB", [128, NH, 128], F32)
        DTT = sb("DTT", [128, 2, 128], F32)
        PTT = sb("PTT", [128, 2, 128], BF16)
        KW = sb("KW", [128, NH, 64], BF16)
        T0 = sb("T0", [128, 2, VS], F32)
        T1 = sb("T1", [128, NH, VS], F32)
        RD = sb("RD", [128, 16], F32)
        CST_C = [sb(f"CS{d}", [64, NH, VS], F32) for d in range(2)]
        CST_B = [sb(f"CB{d}", [64, NH, VS], BF16) for d in range(2)]
        HN = sb("HN", [128, 2, 128], F32)
        SSQ = sb("SSQ", [128, 16], F32)

        class Cx:
            def __init__(self, i):
                self.i = i
                self.F0 = sb(f"F0_{i}", [128, FC, CT], F32)
                self.F1 = sb(f"F1_{i}", [128, FC, CT], F32)
                self.F2 = sb(f"F2_{i}", [128, FC, CT], F32)
                self.B0 = sb(f"B0_{i}", [128, FC, CT], BF16)
                self.B1 = sb(f"B1_{i}", [128, FC, CT], BF16)
                self.HID = sb(f"HID_{i}", [128, 32, CT], BF16)
                self.RS = sb(f"RS_{i}", [128, CT], F32)
                self.TMPA = sb(f"TMPA_{i}", [128, 2, CT], F32)
                self.KTOK = sb(f"KTOK_{i}", [128, NCH, 512], BF16)
                self.VAUG = sb(f"VAUG_{i}", [128, NCH, NH, VS], BF16)
                self.SSQ = sb(f"SSQ_{i}", [128, 16], F32)
                self.HN = sb(f"HN_{i}", [128, 2, 128], F32)

            def k(self, name, idx=None):
                return (name + str(self.i), idx)

            def ks(self, name, n):
                return [(name + str(self.i), j) for j in range(n)]

        CXS = [Cx(0), Cx(1)]
        MODBUF = [sb(f"MODBUF{i}", [128, FC, CT], F32) for i in range(2)]

        IDENT = CST[:, C_IDENT * 128:(C_IDENT + 1) * 128]
        MINC = [CST[:, C_MINC0 * 128:(C_MINC0 + 1) * 128], CST[:, C_MINC1 * 128:(C_MINC1 + 1) * 128]]
        MSTR = [CST[:, C_MSTR0 * 128:(C_MSTR0 + 1) * 128], CST[:, C_MSTR1 * 128:(C_MSTR1 + 1) * 128]]
        NEG1 = CST[:, C_NEG1 * 128:(C_NEG1 + 1) * 128]
        NBIGM = [CST[:, C_NBIG0 * 128:(C_NBIG0 + 1) * 128], CST[:, C_NBIG1 * 128:(C_NBIG1 + 1) * 128]]
        ONES32 = CST[:, C_ONES * 128:(C_ONES + 1) * 128]
        ONE_COL = SM[:, S_MISC:S_MISC + 1]
        EPS_COL = SM[:, S_MISC + 1:S_MISC + 2]

        def psk(b):
            return [("ps", b)]

        def acq(n=1):
            while True:
                r = banks.try_acq(n)
                if r is not None:
                    return r
                yield "blocked"

        def reg(pair, mi, ntok):
            b = pair * 2 + mi // 2
            c0 = (mi % 2) * 256
            return b, c0

        P.dma("sp", lambda e: e.dma_start(out=SM[:], in_=small_d), writes=["SM"])
        P.dma("sp", lambda e: e.dma_start(out=CST[:], in_=consts_d), writes=["CST"])
        P.op("act", lambda e: e.activation(out=ONESB[:], in_=ONES32, func=AF.Copy), reads=["CST"], writes=["ONESB"])
        P.op("act", lambda e: e.activation(
            out=WIFB[:], in_=SM[:, S_WIF:S_WIF + 256].rearrange("p (k j) -> p k j", k=FC), func=AF.Copy),
            reads=["SM"], writes=["WIFB"])
        P.op("act", lambda e: e.activation(
            out=SIL[:], in_=SM[:, S_CC:S_CC + 16].rearrange("p (k j) -> p k j", k=FC), func=AF.Silu),
            reads=["SM"], writes=["SIL"])
        for d in range(2):
            P.op("pool", lambda e, d=d: e.memset(CST_C[d][:].rearrange("p a b -> p (a b)"), 0.0),
                 writes=[("CS", d, h) for h in range(NH)])
            P.op("pool", lambda e, d=d: e.memset(CST_B[d][:].rearrange("p a b -> p (a b)"), 0.0),
                 writes=[("CB", d, h) for h in range(NH)])
        for cx in CXS:
            P.op("pool", lambda e, cx=cx: e.memset(cx.VAUG[:].rearrange("p a b c -> p (a b c)"), 1.0),
                 writes=cx.ks("VAUG", NCH))

        PIECE = 1 << 20
        wpieces = {}
        for n, src in wsrc.items():
            R, C = src.shape
            npc = 1
            while R * C // npc > PIECE:
                npc *= 2
            rp = R // npc
            keys = []
            for r0 in range(0, R, rp):
                key = ("wsc", n, r0)
                keys.append(key)
                P.dma("pool", lambda e, n=n, src=src, r0=r0, rp=rp: e.dma_start(
                    out=wsc[n][r0:r0 + rp, :], in_=src[r0:r0 + rp, :]), writes=[key])
            wpieces[n] = keys

        flags = {}

        def mod_layer(l):
            pair = (yield from acq(1))[0]
            pb = pair * 2
            for nb in range(24):
                buf = MODBUF[nb % 2]
                bkey = [("MODBUF", nb % 2)]
                P.dma("sp", lambda e, l=l, nb=nb, buf=buf: e.dma_start(
                    out=buf[:], in_=mod_w_d[l, :, nb * 256:(nb + 1) * 256].rearrange("(k p) n -> p k n", p=128)),
                    writes=bkey)

                def mm_mod(e, nb=nb, buf=buf):
                    ins = None
                    for mi in range(2):
                        col = (nb * 2 + mi) * 2
                        for kc in range(FC):
                            ins = e.matmul(ps[pb][:, col:col + 2], lhsT=buf[:, kc, mi * 128:(mi + 1) * 128],
                                           rhs=SIL[:, kc, :], start=(kc == 0), stop=(kc == FC - 1))
                    return ins
                P.op("pe", mm_mod, reads=bkey + ["SIL"], writes=psk(pb))
                yield (1.6, 25.0)
            for s in range(2):
                P.op("dve", lambda e, l=l, s=s: e.tensor_tensor(
                    out=MOD[:, l, s, :], in0=ps[pb][:, 0:96].rearrange("p (m s) -> p m s", s=2)[:, :, s],
                    in1=SM[:, S_MB + l * 48:S_MB + (l + 1) * 48], op=ALU.add),
                    reads=psk(pb) + ["SM"], writes=[("MOD", l, s)])
                nw = lambda j, l=l: SM[:, S_NW + (l * 4 + j) * 8:S_NW + (l * 4 + j) * 8 + 8]
                md = lambda j, l=l, s=s: MOD[:, l, s, j * 8:(j + 1) * 8]
                P.op("dve", lambda e, l=l, s=s, nw=nw, md=md: e.scalar_tensor_tensor(
                    out=PAR[:, l, s, 0, :], in0=md(1), scalar=1.0, in1=nw(0), op0=ALU.add, op1=ALU.mult),
                    reads=[("MOD", l, s), "SM"], writes=[("PAR", l, s, 0)])
                P.op("dve", lambda e, l=l, s=s, nw=nw, md=md: e.tensor_tensor(
                    out=PAR[:, l, s, 1, :], in0=md(2), in1=nw(1), op=ALU.mult),
                    reads=[("MOD", l, s), "SM"], writes=[("PAR", l, s, 1)])
                P.op("dve", lambda e, l=l, s=s, nw=nw, md=md: e.scalar_tensor_tensor(
                    out=PAR[:, l, s, 2, :], in0=md(4), scalar=1.0, in1=nw(2), op0=ALU.add, op1=ALU.mult),
                    reads=[("MOD", l, s), "SM"], writes=[("PAR", l, s, 2)])
                P.op("dve", lambda e, l=l, s=s, nw=nw, md=md: e.tensor_tensor(
                    out=PAR[:, l, s, 3, :], in0=md(5), in1=nw(3), op=ALU.mult),
                    reads=[("MOD", l, s), "SM"], writes=[("PAR", l, s, 3)])
            banks.rel([pair])
            flags[("mod", l)] = True

        for _r in mod_layer(0):
            pass

        def par(l, s, which):
            if which == "a1":
                return PAR[:, l, s, 0, :], [("PAR", l, s, 0)]
            if which == "g1":
                return PAR[:, l, s, 1, :], [("PAR", l, s, 1)]
            if which == "a2":
                return PAR[:, l, s, 2, :], [("PAR", l, s, 2)]
            if which == "g2":
                return PAR[:, l, s, 3, :], [("PAR", l, s, 3)]
            if which == "sh1":
                return MOD[:, l, s, 0:8], [("MOD", l, s)]
            if which == "sh2":
                return MOD[:, l, s, 24:32], [("MOD", l, s)]
            raise KeyError(which)

        sched = [] if dry else list(sched_in)
        wstate = {"issued": 0, "used": 0}
        NSLOT = len(WRING)

        def w_issue():
            n = wstate["issued"]
            name, r0, c0 = sched[n]
            slot = n % NSLOT
            P.dma("sp", lambda e, name=name, r0=r0, c0=c0, slot=slot: e.dma_start(
                out=WRING[slot][:], in_=wsc[name][r0:r0 + 1024, c0:c0 + 512].rearrange("(k p) n -> p k n", p=128)),
                reads=wpieces[name], writes=[("WR", slot)])
            wstate["issued"] = n + 1

        def w_next(expect):
            n = wstate["used"]
            if dry:
                sched.append(expect)
            else:
                assert sched[n] == expect, (n, sched[n], expect)
                while wstate["issued"] < min(len(sched), n + NSLOT):
                    w_issue()
            wstate["used"] = n + 1
            slot = n % NSLOT
            return WRING[slot], ("WR", slot)

        def fm_mm(cx, slot, wkey, src, skey, koff, pair, first, last, ncol=4, msize=128, col0=0):
            for mi in range(ncol):
                b, c0 = reg(pair, mi, CT)

                def f(e, mi=mi, b=b, c0=c0):
                    ins = None
                    for kc in range(FC):
                        ins = e.matmul(ps[b][0:msize, c0:c0 + CT],
                                       lhsT=slot[:, kc, col0 + mi * msize:col0 + (mi + 1) * msize],
                                       rhs=src[:, koff + kc, 0:CT],
                                       start=(first and kc == 0 and mi % 2 == 0), stop=(last and kc == FC - 1),
                                       skip_group_check=True)
                    return ins
                P.op("pe", f, reads=[wkey] + [cx.k(skey, koff + kc) for kc in range(FC)], writes=psk(b))

        def rstd_from_sq(cx):
            pair = (yield from acq(1))[0]
            b = pair * 2

            def f(e):
                ins = None
                for fc in range(FC):
                    ins = e.matmul(ps[b][:, 0:CT], lhsT=ONESB[:], rhs=cx.B1[:, fc, :],
                                   start=(fc == 0), stop=(fc == FC - 1))
                return ins
            P.op("pe", f, reads=["ONESB"] + cx.ks("B1", FC), writes=psk(b))
            yield (1.0, 1.0)
            P.op("act", lambda e: e.activation(out=cx.RS[:], in_=ps[b][:, 0:CT], func=AF.Sqrt, bias=EPS_COL, scale=1.0 / D),
                 reads=psk(b) + ["SM"], writes=[cx.k("RS")])
            P.op("dve", lambda e: e.reciprocal(out=cx.RS[:], in_=cx.RS[:]), reads=[cx.k("RS")], writes=[cx.k("RS")])
            banks.rel([pair])

        def norm_mod(cx, l, s, which_a, which_sh):
            a_ap, a_k = par(l, s, which_a)
            sh_ap, sh_k = par(l, s, which_sh)
            for fc in range(FC):
                eng = ("act", "pool", "dve")[fc % 3]
                if eng == "act":
                    P.op("act", lambda e, fc=fc: e.activation(out=cx.B1[:, fc, :], in_=cx.F0[:, fc, :], func=AF.Square),
                         reads=[cx.k("F0", fc)], writes=[cx.k("B1", fc)])
                else:
                    P.op(eng, lambda e, fc=fc: e.tensor_tensor(out=cx.B1[:, fc, :], in0=cx.F0[:, fc, :], in1=cx.F0[:, fc, :],
                                                               op=ALU.mult),
                         reads=[cx.k("F0", fc)], writes=[cx.k("B1", fc)])
            yield (0.0, 8.0)
            yield from rstd_from_sq(cx)
            for fc in range(FC):
                P.op("dve", lambda e, fc=fc: e.tensor_tensor(out=cx.TMPA[:, fc % 2, :], in0=cx.F0[:, fc, :],
                                                             in1=cx.RS[:], op=ALU.mult),
                     reads=[cx.k("F0", fc), cx.k("RS")], writes=[cx.k("TMPA", fc % 2)])
                P.op("act", lambda e, fc=fc: e.activation(out=cx.B0[:, fc, :], in_=cx.TMPA[:, fc % 2, :],
                                                          func=AF.Identity, bias=sh_ap[:, fc:fc + 1],
                                                          scale=a_ap[:, fc:fc + 1]),
                     reads=[cx.k("TMPA", fc % 2)] + a_k + sh_k, writes=[cx.k("B0", fc)])
            yield (0.0, 9.0)

        def evac_branch(cx, pair, fc0):
            for mi in range(4):
                fc = fc0 + mi
                b, c0 = reg(pair, mi, CT)
                if mi % 2 == 0:
                    P.op("dve", lambda e, fc=fc, b=b, c0=c0: e.tensor_copy(out=cx.F1[:, fc, :], in_=ps[b][:, c0:c0 + CT]),
                         reads=psk(b), writes=[cx.k("F1", fc)])
                else:
                    P.op("act", lambda e, fc=fc, b=b, c0=c0: e.activation(out=cx.F1[:, fc, :], in_=ps[b][:, c0:c0 + CT], func=AF.Copy),
                         reads=psk(b), writes=[cx.k("F1", fc)])
                P.op("pool", lambda e, fc=fc: e.tensor_tensor(out=cx.B1[:, fc, :], in0=cx.F1[:, fc, :], in1=cx.F1[:, fc, :],
                                                              op=ALU.mult),
                     reads=[cx.k("F1", fc)], writes=[cx.k("B1", fc)])

        def residual(cx, l, s, which_g):
            g_ap, g_k = par(l, s, which_g)
            yield from rstd_from_sq(cx)
            for fc in range(FC):
                P.op("dve", lambda e, fc=fc: e.scalar_tensor_tensor(
                    out=cx.TMPA[:, fc % 2, :], in0=cx.F1[:, fc, :], scalar=g_ap[:, fc:fc + 1],
                    in1=cx.RS[:], op0=ALU.mult, op1=ALU.mult),
                    reads=[cx.k("F1", fc), cx.k("RS")] + g_k, writes=[cx.k("TMPA", fc % 2)])
                P.op("pool", lambda e, fc=fc: e.tensor_tensor(out=cx.F0[:, fc, :], in0=cx.F0[:, fc, :],
                                                              in1=cx.TMPA[:, fc % 2, :], op=ALU.add),
                     reads=[cx.k("F0", fc), cx.k("TMPA", fc % 2)], writes=[cx.k("F0", fc)])
            yield (0.0, 8.0)

        def mlp(cx, l, s):
            yield from norm_mod(cx, l, s, "a2", "sh2")
            w1n, w2n = f"w1_{l}", f"w2_{l}"
            for j in range(8):
                pair = (yield from acq(1))[0]
                slot, wkey = w_next((w1n, 0, 512 * j))
                fm_mm(cx, slot, wkey, cx.B0, "B0", 0, pair, True, True)
                for mi in range(4):
                    hc = 4 * j + mi
                    b, c0 = reg(pair, mi, CT)
                    P.op("act", lambda e, b=b, c0=c0, hc=hc: e.activation(out=cx.TMPA[:, hc % 2, :], in_=ps[b][:, c0:c0 + CT],
                                                                          func=AF.Relu),
                         reads=psk(b), writes=[cx.k("TMPA", hc % 2)])
                    eng = "dve" if (hc % 2 == 0) else "pool"
                    P.op(eng, lambda e, hc=hc: e.tensor_tensor(out=cx.HID[:, hc, :], in0=cx.TMPA[:, hc % 2, :],
                                                               in1=cx.TMPA[:, hc % 2, :], op=ALU.mult),
                         reads=[cx.k("TMPA", hc % 2)], writes=[cx.k("HID", hc)])
                banks.rel([pair])
                yield (3.7, 1.5)
            for ch in range(2):
                pair = (yield from acq(1))[0]
                for kg in range(4):
                    slot, wkey = w_next((w2n, 1024 * kg, 512 * ch))
                    fm_mm(cx, slot, wkey, cx.HID, "HID", 8 * kg, pair, kg == 0, kg == 3)
                    if kg < 3:
                        yield (3.7, 0.0)
                evac_branch(cx, pair, 4 * ch)
                banks.rel([pair])
                yield (3.7, 4.0)
            yield from residual(cx, l, s, "g2")

        def load_tokens(cx, src_rows):
            xtok = cx.F2[:].rearrange("p a b -> p (a b)").rearrange("p (t f) -> p t f", f=D)
            P.dma("sp", lambda e: e.dma_start(out=xtok, in_=src_rows.rearrange("(t p) f -> p t f", p=128)),
                  writes=cx.ks("F2", FC))
            yield (0.0, 12.0)
            for g in range(2):
                pair = (yield from acq(1))[0]
                for q in range(4):
                    fc = g * 4 + q
                    b, c0 = reg(pair, q, CT)

                    def f(e, fc=fc, b=b, c0=c0):
                        ins = None
                        for tt in range(NCH):
                            ins = e.transpose(out=ps[b][:, c0 + tt * 128:c0 + (tt + 1) * 128],
                                              in_=xtok[:, tt, fc * 128:(fc + 1) * 128], identity=IDENT)
                        return ins
                    P.op("pe", f, reads=cx.ks("F2", FC) + ["CST"], writes=psk(b))
                    if fc % 2 == 0:
                        P.op("act", lambda e, fc=fc, b=b, c0=c0: e.activation(out=cx.F0[:, fc, :], in_=ps[b][:, c0:c0 + CT], func=AF.Copy),
                             reads=psk(b), writes=[cx.k("F0", fc)])
                    else:
                        P.op("dve", lambda e, fc=fc, b=b, c0=c0: e.tensor_copy(out=cx.F0[:, fc, :], in_=ps[b][:, c0:c0 + CT]),
                             reads=psk(b), writes=[cx.k("F0", fc)])
                banks.rel([pair])
                yield (1.8, 1.5)

        def layer0(cx, s, rowlen):
            yield from norm_mod(cx, 0, s, "a1", "sh1")
            BG = lambda fc: cx.HID[:, fc, :]
            CG = lambda fc: cx.HID[:, 8 + fc, :]
            GG = lambda fc: cx.HID[:, 16 + fc, :]
            for j in range(6):
                pair = (yield from acq(1))[0]
                slot, wkey = w_next(("win0", 0, 512 * j))
                fm_mm(cx, slot, wkey, cx.B0, "B0", 0, pair, True, True)
                for mi in range(4):
                    b, c0 = reg(pair, mi, CT)
                    m = 4 * j + mi
                    src = ps[b][:, c0:c0 + CT]
                    if m < 8:
                        P.op("act", lambda e, m=m, src=src: e.activation(out=BG(m), in_=src, func=AF.Copy),
                             reads=psk(b), writes=[cx.k("HID", m)])
                    elif m < 16:
                        P.op("act", lambda e, m=m, src=src: e.activation(out=CG(m - 8), in_=src, func=AF.Copy),
                             reads=psk(b), writes=[cx.k("HID", m)])
                    else:
                        fc = m - 16
                        P.op("dve", lambda e, fc=fc, src=src: e.tensor_tensor(out=cx.F2[:, fc, :], in0=src, in1=CG(fc), op=ALU.mult),
                             reads=psk(b) + [cx.k("HID", 8 + fc)], writes=[cx.k("F2", fc)])
                banks.rel([pair])
                yield (3.7, 2.0)
            cw = lambda k, fc: SM[:, S_CW + k * 8 + fc:S_CW + k * 8 + fc + 1]
            for fc in range(FC):
                yv = cx.TMPA[:, fc % 2, :]
                y3 = yv.rearrange("p (r w) -> p r w", w=rowlen)
                u3 = cx.F2[:, fc, :].rearrange("p (r w) -> p r w", w=rowlen)
                P.op("act", lambda e, fc=fc, yv=yv: e.activation(out=yv, in_=cx.F2[:, fc, :], func=AF.Identity,
                                                                 bias=0.0, scale=cw(1, fc)),
                     reads=[cx.k("F2", fc), "SM"], writes=[cx.k("TMPA", fc % 2)])
                P.op("dve", lambda e, fc=fc, y3=y3, u3=u3: e.scalar_tensor_tensor(
                    out=y3[:, :, 1:rowlen], in0=u3[:, :, 0:rowlen - 1], scalar=cw(0, fc), in1=y3[:, :, 1:rowlen],
                    op0=ALU.mult, op1=ALU.add),
                    reads=[cx.k("F2", fc), cx.k("TMPA", fc % 2), "SM"], writes=[cx.k("TMPA", fc % 2)])
                P.op("dve", lambda e, fc=fc, y3=y3, u3=u3: e.scalar_tensor_tensor(
                    out=y3[:, :, 0:rowlen - 1], in0=u3[:, :, 1:rowlen], scalar=cw(2, fc), in1=y3[:, :, 0:rowlen - 1],
                    op0=ALU.mult, op1=ALU.add),
                    reads=[cx.k("F2", fc), cx.k("TMPA", fc % 2), "SM"], writes=[cx.k("TMPA", fc % 2)])
                P.op("pool", lambda e, fc=fc, yv=yv: e.tensor_tensor(out=GG(fc), in0=yv, in1=BG(fc), op=ALU.mult),
                     reads=[cx.k("TMPA", fc % 2), cx.k("HID", fc)], writes=[cx.k("HID", 16 + fc)])
                if fc % 4 == 3:
                    yield (0.0, 6.0)
            for j in range(2):
                pair = (yield from acq(1))[0]
                slot, wkey = w_next(("wout0", 0, 512 * j))
                fm_mm(cx, slot, wkey, cx.HID, "HID", 16, pair, True, True)
                evac_branch(cx, pair, 4 * j)
                banks.rel([pair])
                yield (3.7, 4.0)
            yield from residual(cx, 0, s, "g1")
            yield from mlp(cx, 0, s)

        def gates_mm(cx, gi0):
            pair = (yield from acq(1))[0]
            bank = pair * 2

            def f(e):
                ins = None
                for c in range(NCH):
                    for kc in range(FC):
                        ins = e.matmul(ps[bank][:, c * 32:(c + 1) * 32], lhsT=cx.B0[:, kc, c * 128:(c + 1) * 128],
                                       rhs=WIFB[:, kc, :], start=(kc == 0), stop=(kc == FC - 1))
                return ins
            P.op("pe", f, reads=["WIFB"] + cx.ks("B0", FC), writes=psk(bank))
            for c in range(NCH):
                P.op("dve", lambda e, c=c: e.tensor_tensor(out=GT[:, gi0 + c, :], in0=ps[bank][:, c * 32:(c + 1) * 32],
                                                           in1=SM[:, S_BIF:S_BIF + 32], op=ALU.add),
                     reads=psk(bank) + ["SM"], writes=[("GT", gi0 + c)])
            banks.rel([pair])
            yield (1.0, 1.5)

        def scan_chunk(cx, d, gi, c, with_out, first_dir=True):
            pairs = yield from acq(2)
            bY = [pairs[0] * 2, pairs[0] * 2 + 1]
            bXs = [pairs[1] * 2, pairs[1] * 2 + 1]
            gb = pairs[1] * 2 + 1
            QT = cx.HID[:, 0:8, :]
            KT = cx.HID[:, 8:16, :]
            igs = GT[:, gi, d * 16:d * 16 + 8]
            fps = GT[:, gi, d * 16 + 8:d * 16 + 16]
            gk = [("GT", gi)]
            P.op("act", lambda e: e.activation(out=LFN[:, 0:8], in_=fps, func=AF.Exp, scale=-1.0),
                 reads=gk, writes=[("LFN", 0)])
            P.op("act", lambda e: e.activation(out=LFN[:, 8:16], in_=LFN[:, 0:8], func=AF.Ln, bias=ONE_COL, scale=1.0),
                 reads=[("LFN", 0), "SM"], writes=[("LFN", 1)])
            lfn = LFN[:, 8:16]
            yield (0.0, 3.0)

            def g3(e):
                e.matmul(ps[gb][:, 0:8], lhsT=MINC[d], rhs=lfn, start=True, stop=True)
                e.matmul(ps[gb][:, 8:16], lhsT=MSTR[d], rhs=lfn, start=True, stop=True)
                return e.matmul(ps[gb][:, 16:24], lhsT=NEG1, rhs=lfn, start=True, stop=True)
            P.op("pe", g3, reads=[("LFN", 1), "CST"], writes=psk(gb))
            C1 = G3S[:, 0:8]
            EB = G3S[:, 8:16]
            WKP = G3S[:, 16:24]
            WK = G3S[:, 24:32]
            AT = G3S[:, 32:40]
            if with_out:
                for h in range(NH):
                    if h % 2 == 0:
                        P.op("dve", lambda e, h=h: e.tensor_scalar(out=LFB[:, h, :], in0=ONES32, scalar1=LFN[:, 8 + h:9 + h],
                                                                   scalar2=None, op0=ALU.mult),
                             reads=[("LFN", 1), "CST"], writes=[("LFB", h)])
                    else:
                        P.op("act", lambda e, h=h: e.activation(out=LFB[:, h, :], in_=ONES32, func=AF.Identity, bias=0.0,
                                                                scale=LFN[:, 8 + h:9 + h]),
                             reads=[("LFN", 1), "CST"], writes=[("LFB", h)])
            yield (0.3, 5.0)
            if with_out:
                P.op("dve", lambda e: e.tensor_tensor(out=C1, in0=igs, in1=ps[gb][:, 0:8], op=ALU.subtract),
                     reads=gk + psk(gb), writes=[("G3S", 0)])
            P.op("dve", lambda e: e.tensor_tensor(out=WKP, in0=igs, in1=ps[gb][:, 8:16], op=ALU.add),
                 reads=gk + psk(gb), writes=[("G3S", 2)])
            if with_out:
                P.op("act", lambda e: e.activation(out=EB, in_=ps[gb][:, 0:8], func=AF.Exp),
                     reads=psk(gb), writes=[("G3S", 1)])
            P.op("act", lambda e: e.activation(out=AT, in_=ps[gb][:, 16:24], func=AF.Exp),
                 reads=psk(gb), writes=[("G3S", 4)])
            P.op("act", lambda e: e.activation(out=WK, in_=WKP, func=AF.Exp), reads=[("G3S", 2)], writes=[("G3S", 3)])
            for h in range(NH):
                if h % 2 == 0:
                    P.op("dve", lambda e, h=h: e.tensor_scalar(out=KW[:, h, :], in0=cx.KTOK[:, c, h * 64:(h + 1) * 64],
                                                               scalar1=G3S[:, 24 + h:25 + h], scalar2=None, op0=ALU.mult),
                         reads=[cx.k("KTOK", c), ("G3S", 3)], writes=[("KW", h)])
                else:
                    P.op("act", lambda e, h=h: e.activation(out=KW[:, h, :], in_=cx.KTOK[:, c, h * 64:(h + 1) * 64],
                                                            func=AF.Identity, bias=0.0, scale=G3S[:, 24 + h:25 + h]),
                         reads=[cx.k("KTOK", c), ("G3S", 3)], writes=[("KW", h)])
            yield (0.0, 6.0)
            cs = slice(c * 128, (c + 1) * 128)
            vk = cx.k("VAUG", c)

            def head_front(h):
                by = bY[h % 2]
                rot = h % 2

                def bbm(e):
                    e.matmul(ps[by][:, 0:128], lhsT=LFB[:, h, :], rhs=MINC[d], start=True, stop=False)
                    e.matmul(ps[by][:, 0:128], lhsT=IDENT, rhs=NBIGM[d], start=False, stop=True)
                    return e.matmul(ps[by][:, 128:256], lhsT=KT[0:64, h, cs], rhs=QT[0:64, h, cs], start=True, stop=True)
                P.op("pe", bbm, reads=[("LFB", h), "CST", cx.k("HID", 8 + h), cx.k("HID", h)], writes=psk(by))
                P.op("act", lambda e: e.activation(out=DTT[:, rot, :], in_=ps[by][:, 0:128], func=AF.Exp,
                                                   bias=G3S[:, h:h + 1], scale=1.0),
                     reads=psk(by) + [("G3S", 0)], writes=[("DTT", rot)])
                P.op("dve", lambda e: e.tensor_tensor(out=PTT[:, rot, :], in0=ps[by][:, 128:256], in1=DTT[:, rot, :], op=ALU.mult),
                     reads=psk(by) + [("DTT", rot)], writes=[("PTT", rot)])

            def head_back(h):
                rot = h % 2
                bX = bXs[h % 2]
                vrhs = cx.VAUG[:, c, h, 0:DV + 1]

                def mm(e):
                    ins = None
                    if with_out:
                        e.matmul(ps[bX][:, 0:DV + 1], lhsT=PTT[:, rot, :], rhs=vrhs, start=True, stop=True)
                        e.matmul(ps[bX][:, 130:130 + DV + 1], lhsT=QT[0:64, h, cs], rhs=CST_B[d][:, h, 0:DV + 1],
                                 start=True, stop=True)
                    return e.matmul(ps[bX][0:64, 260:260 + DV + 1], lhsT=KW[:, h, :], rhs=vrhs, start=True, stop=True)
                rd = [("KW", h), vk]
                if with_out:
                    rd += [("PTT", rot), cx.k("HID", h), ("CB", d, h)]
                P.op("pe", mm, reads=rd, writes=psk(bX))
                if with_out:
                    P.op("act", lambda e: e.activation(out=T0[:, rot, 0:DV + 1], in_=ps[bX][:, 0:DV + 1], func=AF.Copy),
                         reads=psk(bX), writes=[("T0", rot)])
                    P.op("dve", lambda e: e.scalar_tensor_tensor(
                        out=T1[:, h, 0:DV + 1], in0=ps[bX][:, 130:130 + DV + 1], scalar=G3S[:, 8 + h:9 + h],
                        in1=T0[:, rot, 0:DV + 1], op0=ALU.mult, op1=ALU.add),
                        reads=psk(bX) + [("G3S", 1), ("T0", rot)], writes=[("T1", h)])
                P.op("dve", lambda e: e.scalar_tensor_tensor(
                    out=CST_C[d][:, h, 0:DV + 1], in0=CST_C[d][:, h, 0:DV + 1], scalar=G3S[0:64, 32 + h:33 + h],
                    in1=ps[bX][0:64, 260:260 + DV + 1], op0=ALU.mult, op1=ALU.add),
                    reads=[("CS", d, h), ("G3S", 4)] + psk(bX), writes=[("CS", d, h)])
                P.op("act", lambda e: e.activation(out=CST_B[d][:, h, 0:DV + 1], in_=CST_C[d][:, h, 0:DV + 1], func=AF.Copy),
                     reads=[("CS", d, h)], writes=[("CB", d, h)])

            if with_out:
                head_front(0)
                yield (0.7, 4.0)
            for h in range(NH):
                if with_out and h + 1 < NH:
                    head_front(h + 1)
                head_back(h)
                yield (1.0, 4.0)
            if with_out:
                den = T1[:, :, DV]
                P.op("act", lambda e: e.activation(out=RD[:, 0:8], in_=den, func=AF.Abs),
                     reads=[("T1", h) for h in range(NH)], writes=[("RD", 0)])
                P.op("dve", lambda e: e.tensor_scalar_max(out=RD[:, 0:8], in0=RD[:, 0:8], scalar1=1.0),
                     reads=[("RD", 0)], writes=[("RD", 0)])
                P.op("dve", lambda e: e.reciprocal(out=RD[:, 8:16], in_=RD[:, 0:8]), reads=[("RD", 0)], writes=[("RD", 1)])
                hs = cx.F1[:].rearrange("p a b -> p (a b)").rearrange("p (c f) -> p c f", f=D)
                for h in range(NH):
                    dst = hs[:, c, h * DV:(h + 1) * DV]
                    key = cx.k("F1", 4 * c + h // 2)
                    if first_dir:
                        eng = "act" if h % 2 == 0 else "pool"
                        if eng == "act":
                            P.op("act", lambda e, h=h, dst=dst: e.activation(out=dst, in_=T1[:, h, 0:DV], func=AF.Identity,
                                                                             bias=0.0, scale=RD[:, 8 + h:9 + h]),
                                 reads=[("T1", h), ("RD", 1)], writes=[key])
                        else:
                            P.op("dve", lambda e, h=h, dst=dst: e.tensor_scalar(out=dst, in0=T1[:, h, 0:DV],
                                                                                 scalar1=RD[:, 8 + h:9 + h], scalar2=None,
                                                                                 op0=ALU.mult),
                                 reads=[("T1", h), ("RD", 1)], writes=[key])
                    else:
                        P.op("dve", lambda e, h=h, dst=dst: e.scalar_tensor_tensor(
                            out=dst, in0=T1[:, h, 0:DV], scalar=RD[:, 8 + h:9 + h], in1=dst, op0=ALU.mult, op1=ALU.add),
                            reads=[("T1", h), ("RD", 1), key], writes=[key])
            banks.rel(pairs)
            yield (0.0, 4.0)

        def l1_proj(cx, full):
            if full:
                pairs = yield from acq(2)
                slot, wkey = w_next(("qkvo", 0, 0))
                for g in range(2):
                    pair = pairs[g]
                    fm_mm(cx, slot, wkey, cx.B0, "B0", 0, pair, True, True, ncol=4, msize=64, col0=g * 256)
                    for q in range(4):
                        h = g * 4 + q
                        b, c0 = reg(pair, q, CT)
                        P.op("act", lambda e, h=h, b=b, c0=c0: e.activation(out=cx.HID[0:64, h, :], in_=ps[b][0:64, c0:c0 + CT],
                                                                            func=AF.Copy, scale=DQK ** -0.5),
                             reads=psk(b), writes=[cx.k("HID", h)])
                banks.rel(pairs)
                yield (3.7, 2.0)
            pairs = yield from acq(2)
            slot, wkey = w_next(("qkvo", 0, 512))
            if full:
                for g in range(2):
                    pair = pairs[g]
                    fm_mm(cx, slot, wkey, cx.B0, "B0", 0, pair, True, True, ncol=4, msize=64, col0=g * 256)
                    for q in range(4):
                        h = g * 4 + q
                        b, c0 = reg(pair, q, CT)
                        P.op("dve", lambda e, h=h, b=b, c0=c0: e.tensor_copy(out=cx.HID[0:64, 8 + h, :], in_=ps[b][0:64, c0:c0 + CT]),
                             reads=psk(b), writes=[cx.k("HID", 8 + h)])
            pair = pairs[0]
            for c in range(NCH):
                b = pair * 2 + c

                def f(e, c=c, b=b, slot=slot):
                    ins = None
                    for kc in range(FC):
                        ins = e.matmul(ps[b][:, 0:512], lhsT=cx.B0[:, kc, c * 128:(c + 1) * 128], rhs=slot[:, kc, :],
                                       start=(kc == 0), stop=(kc == FC - 1))
                    return ins
                P.op("pe", f, reads=[wkey] + cx.ks("B0", FC), writes=psk(b))
                P.op("act", lambda e, c=c, b=b: e.activation(out=cx.KTOK[:, c, :], in_=ps[b][:, 0:512], func=AF.Copy),
                     reads=psk(b), writes=[cx.k("KTOK", c)])
            banks.rel(pairs)
            yield (5.5, 2.0)
            for j in range(2):
                pair = (yield from acq(1))[0]
                slot, wkey = w_next(("qkvo", 0, 1024 + 512 * j))
                for c in range(NCH):
                    b = pair * 2 + c

                    def f(e, c=c, b=b, slot=slot):
                        ins = None
                        for kc in range(FC):
                            ins = e.matmul(ps[b][:, 0:512], lhsT=cx.B0[:, kc, c * 128:(c + 1) * 128], rhs=slot[:, kc, :],
                                           start=(kc == 0), stop=(kc == FC - 1))
                        return ins
                    P.op("pe", f, reads=[wkey] + cx.ks("B0", FC), writes=psk(b))
                    dst = cx.VAUG[:, c, 4 * j:4 * j + 4, 0:DV]
                    src = ps[b][:, 0:512].rearrange("p (h v) -> p h v", v=DV)
                    if c % 2 == 0:
                        P.op("dve", lambda e, dst=dst, src=src: e.tensor_copy(out=dst, in_=src),
                             reads=psk(b), writes=[cx.k("VAUG", c)])
                    else:
                        P.op("act", lambda e, dst=dst, src=src: e.activation(out=dst, in_=src, func=AF.Copy),
                             reads=psk(b), writes=[cx.k("VAUG", c)])
                banks.rel([pair])
                yield (3.6, 2.0)
            if full:
                for j in range(2):
                    pair = (yield from acq(1))[0]
                    slot, wkey = w_next(("qkvo", 0, 2048 + 512 * j))
                    fm_mm(cx, slot, wkey, cx.B0, "B0", 0, pair, True, True)
                    for mi in range(4):
                        h = 4 * j + mi
                        b, c0 = reg(pair, mi, CT)
                        P.op("act", lambda e, h=h, b=b, c0=c0: e.activation(out=cx.HID[:, 16 + h, :], in_=ps[b][:, c0:c0 + CT], func=AF.Sigmoid),
                             reads=psk(b), writes=[cx.k("HID", 16 + h)])
                    banks.rel([pair])
                    yield (3.7, 2.0)


        def wait_flag(name):
            while not flags.get(name):
                yield "blocked"

        flat3 = lambda t: t[:].rearrange("p a b -> p (a b)")

        def ctx_context(cx):
            yield from load_tokens(cx, ctx_d)
            yield from layer0(cx, 1, CTX)
            yield from wait_flag(("mod", 1))
            yield from norm_mod(cx, 1, 1, "a1", "sh1")
            yield from l1_proj(cx, False)
            yield from gates_mm(cx, 2 * NCX)
            for d in range(2):
                order = [0, 1] if d == 0 else [1, 0]
                for c in order:
                    yield from scan_chunk(cx, d, 2 * NCX + c, c, False)
            flags[("fscan", -1)] = True
            flags[("bscan", NCX)] = True

        def sweep1_context(cx, i):
            yield from load_tokens(cx, x_d[i * CT:(i + 1) * CT, :])
            yield from layer0(cx, 0, 64)
            yield from wait_flag(("mod", 1))
            P.dma("sp", lambda e: e.dma_start(out=x1_s[i], in_=flat3(cx.F0)), reads=cx.ks("F0", FC), writes=[("x1_s", i)])
            if DEBUG:
                P.dma("sp", lambda e: e.dma_start(out=dbg_d[i], in_=flat3(cx.F0)), reads=cx.ks("F0", FC), writes=[("dbg", i)])
            yield from norm_mod(cx, 1, 0, "a1", "sh1")
            yield from l1_proj(cx, True)
            yield from gates_mm(cx, 2 * i)
            P.dma("sp", lambda e: e.dma_start(out=qt_s[i], in_=cx.HID[0:64, 0:8, :].rearrange("p a b -> p (a b)")),
                  reads=[cx.k("HID", h) for h in range(8)], writes=[("qt_s", i)])
            P.dma("sp", lambda e: e.dma_start(out=kt_s[i], in_=cx.HID[0:64, 8:16, :].rearrange("p a b -> p (a b)")),
                  reads=[cx.k("HID", 8 + h) for h in range(8)], writes=[("kt_s", i)])
            P.dma("sp", lambda e: e.dma_start(out=so_s[i], in_=cx.HID[:, 16:24, :].rearrange("p a b -> p (a b)")),
                  reads=[cx.k("HID", 16 + h) for h in range(8)], writes=[("so_s", i)])
            P.dma("sp", lambda e: e.dma_start(out=ktok_s[i], in_=flat3(cx.KTOK)), reads=cx.ks("KTOK", NCH), writes=[("ktok_s", i)])
            P.dma("sp", lambda e: e.dma_start(out=vaug_s[i], in_=cx.VAUG[:].rearrange("p a b c -> p (a b c)")),
                  reads=cx.ks("VAUG", NCH), writes=[("vaug_s", i)])
            yield from wait_flag(("fscan", i - 1))
            for c in range(NCH):
                yield from scan_chunk(cx, 0, 2 * i + c, c, True, first_dir=True)
            flags[("fscan", i)] = True
            P.dma("sp", lambda e: e.dma_start(out=hf_s[i], in_=flat3(cx.F1)), reads=cx.ks("F1", FC), writes=[("hf_s", i)])
            flags[("s1done", i)] = True
            yield (0.0, 0.0)

        def sweep2_context(cx, i):
            yield from wait_flag(("s1done", i))
            P.dma("sp", lambda e: e.dma_start(out=cx.HID[0:64, 0:8, :].rearrange("p a b -> p (a b)"), in_=qt_s[i]),
                  reads=[("qt_s", i)], writes=[cx.k("HID", h) for h in range(8)])
            P.dma("sp", lambda e: e.dma_start(out=cx.HID[0:64, 8:16, :].rearrange("p a b -> p (a b)"), in_=kt_s[i]),
                  reads=[("kt_s", i)], writes=[cx.k("HID", 8 + h) for h in range(8)])
            P.dma("sp", lambda e: e.dma_start(out=flat3(cx.KTOK), in_=ktok_s[i]), reads=[("ktok_s", i)], writes=cx.ks("KTOK", NCH))
            P.dma("sp", lambda e: e.dma_start(out=cx.VAUG[:].rearrange("p a b c -> p (a b c)"), in_=vaug_s[i]),
                  reads=[("vaug_s", i)], writes=cx.ks("VAUG", NCH))
            P.dma("sp", lambda e: e.dma_start(out=flat3(cx.F1), in_=hf_s[i]), reads=[("hf_s", i)], writes=cx.ks("F1", FC))
            P.dma("sp", lambda e: e.dma_start(out=cx.HID[:, 16:24, :].rearrange("p a b -> p (a b)"), in_=so_s[i]),
                  reads=[("so_s", i)], writes=[cx.k("HID", 16 + h) for h in range(8)])
            P.dma("sp", lambda e: e.dma_start(out=flat3(cx.F0), in_=x1_s[i]), reads=[("x1_s", i)], writes=cx.ks("F0", FC))
            yield (0.0, 15.0)
            yield from wait_flag(("bscan", i + 1))
            hs = cx.F1[:].rearrange("p a b -> p (a b)").rearrange("p (c f) -> p c f", f=D)
            for c in reversed(range(NCH)):
                yield from scan_chunk(cx, 1, 2 * i + c, c, True, first_dir=False)
                if c == 0:
                    flags[("bscan", i)] = True
                hkeys = [cx.k("F1", 4 * c + q) for q in range(4)]
                sq = cx.F2[:].rearrange("p a b -> p (a b)")[:, 0:D]
                sqk = [cx.k("F2", q) for q in range(4)]
                P.op("dve", lambda e, c=c, sq=sq: e.tensor_tensor(out=sq, in0=hs[:, c, :], in1=hs[:, c, :], op=ALU.mult),
                     reads=hkeys, writes=sqk)
                P.op("dve", lambda e, sq=sq: e.tensor_reduce(out=cx.SSQ[:, 0:8], in_=sq.rearrange("p (h v) -> p h v", v=DV),
                                                             axis=AX.X, op=ALU.add),
                     reads=sqk, writes=[cx.k("SSQ", 0)])
                P.op("act", lambda e: e.activation(out=cx.SSQ[:, 8:16], in_=cx.SSQ[:, 0:8], func=AF.Sqrt, bias=EPS_COL, scale=1.0 / DV),
                     reads=[cx.k("SSQ", 0), "SM"], writes=[cx.k("SSQ", 1)])
                P.op("dve", lambda e: e.reciprocal(out=cx.SSQ[:, 8:16], in_=cx.SSQ[:, 8:16]), reads=[cx.k("SSQ", 1)], writes=[cx.k("SSQ", 1)])
                yield (0.0, 8.0)
                pair = (yield from acq(1))[0]
                for h in range(NH):
                    rot = h % 2
                    b = pair * 2 + rot
                    P.op("act", lambda e, c=c, h=h, rot=rot: e.activation(out=cx.HN[:, rot, :], in_=hs[:, c, h * DV:(h + 1) * DV],
                                                                          func=AF.Identity, bias=0.0, scale=cx.SSQ[:, 8 + h:9 + h]),
                         reads=hkeys + [cx.k("SSQ", 1)], writes=[cx.k("HN", rot)])
                    P.op("pe", lambda e, rot=rot, b=b: e.transpose(out=ps[b][:, 0:128], in_=cx.HN[:, rot, :], identity=IDENT),
                         reads=[cx.k("HN", rot), "CST"], writes=psk(b))
                    P.op("dve", lambda e, c=c, h=h, b=b: e.scalar_tensor_tensor(
                        out=cx.HID[:, 24 + h, c * 128:(c + 1) * 128], in0=ps[b][:, 0:128],
                        scalar=SM[:, S_MLNW + h:S_MLNW + h + 1], in1=cx.HID[:, 16 + h, c * 128:(c + 1) * 128],
                        op0=ALU.mult, op1=ALU.mult),
                        reads=psk(b) + ["SM", cx.k("HID", 16 + h)], writes=[cx.k("HID", 24 + h)])
                    if h % 2 == 1:
                        yield (0.5, 3.0)
                banks.rel([pair])
            for j in range(2):
                pair = (yield from acq(1))[0]
                slot, wkey = w_next(("mlout", 0, 512 * j))
                fm_mm(cx, slot, wkey, cx.HID, "HID", 24, pair, True, True)
                evac_branch(cx, pair, 4 * j)
                banks.rel([pair])
                yield (3.7, 4.0)
            yield from residual(cx, 1, 0, "g1")
            yield from mlp(cx, 1, 0)
            xt = cx.F2[:].rearrange("p a b -> p (a b)").rearrange("p (t f) -> p t f", f=D)
            for tt in range(NCH):
                pair = (yield from acq(1))[0]
                for half in range(2):
                    b = pair * 2 + half

                    def f(e, tt=tt, half=half, b=b):
                        ins = None
                        for q in range(4):
                            fc = half * 4 + q
                            ins = e.transpose(out=ps[b][:, q * 128:(q + 1) * 128], in_=cx.F0[:, fc, tt * 128:(tt + 1) * 128],
                                              identity=IDENT)
                        return ins
                    P.op("pe", f, reads=[cx.k("F0", half * 4 + q) for q in range(4)] + ["CST"], writes=psk(b))
                    wk = [cx.k("F2", tt * 4 + half * 2), cx.k("F2", tt * 4 + half * 2 + 1)]
                    if half == 0:
                        P.op("act", lambda e, tt=tt, half=half, b=b: e.activation(out=xt[:, tt, half * 512:(half + 1) * 512],
                                                                                  in_=ps[b][:, 0:512], func=AF.Copy),
                             reads=psk(b), writes=wk)
                    else:
                        P.op("dve", lambda e, tt=tt, half=half, b=b: e.tensor_copy(out=xt[:, tt, half * 512:(half + 1) * 512],
                                                                                   in_=ps[b][:, 0:512]),
                             reads=psk(b), writes=wk)
                banks.rel([pair])
                yield (1.8, 2.0)
            P.dma("sp", lambda e: e.dma_start(out=out_d[i * CT:(i + 1) * CT, :].rearrange("(t p) f -> p t f", p=128), in_=xt),
                  reads=cx.ks("F2", FC), writes=[("out", i)])
            yield (0.0, 0.0)

        jobs = [("ctx", None)] + [("s1", i) for i in range(NCX_RUN)] + [("s2", i) for i in reversed(range(NCX_RUN))]
        if STOP == "ctx":
            jobs = jobs[:1]
        elif STOP == "sweep1":
            jobs = jobs[:1 + NCX_RUN]

        def make(job, cx):
            kind, i = job
            if kind == "ctx":
                return ctx_context(cx)
            if kind == "s1":
                return sweep1_context(cx, i)
            return sweep2_context(cx, i)

        if NCX_RUN < NCX:
            for i in range(NCX_RUN, NCX):
                flags[("s1done", i)] = True
            flags[("bscan", NCX_RUN)] = True
        active = [[mod_layer(1), None, 0.0]]
        free_cx = [CXS[0], CXS[1]]
        pending = list(jobs)

        pe_clock = 0.0
        guard = 0
        while active or pending:
            guard += 1
            assert guard < 2000000, "scheduler livelock"
            while pending and free_cx:
                cx_ = free_cx.pop(0)
                active.append([make(pending.pop(0), cx_), cx_, pe_clock])
            order = sorted(active, key=lambda a_: max(a_[2], pe_clock))
            progressed = False
            for ent in order:
                try:
                    r = next(ent[0])
                except StopIteration:
                    active.remove(ent)
                    if ent[1] is not None:
                        free_cx.append(ent[1])
                    progressed = True
                    break
                if r == "blocked":
                    continue
                pe_us, lat_us = r if r is not None else (0.0, 0.0)
                start = max(pe_clock, ent[2])
                pe_clock = start + pe_us
                ent[2] = pe_clock + lat_us
                progressed = True
                break
            assert progressed, "all pipeline contexts blocked"
        assert not pending
        assert dry or wstate["used"] == len(sched), (wstate, len(sched))
        if dry:
            return sched
        P.emit()
    return nc


_NC_CACHE = {}


def kernel(x, c, ctx, c_ctx, norm_w, mod_w, mod_b, mlp_w1, mlp_w2, conv_w_in, conv_w, conv_w_out,
           ml_w_qkvo, ml_w_if, ml_b_if, ml_norm_w, ml_w_out):
    f32 = lambda a: np.ascontiguousarray(np.asarray(a, dtype=np.float32))
    x = f32(x); c = f32(c); ctx = f32(ctx); c_ctx = f32(c_ctx)
    if "nc" not in _NC_CACHE:
        _NC_CACHE["nc"] = build_nc()
    nc = _NC_CACHE["nc"]
    consts = _consts()
    shared = {
        "consts": consts, "mod_w": f32(mod_w), "mlp_w1": f32(mlp_w1), "mlp_w2": f32(mlp_w2),
        "conv_w_in": f32(conv_w_in[0]), "conv_w_out": f32(conv_w_out[0]),
        "ml_w_qkvo": f32(ml_w_qkvo[0]), "ml_w_out": f32(ml_w_out[0]),
    }
    in_maps = []
    for b in range(NCORES):
        m = dict(shared)
        m["x"] = x[b]
        m["ctx"] = ctx[b]
        m["small"] = _small(c[b], c_ctx, f32(norm_w), f32(mod_b), f32(conv_w), f32(ml_norm_w), f32(ml_b_if), f32(ml_w_if))
        in_maps.append(m)
    res = run_bass_kernel_spmd(nc, in_maps, core_ids=list(range(NCORES)))
    _NC_CACHE["last"] = res
    return np.stack([np.asarray(r["out"], dtype=np.float32) for r in res.results], axis=0)
```

```python
import contextlib
import numpy as np
import concourse.bass as bass
import concourse.mybir as mybir
from concourse.bass_utils import run_bass_kernel_spmd

F32 = mybir.dt.float32
BF16 = mybir.dt.bfloat16
AF = mybir.ActivationFunctionType
ALU = mybir.AluOpType
AX = mybir.AxisListType

D = 1024
FC = 8
SEQ = 4096
CTX = 256
TT = 512
NT = SEQ // TT
DFF = 4096
NH = 8
DQK = 64
DV = 128
VS = 130
EPS = 1e-6
DEBUG = False
STOP = None
NCORES = 8
GG_ENG = "dve"
NCX_RUN = 16


class _Stop(Exception):
    pass

SEM_EPOCH = 12000
N_DMA_SEMS = 10


class Op:
    __slots__ = ("eng", "fn", "reads", "writes", "dma", "deps", "sig", "signo", "dsem", "dval")

    def __init__(self, eng, fn, reads, writes, dma):
        self.eng = eng
        self.fn = fn
        self.reads = reads
        self.writes = writes
        self.dma = dma
        self.deps = None
        self.sig = False
        self.signo = 0
        self.dsem = None
        self.dval = 0


class Prog:
    ENGS = ("pe", "act", "dve", "pool", "sp")

    def __init__(self, nc):
        self.nc = nc
        self.ops = []

    def op(self, eng, fn, reads=(), writes=()):
        self.ops.append(Op(eng, fn, tuple(reads), tuple(writes), False))

    def dma(self, eng, fn, reads=(), writes=()):
        self.ops.append(Op(eng, fn, tuple(reads), tuple(writes), True))

    def emit(self):
        nc = self.nc
        ops = self.ops
        last_w = {}
        readers = {}
        for i, o in enumerate(ops):
            deps = {}
            for b in o.reads:
                j = last_w.get(b)
                if j is not None:
                    deps[j] = True
                if b[0] == "ps":
                    for j in readers.get(b, ()):
                        if ops[j].eng != o.eng:
                            deps.setdefault(j, False)
            for b in o.writes:
                j = last_w.get(b)
                if j is not None:
                    deps.setdefault(j, False)
                for j in readers.get(b, ()):
                    deps.setdefault(j, False)
            deps.pop(i, None)
            o.deps = deps
            for b in o.reads:
                readers.setdefault(b, []).append(i)
            for b in o.writes:
                last_w[b] = i
                readers[b] = []
        for o in ops:
            for j, raw in o.deps.items():
                p = ops[j]
                if p.dma:
                    continue
                if o.dma or p.eng != o.eng:
                    p.sig = True
                elif raw and o.eng in ("act", "dve", "pool"):
                    p.sig = True
        cnt = {e: 0 for e in self.ENGS}
        for o in ops:
            if o.sig and not o.dma:
                cnt[o.eng] += 1
                o.signo = cnt[o.eng]
        engobj = {"pe": nc.tensor, "act": nc.scalar, "dve": nc.vector, "pool": nc.gpsimd, "sp": nc.sync}
        with contextlib.ExitStack() as st:
            esems = {}
            for e in self.ENGS:
                n_ep = (cnt[e] + SEM_EPOCH - 1) // SEM_EPOCH
                esems[e] = [st.enter_context(nc.semaphore(f"s_{e}_{k}")) for k in range(n_ep)]
            dsems = {}
            for e in ("sp", "pool", "act"):
                if any(o.dma and o.eng == e for o in ops):
                    dsems[e] = [st.enter_context(nc.semaphore(f"d_{e}_{k}")) for k in range(N_DMA_SEMS)]
            duse = {e: [0] * N_DMA_SEMS for e in dsems}
            dlast = {e: [None] * N_DMA_SEMS for e in dsems}
            dnext = {e: 0 for e in dsems}
            seen = {e: {} for e in self.ENGS}

            def wait(e, sem, key, val):
                s = seen[e]
                if s.get(key, 0) >= val:
                    return
                s[key] = val
                engobj[e].wait_ge(sem, val)

            def wait_op(e, p):
                if p.dma:
                    wait(e, p.dsem, ("d", id(p.dsem)), p.dval)
                else:
                    k = (p.signo - 1) // SEM_EPOCH
                    v = (p.signo - 1) % SEM_EPOCH + 1
                    wait(e, esems[p.eng][k], (p.eng, k), v)

            for o in ops:
                e = o.eng
                for j, raw in o.deps.items():
                    p = ops[j]
                    if p.dma or o.dma or p.eng != e:
                        wait_op(e, p)
                    elif raw and e in ("act", "dve", "pool"):
                        wait_op(e, p)
                if o.dma:
                    k = dnext[e]
                    dnext[e] = (k + 1) % N_DMA_SEMS
                    prev = dlast[e][k]
                    if prev is not None:
                        wait_op(e, prev)
                    duse[e][k] += 1
                    o.dsem = dsems[e][k]
                    o.dval = 16 * duse[e][k]
                    dlast[e][k] = o
                    ins = o.fn(engobj[e])
                    ins.then_inc(o.dsem, 16)
                else:
                    ins = o.fn(engobj[e])
                    if o.sig:
                        k = (o.signo - 1) // SEM_EPOCH
                        ins.then_inc(esems[e][k], 1)
            for e in dsems:
                for k in range(N_DMA_SEMS):
                    p = dlast[e][k]
                    if p is not None:
                        wait_op(e, p)


C_IDENT, C_MINC0, C_MINC1, C_MSTR0, C_MSTR1, C_NEG1, C_NBIG0, C_NBIG1, C_ONES = range(9)
NBIG = -30000.0


def _consts():
    u = np.arange(128)[:, None]
    t = np.arange(128)[None, :]
    mats = [
        (u == t).astype(np.float32),
        -(u <= t).astype(np.float32),
        -(u >= t).astype(np.float32),
        -(u > t).astype(np.float32),
        -(u < t).astype(np.float32),
        -np.ones((128, 128), np.float32),
        NBIG * (t < u).astype(np.float32),
        NBIG * (t > u).astype(np.float32),
        np.ones((128, 128), np.float32),
    ]
    return np.ascontiguousarray(np.concatenate(mats, axis=1))


S_CC = 0
S_NW = 16
S_MB = 80
S_CW = 176
S_MLNW = 200
S_BIF = 208
S_WIF = 240
S_MISC = 496
NSMALL = 500


def _fm(v):
    v = np.asarray(v, np.float32)
    lead = v.shape[:-1]
    a = v.reshape(lead + (FC, 128))
    a = np.moveaxis(a, -1, 0)
    return a


def _small(c_b, c_ctx, norm_w, mod_b, conv_w, ml_norm_w, ml_b_if, ml_w_if):
    s = np.zeros((128, NSMALL), np.float32)
    cc = np.stack([_fm(c_b), _fm(c_ctx)], axis=-1)
    s[:, S_CC:S_CC + 16] = cc.reshape(128, 16)
    s[:, S_NW:S_NW + 64] = _fm(norm_w).reshape(128, 64)
    mb = np.asarray(mod_b, np.float32).reshape(2, 48, 128)
    s[:, S_MB:S_MB + 96] = np.moveaxis(mb, -1, 0).reshape(128, 96)
    s[:, S_CW:S_CW + 24] = _fm(conv_w[0]).reshape(128, 24)
    s[:, S_MLNW:S_MLNW + 8] = _fm(ml_norm_w[0]).reshape(128, 8)
    s[:, S_BIF:S_BIF + 32] = np.asarray(ml_b_if[0], np.float32).reshape(1, 32)
    wif = np.concatenate([ml_w_if[0, 0], ml_w_if[0, 1]], axis=1)
    wif = wif.reshape(FC, 128, 32).transpose(1, 0, 2)
    s[:, S_WIF:S_WIF + 256] = wif.reshape(128, 256)
    s[:, S_MISC + 0] = 1.0
    s[:, S_MISC + 1] = EPS
    return s


CT = 256
NCX = SEQ // CT
NCH = CT // 128
SKEW = 40


class _BankAlloc:
    def __init__(self):
        self.free = [0, 1, 2, 3]

    def try_acq(self, n):
        if len(self.free) < n:
            return None
        r = self.free[:n]
        self.free = self.free[n:]
        return r

    def rel(self, pairs):
        self.free = self.free + list(pairs)


def build_nc():
    sched = _build(None)
    return _build(sched)


def _build(sched_in):
    dry = sched_in is None
    nc = bass.Bass("TRN2", target_bir_lowering=False)
    dt_in = lambda n, shp: nc.dram_tensor(n, list(shp), F32, kind="ExternalInput").ap()
    x_d = dt_in("x", [SEQ, D])
    ctx_d = dt_in("ctx", [CTX, D])
    small_d = dt_in("small", [128, NSMALL])
    consts_d = dt_in("consts", [128, 9 * 128])
    mod_w_d = dt_in("mod_w", [2, D, 6 * D])
    mlp_w1_d = dt_in("mlp_w1", [2, D, DFF])
    mlp_w2_d = dt_in("mlp_w2", [2, DFF, D])
    conv_w_in_d = dt_in("conv_w_in", [D, 3 * D])
    conv_w_out_d = dt_in("conv_w_out", [D, D])
    ml_w_qkvo_d = dt_in("ml_w_qkvo", [D, 3 * D])
    ml_w_out_d = dt_in("ml_w_out", [D, D])
    out_d = nc.dram_tensor("out", [SEQ, D], F32, kind="ExternalOutput").ap()

    def scratch(n, shp, dt):
        return nc.dram_tensor(n, list(shp), dt).ap()

    wsrc = {
        "win0": conv_w_in_d, "wout0": conv_w_out_d, "w1_0": mlp_w1_d[0], "w2_0": mlp_w2_d[0],
        "qkvo": ml_w_qkvo_d, "mlout": ml_w_out_d, "w1_1": mlp_w1_d[1], "w2_1": mlp_w2_d[1],
    }
    wsc = {n: scratch("sc_" + n, a.shape, BF16) for n, a in wsrc.items()}
    x1_s = scratch("x1_s", [NCX, 128, FC * CT], F32)
    qt_s = scratch("qt_s", [NCX, 64, NH * CT], BF16)
    kt_s = scratch("kt_s", [NCX, 64, NH * CT], BF16)
    ktok_s = scratch("ktok_s", [NCX, 128, NCH * 512], BF16)
    vaug_s = scratch("vaug_s", [NCX, 128, NCH * NH * VS], BF16)
    so_s = scratch("so_s", [NCX, 128, NH * CT], BF16)
    hf_s = scratch("hf_s", [NCX, 128, NCH * D], F32)
    dbg_d = None
    if DEBUG:
        dbg_d = nc.dram_tensor("dbg", [NCX, 128, FC * CT], F32, kind="ExternalOutput").ap()

    with contextlib.ExitStack() as st:
        def sb(name, shape, dt):
            return st.enter_context(nc.sbuf_tensor(name, list(shape), dt))

        P = Prog(nc)
        ps = [st.enter_context(nc.psum_tensor(f"ps{i}", [128, 512], F32)) for i in range(8)]
        banks = _BankAlloc()

        SM = sb("SM", [128, NSMALL], F32)
        CST = sb("CST", [128, 9 * 128], F32)
        ONESB = sb("ONESB", [128, 128], BF16)
        WIFB = sb("WIFB", [128, FC, 32], BF16)
        SIL = sb("SIL", [128, FC, 2], F32)
        MOD = sb("MOD", [128, 2, 2, 48], F32)
        PAR = sb("PAR", [128, 2, 2, 4, 8], F32)
        GT = sb("GT", [128, 2 * NCX + 2, 32], F32)
        WRING = [sb(f"WR{i}", [128, FC, 512], BF16) for i in range(4)]
        LG = sb("LG", [128, 2 * NCX + 2, 32], F32)
        G3S = sb("G3S", [128, 40], F32)
        LFB = sb("LFB", [128, NH, 128], F32)
        DTT = sb("DTT", [128, 2, 128], F32)
        PTT = sb("PTT", [128, 2, 128], BF16)
        KW = sb("KW", [128, NH, 64], BF16)
        T0 = sb("T0", [128, 2, VS], F32)
        T1 = sb("T1", [128, NH, VS], F32)
        RD = sb("RD", [128, 16], F32)
        CST_C = [sb(f"CS{d}", [64, NH, VS], F32) for d in range(2)]
        CST_B = [sb(f"CB{d}", [64, NH, VS], BF16) for d in range(2)]
        HN = sb("HN", [128, 2, 128], F32)
        SSQ = sb("SSQ", [128, 16], F32)

        class Cx:
            def __init__(self, i):
                self.i = i
                self.F0 = sb(f"F0_{i}", [128, FC, CT], F32)
                self.F1 = sb(f"F1_{i}", [128, FC, CT], F32)
                self.F2 = sb(f"F2_{i}", [128, FC, CT], F32)
                self.B0 = sb(f"B0_{i}", [128, FC, CT], BF16)
                self.B1 = sb(f"B1_{i}", [128, FC, CT], BF16)
                self.HID = sb(f"HID_{i}", [128, 32, CT], BF16)
                self.RS = sb(f"RS_{i}", [128, CT], F32)
                self.TMPA = sb(f"TMPA_{i}", [128, 2, CT], F32)
                self.KTOK = sb(f"KTOK_{i}", [128, NCH, 512], BF16)
                self.VAUG = sb(f"VAUG_{i}", [128, NCH, NH, VS], BF16)
                self.SSQ = sb(f"SSQ_{i}", [128, 16], F32)
                self.HN = sb(f"HN_{i}", [128, 2, 128], F32)

            def k(self, name, idx=None):
                return (name + str(self.i), idx)

            def ks(self, name, n):
                return [(name + str(self.i), j) for j in range(n)]

        CXS = [Cx(0), Cx(1)]
        MODBUF = [sb(f"MODBUF{i}", [128, FC, CT], F32) for i in range(2)]

        IDENT = CST[:, C_IDENT * 128:(C_IDENT + 1) * 128]
        MINC = [CST[:, C_MINC0 * 128:(C_MINC0 + 1) * 128], CST[:, C_MINC1 * 128:(C_MINC1 + 1) * 128]]
        MSTR = [CST[:, C_MSTR0 * 128:(C_MSTR0 + 1) * 128], CST[:, C_MSTR1 * 128:(C_MSTR1 + 1) * 128]]
        NEG1 = CST[:, C_NEG1 * 128:(C_NEG1 + 1) * 128]
        NBIGM = [CST[:, C_NBIG0 * 128:(C_NBIG0 + 1) * 128], CST[:, C_NBIG1 * 128:(C_NBIG1 + 1) * 128]]
        ONES32 = CST[:, C_ONES * 128:(C_ONES + 1) * 128]
        ONE_COL = SM[:, S_MISC:S_MISC + 1]
        EPS_COL = SM[:, S_MISC + 1:S_MISC + 2]

        def psk(b):
            return [("ps", b)]

        def acq(n=1):
            while True:
                r = banks.try_acq(n)
                if r is not None:
                    return r
                yield "blocked"

        def reg(pair, mi, ntok):
            b = pair * 2 + mi // 2
            c0 = (mi % 2) * 256
            return b, c0

        P.dma("sp", lambda e: e.dma_start(out=SM[:], in_=small_d), writes=["SM"])
        P.dma("sp", lambda e: e.dma_start(out=CST[:], in_=consts_d), writes=["CST"])
        P.op("act", lambda e: e.activation(out=ONESB[:], in_=ONES32, func=AF.Copy), reads=["CST"], writes=["ONESB"])
        P.op("act", lambda e: e.activation(
            out=WIFB[:], in_=SM[:, S_WIF:S_WIF + 256].rearrange("p (k j) -> p k j", k=FC), func=AF.Copy),
            reads=["SM"], writes=["WIFB"])
        P.op("act", lambda e: e.activation(
            out=SIL[:], in_=SM[:, S_CC:S_CC + 16].rearrange("p (k j) -> p k j", k=FC), func=AF.Silu),
            reads=["SM"], writes=["SIL"])
        for d in range(2):
            P.op("pool", lambda e, d=d: e.memset(CST_C[d][:].rearrange("p a b -> p (a b)"), 0.0),
                 writes=[("CS", d, h) for h in range(NH)])
            P.op("pool", lambda e, d=d: e.memset(CST_B[d][:].rearrange("p a b -> p (a b)"), 0.0),
                 writes=[("CB", d, h) for h in range(NH)])
        for cx in CXS:
            P.op("pool", lambda e, cx=cx: e.memset(cx.VAUG[:].rearrange("p a b c -> p (a b c)"), 1.0),
                 writes=cx.ks("VAUG", NCH))

        PIECE = 1 << 20
        wpieces = {}
        for n, src in wsrc.items():
            R, C = src.shape
            npc = 1
            while R * C // npc > PIECE:
                npc *= 2
            rp = R // npc
            keys = []
            for r0 in range(0, R, rp):
                key = ("wsc", n, r0)
                keys.append(key)
                P.dma("pool", lambda e, n=n, src=src, r0=r0, rp=rp: e.dma_start(
                    out=wsc[n][r0:r0 + rp, :], in_=src[r0:r0 + rp, :]), writes=[key])
            wpieces[n] = keys

        flags = {}

        def mod_layer(l):
            pair = (yield from acq(1))[0]
            pb = pair * 2
            def dma_blk(nb):
                P.dma("sp", lambda e, l=l, nb=nb: e.dma_start(
                    out=MODBUF[nb % 2][:], in_=mod_w_d[l, :, nb * 256:(nb + 1) * 256].rearrange("(k p) n -> p k n", p=128)),
                    writes=[("MODBUF", nb % 2)])
            dma_blk(0)
            for nb in range(24):
                buf = MODBUF[nb % 2]
                bkey = [("MODBUF", nb % 2)]
                if nb + 1 < 24:
                    dma_blk(nb + 1)

                def mm_mod(e, nb=nb, buf=buf):
                    ins = None
                    for mi in range(2):
                        col = (nb * 2 + mi) * 2
                        for kc in range(FC):
                            ins = e.matmul(ps[pb][:, col:col + 2], lhsT=buf[:, kc, mi * 128:(mi + 1) * 128],
                                           rhs=SIL[:, kc, :], start=(kc == 0), stop=(kc == FC - 1))
                    return ins
                P.op("pe", mm_mod, reads=bkey + ["SIL"], writes=psk(pb))
                yield (1.6, 25.0)
            for s in range(2):
                P.op("dve", lambda e, l=l, s=s: e.tensor_tensor(
                    out=MOD[:, l, s, :], in0=ps[pb][:, 0:96].rearrange("p (m s) -> p m s", s=2)[:, :, s],
                    in1=SM[:, S_MB + l * 48:S_MB + (l + 1) * 48], op=ALU.add),
                    reads=psk(pb) + ["SM"], writes=[("MOD", l, s)])
                nw = lambda j, l=l: SM[:, S_NW + (l * 4 + j) * 8:S_NW + (l * 4 + j) * 8 + 8]
                md = lambda j, l=l, s=s: MOD[:, l, s, j * 8:(j + 1) * 8]
                P.op("dve", lambda e, l=l, s=s, nw=nw, md=md: e.scalar_tensor_tensor(
                    out=PAR[:, l, s, 0, :], in0=md(1), scalar=1.0, in1=nw(0), op0=ALU.add, op1=ALU.mult),
                    reads=[("MOD", l, s), "SM"], writes=[("PAR", l, s, 0)])
                P.op("dve", lambda e, l=l, s=s, nw=nw, md=md: e.tensor_tensor(
                    out=PAR[:, l, s, 1, :], in0=md(2), in1=nw(1), op=ALU.mult),
                    reads=[("MOD", l, s), "SM"], writes=[("PAR", l, s, 1)])
                P.op("dve", lambda e, l=l, s=s, nw=nw, md=md: e.scalar_tensor_tensor(
                    out=PAR[:, l, s, 2, :], in0=md(4), scalar=1.0, in1=nw(2), op0=ALU.add, op1=ALU.mult),
                    reads=[("MOD", l, s), "SM"], writes=[("PAR", l, s, 2)])
                P.op("dve", lambda e, l=l, s=s, nw=nw, md=md: e.tensor_tensor(
                    out=PAR[:, l, s, 3, :], in0=md(5), in1=nw(3), op=ALU.mult),
                    reads=[("MOD", l, s), "SM"], writes=[("PAR", l, s, 3)])
            banks.rel([pair])
            flags[("mod", l)] = True

        for _r in mod_layer(0):
            pass

        def par(l, s, which):
            if which == "a1":
                return PAR[:, l, s, 0, :], [("PAR", l, s, 0)]
            if which == "g1":
                return PAR[:, l, s, 1, :], [("PAR", l, s, 1)]
            if which == "a2":
                return PAR[:, l, s, 2, :], [("PAR", l, s, 2)]
            if which == "g2":
                return PAR[:, l, s, 3, :], [("PAR", l, s, 3)]
            if which == "sh1":
                return MOD[:, l, s, 0:8], [("MOD", l, s)]
            if which == "sh2":
                return MOD[:, l, s, 24:32], [("MOD", l, s)]
            raise KeyError(which)

        sched = [] if dry else list(sched_in)
        wstate = {"issued": 0, "used": 0}
        NSLOT = len(WRING)

        def w_issue():
            n = wstate["issued"]
            name, r0, c0 = sched[n]
            slot = n % NSLOT
            P.dma("sp", lambda e, name=name, r0=r0, c0=c0, slot=slot: e.dma_start(
                out=WRING[slot][:], in_=wsc[name][r0:r0 + 1024, c0:c0 + 512].rearrange("(k p) n -> p k n", p=128)),
                reads=wpieces[name], writes=[("WR", slot)])
            wstate["issued"] = n + 1

        def w_next(expect):
            n = wstate["used"]
            if dry:
                sched.append(expect)
            else:
                assert sched[n] == expect, (n, sched[n], expect)
                while wstate["issued"] < min(len(sched), n + NSLOT):
                    w_issue()
            wstate["used"] = n + 1
            slot = n % NSLOT
            return WRING[slot], ("WR", slot)

        def fm_mm(cx, slot, wkey, src, skey, koff, pair, first, last, ncol=4, msize=128, col0=0):
            for mi in range(ncol):
                b, c0 = reg(pair, mi, CT)

                def f(e, mi=mi, b=b, c0=c0):
                    ins = None
                    for kc in range(FC):
                        ins = e.matmul(ps[b][0:msize, c0:c0 + CT],
                                       lhsT=slot[:, kc, col0 + mi * msize:col0 + (mi + 1) * msize],
                                       rhs=src[:, koff + kc, 0:CT],
                                       start=(first and kc == 0 and mi % 2 == 0), stop=(last and kc == FC - 1),
                                       skip_group_check=True)
                    return ins
                P.op("pe", f, reads=[wkey] + [cx.k(skey, koff + kc) for kc in range(FC)], writes=psk(b))

        def rstd_from_sq(cx):
            pair = (yield from acq(1))[0]
            b = pair * 2

            def f(e):
                ins = None
                for fc in range(FC):
                    ins = e.matmul(ps[b][:, 0:CT], lhsT=ONESB[:], rhs=cx.B1[:, fc, :],
                                   start=(fc == 0), stop=(fc == FC - 1))
                return ins
            P.op("pe", f, reads=["ONESB"] + cx.ks("B1", FC), writes=psk(b))
            yield (1.0, 1.0)
            P.op("act", lambda e: e.activation(out=cx.RS[:], in_=ps[b][:, 0:CT], func=AF.Sqrt, bias=EPS_COL, scale=1.0 / D),
                 reads=psk(b) + ["SM"], writes=[cx.k("RS")])
            P.op("dve", lambda e: e.reciprocal(out=cx.RS[:], in_=cx.RS[:]), reads=[cx.k("RS")], writes=[cx.k("RS")])
            banks.rel([pair])

        def norm_mod(cx, l, s, which_a, which_sh):
            a_ap, a_k = par(l, s, which_a)
            sh_ap, sh_k = par(l, s, which_sh)
            for fc in range(FC):
                eng = ("act", "pool", "dve")[fc % 3]
                if eng == "act":
                    P.op("act", lambda e, fc=fc: e.activation(out=cx.B1[:, fc, :], in_=cx.F0[:, fc, :], func=AF.Square),
                         reads=[cx.k("F0", fc)], writes=[cx.k("B1", fc)])
                else:
                    P.op(eng, lambda e, fc=fc: e.tensor_tensor(out=cx.B1[:, fc, :], in0=cx.F0[:, fc, :], in1=cx.F0[:, fc, :],
                                                               op=ALU.mult),
                         reads=[cx.k("F0", fc)], writes=[cx.k("B1", fc)])
            yield (0.0, 8.0)
            yield from rstd_from_sq(cx)
            for fc in range(FC):
                P.op("dve", lambda e, fc=fc: e.tensor_tensor(out=cx.TMPA[:, fc % 2, :], in0=cx.F0[:, fc, :],
                                                             in1=cx.RS[:], op=ALU.mult),
                     reads=[cx.k("F0", fc), cx.k("RS")], writes=[cx.k("TMPA", fc % 2)])
                P.op("act", lambda e, fc=fc: e.activation(out=cx.B0[:, fc, :], in_=cx.TMPA[:, fc % 2, :],
                                                          func=AF.Identity, bias=sh_ap[:, fc:fc + 1],
                                                          scale=a_ap[:, fc:fc + 1]),
                     reads=[cx.k("TMPA", fc % 2)] + a_k + sh_k, writes=[cx.k("B0", fc)])
            yield (0.0, 12.0)

        def evac_branch(cx, pair, fc0):
            for mi in range(4):
                fc = fc0 + mi
                b, c0 = reg(pair, mi, CT)
                if mi % 2 == 0:
                    P.op("dve", lambda e, fc=fc, b=b, c0=c0: e.tensor_copy(out=cx.F1[:, fc, :], in_=ps[b][:, c0:c0 + CT]),
                         reads=psk(b), writes=[cx.k("F1", fc)])
                else:
                    P.op("act", lambda e, fc=fc, b=b, c0=c0: e.activation(out=cx.F1[:, fc, :], in_=ps[b][:, c0:c0 + CT], func=AF.Copy),
                         reads=psk(b), writes=[cx.k("F1", fc)])
                P.op("pool", lambda e, fc=fc: e.tensor_tensor(out=cx.B1[:, fc, :], in0=cx.F1[:, fc, :], in1=cx.F1[:, fc, :],
                                                              op=ALU.mult),
                     reads=[cx.k("F1", fc)], writes=[cx.k("B1", fc)])

        def residual(cx, l, s, which_g):
            g_ap, g_k = par(l, s, which_g)
            yield from rstd_from_sq(cx)
            for fc in range(FC):
                P.op("dve", lambda e, fc=fc: e.scalar_tensor_tensor(
                    out=cx.TMPA[:, fc % 2, :], in0=cx.F1[:, fc, :], scalar=g_ap[:, fc:fc + 1],
                    in1=cx.RS[:], op0=ALU.mult, op1=ALU.mult),
                    reads=[cx.k("F1", fc), cx.k("RS")] + g_k, writes=[cx.k("TMPA", fc % 2)])
                P.op("pool", lambda e, fc=fc: e.tensor_tensor(out=cx.F0[:, fc, :], in0=cx.F0[:, fc, :],
                                                              in1=cx.TMPA[:, fc % 2, :], op=ALU.add),
                     reads=[cx.k("F0", fc), cx.k("TMPA", fc % 2)], writes=[cx.k("F0", fc)])
            yield (0.0, 12.0)

        def mlp(cx, l, s):
            yield from norm_mod(cx, l, s, "a2", "sh2")
            w1n, w2n = f"w1_{l}", f"w2_{l}"
            for j in range(8):
                pair = (yield from acq(1))[0]
                slot, wkey = w_next((w1n, 0, 512 * j))
                fm_mm(cx, slot, wkey, cx.B0, "B0", 0, pair, True, True)
                for mi in range(4):
                    hc = 4 * j + mi
                    b, c0 = reg(pair, mi, CT)
                    P.op("act", lambda e, b=b, c0=c0, hc=hc: e.activation(out=cx.TMPA[:, hc % 2, :], in_=ps[b][:, c0:c0 + CT],
                                                                          func=AF.Relu),
                         reads=psk(b), writes=[cx.k("TMPA", hc % 2)])
                    eng = "dve" if (hc % 2 == 0) else "pool"
                    P.op(eng, lambda e, hc=hc: e.tensor_tensor(out=cx.HID[:, hc, :], in0=cx.TMPA[:, hc % 2, :],
                                                               in1=cx.TMPA[:, hc % 2, :], op=ALU.mult),
                         reads=[cx.k("TMPA", hc % 2)], writes=[cx.k("HID", hc)])
                banks.rel([pair])
                yield (3.7, 1.5)
            for ch in range(2):
                pair = (yield from acq(1))[0]
                for kg in range(4):
                    slot, wkey = w_next((w2n, 1024 * kg, 512 * ch))
                    fm_mm(cx, slot, wkey, cx.HID, "HID", 8 * kg, pair, kg == 0, kg == 3)
                    if kg < 3:
                        yield (3.7, 0.0)
                evac_branch(cx, pair, 4 * ch)
                banks.rel([pair])
                yield (3.7, 4.0)
            yield from residual(cx, l, s, "g2")

        def load_tokens(cx, src_rows):
            xtok = cx.F2[:].rearrange("p a b -> p (a b)").rearrange("p (t f) -> p t f", f=D)
            P.dma("sp", lambda e: e.dma_start(out=xtok, in_=src_rows.rearrange("(t p) f -> p t f", p=128)),
                  writes=cx.ks("F2", FC))
            yield (0.0, 12.0)
            for g in range(2):
                pair = (yield from acq(1))[0]
                for q in range(4):
                    fc = g * 4 + q
                    b, c0 = reg(pair, q, CT)

                    def f(e, fc=fc, b=b, c0=c0):
                        ins = None
                        for tt in range(NCH):
                            ins = e.transpose(out=ps[b][:, c0 + tt * 128:c0 + (tt + 1) * 128],
                                              in_=xtok[:, tt, fc * 128:(fc + 1) * 128], identity=IDENT)
                        return ins
                    P.op("pe", f, reads=cx.ks("F2", FC) + ["CST"], writes=psk(b))
                    if fc % 2 == 0:
                        P.op("act", lambda e, fc=fc, b=b, c0=c0: e.activation(out=cx.F0[:, fc, :], in_=ps[b][:, c0:c0 + CT], func=AF.Copy),
                             reads=psk(b), writes=[cx.k("F0", fc)])
                    else:
                        P.op("dve", lambda e, fc=fc, b=b, c0=c0: e.tensor_copy(out=cx.F0[:, fc, :], in_=ps[b][:, c0:c0 + CT]),
                             reads=psk(b), writes=[cx.k("F0", fc)])
                banks.rel([pair])
                yield (1.8, 1.5)

        def layer0(cx, s, rowlen):
            yield from norm_mod(cx, 0, s, "a1", "sh1")
            BG = lambda fc: cx.HID[:, fc, :]
            CG = lambda fc: cx.HID[:, 8 + fc, :]
            GG = lambda fc: cx.HID[:, 16 + fc, :]
            for j in range(6):
                pair = (yield from acq(1))[0]
                slot, wkey = w_next(("win0", 0, 512 * j))
                fm_mm(cx, slot, wkey, cx.B0, "B0", 0, pair, True, True)
                for mi in range(4):
                    b, c0 = reg(pair, mi, CT)
                    m = 4 * j + mi
                    src = ps[b][:, c0:c0 + CT]
                    if m < 8:
                        P.op("act", lambda e, m=m, src=src: e.activation(out=BG(m), in_=src, func=AF.Copy),
                             reads=psk(b), writes=[cx.k("HID", m)])
                    elif m < 16:
                        P.op("act", lambda e, m=m, src=src: e.activation(out=CG(m - 8), in_=src, func=AF.Copy),
                             reads=psk(b), writes=[cx.k("HID", m)])
                    else:
                        fc = m - 16
                        P.op("dve", lambda e, fc=fc, src=src: e.tensor_tensor(out=cx.F2[:, fc, :], in0=src, in1=CG(fc), op=ALU.mult),
                             reads=psk(b) + [cx.k("HID", 8 + fc)], writes=[cx.k("F2", fc)])
                banks.rel([pair])
                yield (3.7, 2.0)
            cw = lambda k, fc: SM[:, S_CW + k * 8 + fc:S_CW + k * 8 + fc + 1]
            for fc in range(FC):
                yv = cx.TMPA[:, fc % 2, :]
                y3 = yv.rearrange("p (r w) -> p r w", w=rowlen)
                u3 = cx.F2[:, fc, :].rearrange("p (r w) -> p r w", w=rowlen)
                P.op("act", lambda e, fc=fc, yv=yv: e.activation(out=yv, in_=cx.F2[:, fc, :], func=AF.Identity,
                                                                 bias=0.0, scale=cw(1, fc)),
                     reads=[cx.k("F2", fc), "SM"], writes=[cx.k("TMPA", fc % 2)])
                P.op("dve", lambda e, fc=fc, y3=y3, u3=u3: e.scalar_tensor_tensor(
                    out=y3[:, :, 1:rowlen], in0=u3[:, :, 0:rowlen - 1], scalar=cw(0, fc), in1=y3[:, :, 1:rowlen],
                    op0=ALU.mult, op1=ALU.add),
                    reads=[cx.k("F2", fc), cx.k("TMPA", fc % 2), "SM"], writes=[cx.k("TMPA", fc % 2)])
                P.op("dve", lambda e, fc=fc, y3=y3, u3=u3: e.scalar_tensor_tensor(
                    out=y3[:, :, 0:rowlen - 1], in0=u3[:, :, 1:rowlen], scalar=cw(2, fc), in1=y3[:, :, 0:rowlen - 1],
                    op0=ALU.mult, op1=ALU.add),
                    reads=[cx.k("F2", fc), cx.k("TMPA", fc % 2), "SM"], writes=[cx.k("TMPA", fc % 2)])
                P.op("pool", lambda e, fc=fc, yv=yv: e.tensor_tensor(out=GG(fc), in0=yv, in1=BG(fc), op=ALU.mult),
                     reads=[cx.k("TMPA", fc % 2), cx.k("HID", fc)], writes=[cx.k("HID", 16 + fc)])
                if fc % 4 == 3:
                    yield (0.0, 6.0)
            for j in range(2):
                pair = (yield from acq(1))[0]
                slot, wkey = w_next(("wout0", 0, 512 * j))
                fm_mm(cx, slot, wkey, cx.HID, "HID", 16, pair, True, True)
                evac_branch(cx, pair, 4 * j)
                banks.rel([pair])
                yield (3.7, 4.0)
            yield from residual(cx, 0, s, "g1")
            yield from mlp(cx, 0, s)

        def gates_mm(cx, gi0):
            pair = (yield from acq(1))[0]
            bank = pair * 2

            def f(e):
                ins = None
                for c in range(NCH):
                    for kc in range(FC):
                        ins = e.matmul(ps[bank][:, c * 32:(c + 1) * 32], lhsT=cx.B0[:, kc, c * 128:(c + 1) * 128],
                                       rhs=WIFB[:, kc, :], start=(kc == 0), stop=(kc == FC - 1))
                return ins
            P.op("pe", f, reads=["WIFB"] + cx.ks("B0", FC), writes=psk(bank))
            for c in range(NCH):
                P.op("dve", lambda e, c=c: e.tensor_tensor(out=GT[:, gi0 + c, :], in0=ps[bank][:, c * 32:(c + 1) * 32],
                                                           in1=SM[:, S_BIF:S_BIF + 32], op=ALU.add),
                     reads=psk(bank) + ["SM"], writes=[("GT", gi0 + c)])
            banks.rel([pair])
            gks = [("GT", gi0 + c) for c in range(NCH)]
            lks = [("LG", gi0 + c) for c in range(NCH)]
            P.op("act", lambda e: e.activation(out=LG[:, gi0:gi0 + NCH, :], in_=GT[:, gi0:gi0 + NCH, :], func=AF.Exp, scale=-1.0),
                 reads=gks, writes=lks)
            P.op("act", lambda e: e.activation(out=LG[:, gi0:gi0 + NCH, :], in_=LG[:, gi0:gi0 + NCH, :], func=AF.Ln,
                                               bias=ONE_COL, scale=1.0),
                 reads=lks + ["SM"], writes=lks)
            yield (1.0, 3.0)

        def scan_chunk(cx, d, gi, c, with_out, first_dir=True):
            pairs = yield from acq(2)
            bY = [pairs[0] * 2, pairs[0] * 2 + 1]
            bXs = [pairs[1] * 2, pairs[1] * 2 + 1]
            gb = pairs[1] * 2 + 1
            QT = cx.HID[:, 0:8, :]
            KT = cx.HID[:, 8:16, :]
            igs = GT[:, gi, d * 16:d * 16 + 8]
            fps = GT[:, gi, d * 16 + 8:d * 16 + 16]
            gk = [("GT", gi)]
            lfn = LG[:, gi, d * 16 + 8:d * 16 + 16]

            def g3(e):
                e.matmul(ps[gb][:, 0:8], lhsT=MINC[d], rhs=lfn, start=True, stop=True)
                e.matmul(ps[gb][:, 8:16], lhsT=MSTR[d], rhs=lfn, start=True, stop=True)
                return e.matmul(ps[gb][:, 16:24], lhsT=NEG1, rhs=lfn, start=True, stop=True)
            P.op("pe", g3, reads=[("LG", gi), "CST"], writes=psk(gb))
            C1 = G3S[:, 0:8]
            EB = G3S[:, 8:16]
            WKP = G3S[:, 16:24]
            WK = G3S[:, 24:32]
            AT = G3S[:, 32:40]
            if with_out:
                for h in range(NH):
                    if h % 2 == 0:
                        P.op("dve", lambda e, h=h: e.tensor_scalar(out=LFB[:, h, :], in0=ONES32, scalar1=LG[:, gi, d * 16 + 8 + h:d * 16 + 9 + h],
                                                                   scalar2=None, op0=ALU.mult),
                             reads=[("LG", gi), "CST"], writes=[("LFB", h)])
                    else:
                        P.op("act", lambda e, h=h: e.activation(out=LFB[:, h, :], in_=ONES32, func=AF.Identity, bias=0.0,
                                                                scale=LG[:, gi, d * 16 + 8 + h:d * 16 + 9 + h]),
                             reads=[("LG", gi), "CST"], writes=[("LFB", h)])
            yield (0.3, 6.0)
            if with_out:
                P.op("dve", lambda e: e.tensor_tensor(out=C1, in0=igs, in1=ps[gb][:, 0:8], op=ALU.subtract),
                     reads=gk + psk(gb), writes=[("G3S", 0)])
            P.op("dve", lambda e: e.tensor_tensor(out=WKP, in0=igs, in1=ps[gb][:, 8:16], op=ALU.add),
                 reads=gk + psk(gb), writes=[("G3S", 2)])
            if with_out:
                P.op("act", lambda e: e.activation(out=EB, in_=ps[gb][:, 0:8], func=AF.Exp),
                     reads=psk(gb), writes=[("G3S", 1)])
            P.op("act", lambda e: e.activation(out=AT, in_=ps[gb][:, 16:24], func=AF.Exp),
                 reads=psk(gb), writes=[("G3S", 4)])
            P.op("act", lambda e: e.activation(out=WK, in_=WKP, func=AF.Exp), reads=[("G3S", 2)], writes=[("G3S", 3)])
            for h in range(NH):
                if h % 2 == 0:
                    P.op("dve", lambda e, h=h: e.tensor_scalar(out=KW[:, h, :], in0=cx.KTOK[:, c, h * 64:(h + 1) * 64],
                                                               scalar1=G3S[:, 24 + h:25 + h], scalar2=None, op0=ALU.mult),
                         reads=[cx.k("KTOK", c), ("G3S", 3)], writes=[("KW", h)])
                else:
                    P.op("act", lambda e, h=h: e.activation(out=KW[:, h, :], in_=cx.KTOK[:, c, h * 64:(h + 1) * 64],
                                                            func=AF.Identity, bias=0.0, scale=G3S[:, 24 + h:25 + h]),
                         reads=[cx.k("KTOK", c), ("G3S", 3)], writes=[("KW", h)])
            yield (0.0, 10.0)
            cs = slice(c * 128, (c + 1) * 128)
            vk = cx.k("VAUG", c)

            def head_front(h):
                by = bY[h % 2]
                rot = h % 2

                def bbm(e):
                    e.matmul(ps[by][:, 0:128], lhsT=LFB[:, h, :], rhs=MINC[d], start=True, stop=False)
                    e.matmul(ps[by][:, 0:128], lhsT=IDENT, rhs=NBIGM[d], start=False, stop=True)
                    return e.matmul(ps[by][:, 128:256], lhsT=KT[0:64, h, cs], rhs=QT[0:64, h, cs], start=True, stop=True)
                P.op("pe", bbm, reads=[("LFB", h), "CST", cx.k("HID", 8 + h), cx.k("HID", h)], writes=psk(by))
                P.op("act", lambda e: e.activation(out=DTT[:, rot, :], in_=ps[by][:, 0:128], func=AF.Exp,
                                                   bias=G3S[:, h:h + 1], scale=1.0),
                     reads=psk(by) + [("G3S", 0)], writes=[("DTT", rot)])
                P.op("dve", lambda e: e.tensor_tensor(out=PTT[:, rot, :], in0=ps[by][:, 128:256], in1=DTT[:, rot, :], op=ALU.mult),
                     reads=psk(by) + [("DTT", rot)], writes=[("PTT", rot)])

            def head_back(h):
                rot = h % 2
                bX = bXs[h % 2]
                vrhs = cx.VAUG[:, c, h, 0:DV + 1]

                def mm(e):
                    ins = None
                    if with_out:
                        e.matmul(ps[bX][:, 0:DV + 1], lhsT=PTT[:, rot, :], rhs=vrhs, start=True, stop=True)
                        e.matmul(ps[bX][:, 130:130 + DV + 1], lhsT=QT[0:64, h, cs], rhs=CST_B[d][:, h, 0:DV + 1],
                                 start=True, stop=True)
                    return e.matmul(ps[bX][0:64, 260:260 + DV + 1], lhsT=KW[:, h, :], rhs=vrhs, start=True, stop=True)
                rd = [("KW", h), vk]
                if with_out:
                    rd += [("PTT", rot), cx.k("HID", h), ("CB", d, h)]
                P.op("pe", mm, reads=rd, writes=psk(bX))
                if with_out:
                    P.op("act", lambda e: e.activation(out=T0[:, rot, 0:DV + 1], in_=ps[bX][:, 0:DV + 1], func=AF.Copy),
                         reads=psk(bX), writes=[("T0", rot)])
                    P.op("dve", lambda e: e.scalar_tensor_tensor(
                        out=T1[:, h, 0:DV + 1], in0=ps[bX][:, 130:130 + DV + 1], scalar=G3S[:, 8 + h:9 + h],
                        in1=T0[:, rot, 0:DV + 1], op0=ALU.mult, op1=ALU.add),
                        reads=psk(bX) + [("G3S", 1), ("T0", rot)], writes=[("T1", h)])
                P.op("dve", lambda e: e.scalar_tensor_tensor(
                    out=CST_C[d][:, h, 0:DV + 1], in0=CST_C[d][:, h, 0:DV + 1], scalar=G3S[0:64, 32 + h:33 + h],
                    in1=ps[bX][0:64, 260:260 + DV + 1], op0=ALU.mult, op1=ALU.add),
                    reads=[("CS", d, h), ("G3S", 4)] + psk(bX), writes=[("CS", d, h)])
                P.op("act", lambda e: e.activation(out=CST_B[d][:, h, 0:DV + 1], in_=CST_C[d][:, h, 0:DV + 1], func=AF.Copy),
                     reads=[("CS", d, h)], writes=[("CB", d, h)])

            if with_out:
                head_front(0)
                yield (0.7, 6.0)
            for h in range(NH):
                if with_out and h + 1 < NH:
                    head_front(h + 1)
                head_back(h)
                yield (1.0, 4.0)
            if with_out:
                den = T1[:, :, DV]
                P.op("act", lambda e: e.activation(out=RD[:, 0:8], in_=den, func=AF.Abs),
                     reads=[("T1", h) for h in range(NH)], writes=[("RD", 0)])
                P.op("dve", lambda e: e.tensor_scalar_max(out=RD[:, 0:8], in0=RD[:, 0:8], scalar1=1.0),
                     reads=[("RD", 0)], writes=[("RD", 0)])
                P.op("dve", lambda e: e.reciprocal(out=RD[:, 8:16], in_=RD[:, 0:8]), reads=[("RD", 0)], writes=[("RD", 1)])
                hs = cx.F1[:].rearrange("p a b -> p (a b)").rearrange("p (c f) -> p c f", f=D)
                for h in range(NH):
                    dst = hs[:, c, h * DV:(h + 1) * DV]
                    key = cx.k("F1", 4 * c + h // 2)
                    if first_dir:
                        eng = "act" if h % 2 == 0 else "pool"
                        if eng == "act":
                            P.op("act", lambda e, h=h, dst=dst: e.activation(out=dst, in_=T1[:, h, 0:DV], func=AF.Identity,
                                                                             bias=0.0, scale=RD[:, 8 + h:9 + h]),
                                 reads=[("T1", h), ("RD", 1)], writes=[key])
                        else:
                            P.op("dve", lambda e, h=h, dst=dst: e.tensor_scalar(out=dst, in0=T1[:, h, 0:DV],
                                                                                 scalar1=RD[:, 8 + h:9 + h], scalar2=None,
                                                                                 op0=ALU.mult),
                                 reads=[("T1", h), ("RD", 1)], writes=[key])
                    else:
                        P.op("dve", lambda e, h=h, dst=dst: e.scalar_tensor_tensor(
                            out=dst, in0=T1[:, h, 0:DV], scalar=RD[:, 8 + h:9 + h], in1=dst, op0=ALU.mult, op1=ALU.add),
                            reads=[("T1", h), ("RD", 1), key], writes=[key])
            banks.rel(pairs)
            yield (0.0, 4.0)

        def l1_proj(cx, full):
            if full:
                pairs = yield from acq(2)
                slot, wkey = w_next(("qkvo", 0, 0))
                for g in range(2):
                    pair = pairs[g]
                    fm_mm(cx, slot, wkey, cx.B0, "B0", 0, pair, True, True, ncol=4, msize=64, col0=g * 256)
                    for q in range(4):
                        h = g * 4 + q
                        b, c0 = reg(pair, q, CT)
                        P.op("act", lambda e, h=h, b=b, c0=c0: e.activation(out=cx.HID[0:64, h, :], in_=ps[b][0:64, c0:c0 + CT],
                                                                            func=AF.Copy, scale=DQK ** -0.5),
                             reads=psk(b), writes=[cx.k("HID", h)])
                banks.rel(pairs)
                yield (3.7, 2.0)
            pairs = yield from acq(2)
            slot, wkey = w_next(("qkvo", 0, 512))
            if full:
                for g in range(2):
                    pair = pairs[g]
                    fm_mm(cx, slot, wkey, cx.B0, "B0", 0, pair, True, True, ncol=4, msize=64, col0=g * 256)
                    for q in range(4):
                        h = g * 4 + q
                        b, c0 = reg(pair, q, CT)
                        P.op("dve", lambda e, h=h, b=b, c0=c0: e.tensor_copy(out=cx.HID[0:64, 8 + h, :], in_=ps[b][0:64, c0:c0 + CT]),
                             reads=psk(b), writes=[cx.k("HID", 8 + h)])
            pair = pairs[0]
            for c in range(NCH):
                b = pair * 2 + c

                def f(e, c=c, b=b, slot=slot):
                    ins = None
                    for kc in range(FC):
                        ins = e.matmul(ps[b][:, 0:512], lhsT=cx.B0[:, kc, c * 128:(c + 1) * 128], rhs=slot[:, kc, :],
                                       start=(kc == 0), stop=(kc == FC - 1))
                    return ins
                P.op("pe", f, reads=[wkey] + cx.ks("B0", FC), writes=psk(b))
                P.op("act", lambda e, c=c, b=b: e.activation(out=cx.KTOK[:, c, :], in_=ps[b][:, 0:512], func=AF.Copy),
                     reads=psk(b), writes=[cx.k("KTOK", c)])
            banks.rel(pairs)
            yield (5.5, 2.0)
            for j in range(2):
                pair = (yield from acq(1))[0]
                slot, wkey = w_next(("qkvo", 0, 1024 + 512 * j))
                for c in range(NCH):
                    b = pair * 2 + c

                    def f(e, c=c, b=b, slot=slot):
                        ins = None
                        for kc in range(FC):
                            ins = e.matmul(ps[b][:, 0:512], lhsT=cx.B0[:, kc, c * 128:(c + 1) * 128], rhs=slot[:, kc, :],
                                           start=(kc == 0), stop=(kc == FC - 1))
                        return ins
                    P.op("pe", f, reads=[wkey] + cx.ks("B0", FC), writes=psk(b))
                    dst = cx.VAUG[:, c, 4 * j:4 * j + 4, 0:DV]
                    src = ps[b][:, 0:512].rearrange("p (h v) -> p h v", v=DV)
                    if c % 2 == 0:
                        P.op("dve", lambda e, dst=dst, src=src: e.tensor_copy(out=dst, in_=src),
                             reads=psk(b), writes=[cx.k("VAUG", c)])
                    else:
                        P.op("act", lambda e, dst=dst, src=src: e.activation(out=dst, in_=src, func=AF.Copy),
                             reads=psk(b), writes=[cx.k("VAUG", c)])
                banks.rel([pair])
                yield (3.6, 2.0)
            if full:
                for j in range(2):
                    pair = (yield from acq(1))[0]
                    slot, wkey = w_next(("qkvo", 0, 2048 + 512 * j))
                    fm_mm(cx, slot, wkey, cx.B0, "B0", 0, pair, True, True)
                    for mi in range(4):
                        h = 4 * j + mi
                        b, c0 = reg(pair, mi, CT)
                        P.op("act", lambda e, h=h, b=b, c0=c0: e.activation(out=cx.HID[:, 16 + h, :], in_=ps[b][:, c0:c0 + CT], func=AF.Sigmoid),
                             reads=psk(b), writes=[cx.k("HID", 16 + h)])
                    banks.rel([pair])
                    yield (3.7, 2.0)


        def wait_flag(name):
            while not flags.get(name):
                yield "blocked"

        flat3 = lambda t: t[:].rearrange("p a b -> p (a b)")

        def ctx_context(cx):
            yield from load_tokens(cx, ctx_d)
            yield from layer0(cx, 1, CTX)
            yield from wait_flag(("mod", 1))
            yield from norm_mod(cx, 1, 1, "a1", "sh1")
            yield from l1_proj(cx, False)
            yield from gates_mm(cx, 2 * NCX)
            for d in range(2):
                order = [0, 1] if d == 0 else [1, 0]
                for c in order:
                    yield from scan_chunk(cx, d, 2 * NCX + c, c, False)
            flags[("fscan", -1)] = True
            flags[("bscan", NCX)] = True

        def sweep1_context(cx, i):
            yield from load_tokens(cx, x_d[i * CT:(i + 1) * CT, :])
            yield from layer0(cx, 0, 64)
            yield from wait_flag(("mod", 1))
            P.dma("sp", lambda e: e.dma_start(out=x1_s[i], in_=flat3(cx.F0)), reads=cx.ks("F0", FC), writes=[("x1_s", i)])
            if DEBUG:
                P.dma("sp", lambda e: e.dma_start(out=dbg_d[i], in_=flat3(cx.F0)), reads=cx.ks("F0", FC), writes=[("dbg", i)])
            yield from norm_mod(cx, 1, 0, "a1", "sh1")
            yield from l1_proj(cx, True)
            yield from gates_mm(cx, 2 * i)
            P.dma("sp", lambda e: e.dma_start(out=qt_s[i], in_=cx.HID[0:64, 0:8, :].rearrange("p a b -> p (a b)")),
                  reads=[cx.k("HID", h) for h in range(8)], writes=[("qt_s", i)])
            P.dma("sp", lambda e: e.dma_start(out=kt_s[i], in_=cx.HID[0:64, 8:16, :].rearrange("p a b -> p (a b)")),
                  reads=[cx.k("HID", 8 + h) for h in range(8)], writes=[("kt_s", i)])
            P.dma("sp", lambda e: e.dma_start(out=so_s[i], in_=cx.HID[:, 16:24, :].rearrange("p a b -> p (a b)")),
                  reads=[cx.k("HID", 16 + h) for h in range(8)], writes=[("so_s", i)])
            P.dma("sp", lambda e: e.dma_start(out=ktok_s[i], in_=flat3(cx.KTOK)), reads=cx.ks("KTOK", NCH), writes=[("ktok_s", i)])
            P.dma("sp", lambda e: e.dma_start(out=vaug_s[i], in_=cx.VAUG[:].rearrange("p a b c -> p (a b c)")),
                  reads=cx.ks("VAUG", NCH), writes=[("vaug_s", i)])
            yield from wait_flag(("fscan", i - 1))
            for c in range(NCH):
                yield from scan_chunk(cx, 0, 2 * i + c, c, True, first_dir=True)
            flags[("fscan", i)] = True
            P.dma("sp", lambda e: e.dma_start(out=hf_s[i], in_=flat3(cx.F1)), reads=cx.ks("F1", FC), writes=[("hf_s", i)])
            flags[("s1done", i)] = True
            yield (0.0, 0.0)

        def sweep2_context(cx, i):
            yield from wait_flag(("s1done", i))
            P.dma("sp", lambda e: e.dma_start(out=cx.HID[0:64, 0:8, :].rearrange("p a b -> p (a b)"), in_=qt_s[i]),
                  reads=[("qt_s", i)], writes=[cx.k("HID", h) for h in range(8)])
            P.dma("sp", lambda e: e.dma_start(out=cx.HID[0:64, 8:16, :].rearrange("p a b -> p (a b)"), in_=kt_s[i]),
                  reads=[("kt_s", i)], writes=[cx.k("HID", 8 + h) for h in range(8)])
            P.dma("sp", lambda e: e.dma_start(out=flat3(cx.KTOK), in_=ktok_s[i]), reads=[("ktok_s", i)], writes=cx.ks("KTOK", NCH))
            P.dma("sp", lambda e: e.dma_start(out=cx.VAUG[:].rearrange("p a b c -> p (a b c)"), in_=vaug_s[i]),
                  reads=[("vaug_s", i)], writes=cx.ks("VAUG", NCH))
            P.dma("sp", lambda e: e.dma_start(out=flat3(cx.F1), in_=hf_s[i]), reads=[("hf_s", i)], writes=cx.ks("F1", FC))
            P.dma("sp", lambda e: e.dma_start(out=cx.HID[:, 16:24, :].rearrange("p a b -> p (a b)"), in_=so_s[i]),
                  reads=[("so_s", i)], writes=[cx.k("HID", 16 + h) for h in range(8)])
            P.dma("sp", lambda e: e.dma_start(out=flat3(cx.F0), in_=x1_s[i]), reads=[("x1_s", i)], writes=cx.ks("F0", FC))
            yield (0.0, 15.0)
            yield from wait_flag(("bscan", i + 1))
            hs = cx.F1[:].rearrange("p a b -> p (a b)").rearrange("p (c f) -> p c f", f=D)
            for c in reversed(range(NCH)):
                yield from scan_chunk(cx, 1, 2 * i + c, c, True, first_dir=False)
                if c == 0:
                    flags[("bscan", i)] = True
                hkeys = [cx.k("F1", 4 * c + q) for q in range(4)]
                sq = cx.F2[:].rearrange("p a b -> p (a b)")[:, 0:D]
                sqk = [cx.k("F2", q) for q in range(4)]
                P.op("dve", lambda e, c=c, sq=sq: e.tensor_tensor(out=sq, in0=hs[:, c, :], in1=hs[:, c, :], op=ALU.mult),
                     reads=hkeys, writes=sqk)
                P.op("dve", lambda e, sq=sq: e.tensor_reduce(out=cx.SSQ[:, 0:8], in_=sq.rearrange("p (h v) -> p h v", v=DV),
                                                             axis=AX.X, op=ALU.add),
                     reads=sqk, writes=[cx.k("SSQ", 0)])
                P.op("act", lambda e: e.activation(out=cx.SSQ[:, 8:16], in_=cx.SSQ[:, 0:8], func=AF.Sqrt, bias=EPS_COL, scale=1.0 / DV),
                     reads=[cx.k("SSQ", 0), "SM"], writes=[cx.k("SSQ", 1)])
                P.op("dve", lambda e: e.reciprocal(out=cx.SSQ[:, 8:16], in_=cx.SSQ[:, 8:16]), reads=[cx.k("SSQ", 1)], writes=[cx.k("SSQ", 1)])
                yield (0.0, 8.0)
                pair = (yield from acq(1))[0]
                for h in range(NH):
                    rot = h % 2
                    b = pair * 2 + rot
                    P.op("act", lambda e, c=c, h=h, rot=rot: e.activation(out=cx.HN[:, rot, :], in_=hs[:, c, h * DV:(h + 1) * DV],
                                                                          func=AF.Identity, bias=0.0, scale=cx.SSQ[:, 8 + h:9 + h]),
                         reads=hkeys + [cx.k("SSQ", 1)], writes=[cx.k("HN", rot)])
                    P.op("pe", lambda e, rot=rot, b=b: e.transpose(out=ps[b][:, 0:128], in_=cx.HN[:, rot, :], identity=IDENT),
                         reads=[cx.k("HN", rot), "CST"], writes=psk(b))
                    P.op("dve", lambda e, c=c, h=h, b=b: e.scalar_tensor_tensor(
                        out=cx.HID[:, 24 + h, c * 128:(c + 1) * 128], in0=ps[b][:, 0:128],
                        scalar=SM[:, S_MLNW + h:S_MLNW + h + 1], in1=cx.HID[:, 16 + h, c * 128:(c + 1) * 128],
                        op0=ALU.mult, op1=ALU.mult),
                        reads=psk(b) + ["SM", cx.k("HID", 16 + h)], writes=[cx.k("HID", 24 + h)])
                    if h % 2 == 1:
                        yield (0.5, 3.0)
                banks.rel([pair])
            for j in range(2):
                pair = (yield from acq(1))[0]
                slot, wkey = w_next(("mlout", 0, 512 * j))
                fm_mm(cx, slot, wkey, cx.HID, "HID", 24, pair, True, True)
                evac_branch(cx, pair, 4 * j)
                banks.rel([pair])
                yield (3.7, 4.0)
            yield from residual(cx, 1, 0, "g1")
            yield from mlp(cx, 1, 0)
            xt = cx.F2[:].rearrange("p a b -> p (a b)").rearrange("p (t f) -> p t f", f=D)
            for tt in range(NCH):
                pair = (yield from acq(1))[0]
                for half in range(2):
                    b = pair * 2 + half

                    def f(e, tt=tt, half=half, b=b):
                        ins = None
                        for q in range(4):
                            fc = half * 4 + q
                            ins = e.transpose(out=ps[b][:, q * 128:(q + 1) * 128], in_=cx.F0[:, fc, tt * 128:(tt + 1) * 128],
                                              identity=IDENT)
                        return ins
                    P.op("pe", f, reads=[cx.k("F0", half * 4 + q) for q in range(4)] + ["CST"], writes=psk(b))
                    wk = [cx.k("F2", tt * 4 + half * 2), cx.k("F2", tt * 4 + half * 2 + 1)]
                    if half == 0:
                        P.op("act", lambda e, tt=tt, half=half, b=b: e.activation(out=xt[:, tt, half * 512:(half + 1) * 512],
                                                                                  in_=ps[b][:, 0:512], func=AF.Copy),
                             reads=psk(b), writes=wk)
                    else:
                        P.op("dve", lambda e, tt=tt, half=half, b=b: e.tensor_copy(out=xt[:, tt, half * 512:(half + 1) * 512],
                                                                                   in_=ps[b][:, 0:512]),
                             reads=psk(b), writes=wk)
                banks.rel([pair])
                yield (1.8, 2.0)
            P.dma("sp", lambda e: e.dma_start(out=out_d[i * CT:(i + 1) * CT, :].rearrange("(t p) f -> p t f", p=128), in_=xt),
                  reads=cx.ks("F2", FC), writes=[("out", i)])
            yield (0.0, 0.0)

        jobs = [("ctx", None)] + [("s1", i) for i in range(NCX_RUN)] + [("s2", i) for i in reversed(range(NCX_RUN))]
        if STOP == "ctx":
            jobs = jobs[:1]
        elif STOP == "sweep1":
            jobs = jobs[:1 + NCX_RUN]

        def make(job, cx):
            kind, i = job
            if kind == "ctx":
                return ctx_context(cx)
            if kind == "s1":
                return sweep1_context(cx, i)
            return sweep2_context(cx, i)

        if NCX_RUN < NCX:
            for i in range(NCX_RUN, NCX):
                flags[("s1done", i)] = True
            flags[("bscan", NCX_RUN)] = True
        active = [[mod_layer(1), None, 0.0]]
        free_cx = [CXS[0], CXS[1]]
        pending = list(jobs)

        pe_clock = 0.0
        guard = 0
        while active or pending:
            guard += 1
            assert guard < 2000000, "scheduler livelock"
            while pending and free_cx:
                cx_ = free_cx.pop(0)
                active.append([make(pending.pop(0), cx_), cx_, pe_clock])
            order = sorted(active, key=lambda a_: max(a_[2], pe_clock))
            progressed = False
            for ent in order:
                try:
                    r = next(ent[0])
                except StopIteration:
                    active.remove(ent)
                    if ent[1] is not None:
                        free_cx.append(ent[1])
                    progressed = True
                    break
                if r == "blocked":
                    continue
                pe_us, lat_us = r if r is not None else (0.0, 0.0)
                start = max(pe_clock, ent[2])
                pe_clock = start + pe_us
                ent[2] = pe_clock + lat_us
                progressed = True
                break
            assert progressed, "all pipeline contexts blocked"
        assert not pending
        assert dry or wstate["used"] == len(sched), (wstate, len(sched))
        if dry:
            return sched
        P.emit()
    return nc


_NC_CACHE = {}


def kernel(x, c, ctx, c_ctx, norm_w, mod_w, mod_b, mlp_w1, mlp_w2, conv_w_in, conv_w, conv_w_out,
           ml_w_qkvo, ml_w_if, ml_b_if, ml_norm_w, ml_w_out):
    f32 = lambda a: np.ascontiguousarray(np.asarray(a, dtype=np.float32))
    x = f32(x); c = f32(c); ctx = f32(ctx); c_ctx = f32(c_ctx)
    if "nc" not in _NC_CACHE:
        _NC_CACHE["nc"] = build_nc()
    nc = _NC_CACHE["nc"]
    consts = _consts()
    shared = {
        "consts": consts, "mod_w": f32(mod_w), "mlp_w1": f32(mlp_w1), "mlp_w2": f32(mlp_w2),
        "conv_w_in": f32(conv_w_in[0]), "conv_w_out": f32(conv_w_out[0]),
        "ml_w_qkvo": f32(ml_w_qkvo[0]), "ml_w_out": f32(ml_w_out[0]),
    }
    in_maps = []
    for b in range(NCORES):
        m = dict(shared)
        m["x"] = x[b]
        m["ctx"] = ctx[b]
        m["small"] = _small(c[b], c_ctx, f32(norm_w), f32(mod_b), f32(conv_w), f32(ml_norm_w), f32(ml_b_if), f32(ml_w_if))
        in_maps.append(m)
    res = run_bass_kernel_spmd(nc, in_maps, core_ids=list(range(NCORES)))
    _NC_CACHE["last"] = res
    return np.stack([np.asarray(r["out"], dtype=np.float32) for r in res.results], axis=0)
```

```python
import contextlib
import numpy as np
import concourse.bass as bass
import concourse.mybir as mybir
from concourse.bass_utils import run_bass_kernel_spmd

F32 = mybir.dt.float32
BF16 = mybir.dt.bfloat16
AF = mybir.ActivationFunctionType
ALU = mybir.AluOpType
AX = mybir.AxisListType

D = 1024
FC = 8
SEQ = 4096
CTX = 256
TT = 512
NT = SEQ // TT
DFF = 4096
NH = 8
DQK = 64
DV = 128
VS = 130
EPS = 1e-6
DEBUG = False
STOP = None
NCORES = 8
GG_ENG = "dve"
NCX_RUN = 16


class _Stop(Exception):
    pass

SEM_EPOCH = 12000
N_DMA_SEMS = 10


class Op:
    __slots__ = ("eng", "fn", "reads", "writes", "dma", "deps", "sig", "signo", "dsem", "dval")

    def __init__(self, eng, fn, reads, writes, dma):
        self.eng = eng
        self.fn = fn
        self.reads = reads
        self.writes = writes
        self.dma = dma
        self.deps = None
        self.sig = False
        self.signo = 0
        self.dsem = None
        self.dval = 0


class Prog:
    ENGS = ("pe", "act", "dve", "pool", "sp")

    def __init__(self, nc):
        self.nc = nc
        self.ops = []

    def op(self, eng, fn, reads=(), writes=()):
        self.ops.append(Op(eng, fn, tuple(reads), tuple(writes), False))

    def dma(self, eng, fn, reads=(), writes=()):
        self.ops.append(Op(eng, fn, tuple(reads), tuple(writes), True))

    def emit(self):
        nc = self.nc
        ops = self.ops
        last_w = {}
        readers = {}
        for i, o in enumerate(ops):
            deps = {}
            for b in o.reads:
                j = last_w.get(b)
                if j is not None:
                    deps[j] = True
                if b[0] == "ps":
                    for j in readers.get(b, ()):
                        if ops[j].eng != o.eng:
                            deps.setdefault(j, False)
            for b in o.writes:
                j = last_w.get(b)
                if j is not None:
                    deps.setdefault(j, False)
                for j in readers.get(b, ()):
                    deps.setdefault(j, False)
            deps.pop(i, None)
            o.deps = deps
            for b in o.reads:
                readers.setdefault(b, []).append(i)
            for b in o.writes:
                last_w[b] = i
                readers[b] = []
        for o in ops:
            for j, raw in o.deps.items():
                p = ops[j]
                if p.dma:
                    continue
                if o.dma or p.eng != o.eng:
                    p.sig = True
                elif raw and o.eng in ("act", "dve", "pool"):
                    p.sig = True
        cnt = {e: 0 for e in self.ENGS}
        for o in ops:
            if o.sig and not o.dma:
                cnt[o.eng] += 1
                o.signo = cnt[o.eng]
        engobj = {"pe": nc.tensor, "act": nc.scalar, "dve": nc.vector, "pool": nc.gpsimd, "sp": nc.sync}
        with contextlib.ExitStack() as st:
            esems = {}
            for e in self.ENGS:
                n_ep = (cnt[e] + SEM_EPOCH - 1) // SEM_EPOCH
                esems[e] = [st.enter_context(nc.semaphore(f"s_{e}_{k}")) for k in range(n_ep)]
            dsems = {}
            for e in ("sp", "pool", "act"):
                if any(o.dma and o.eng == e for o in ops):
                    dsems[e] = [st.enter_context(nc.semaphore(f"d_{e}_{k}")) for k in range(N_DMA_SEMS)]
            duse = {e: [0] * N_DMA_SEMS for e in dsems}
            dlast = {e: [None] * N_DMA_SEMS for e in dsems}
            dnext = {e: 0 for e in dsems}
            seen = {e: {} for e in self.ENGS}

            def wait(e, sem, key, val):
                s = seen[e]
                if s.get(key, 0) >= val:
                    return
                s[key] = val
                engobj[e].wait_ge(sem, val)

            def wait_op(e, p):
                if p.dma:
                    wait(e, p.dsem, ("d", id(p.dsem)), p.dval)
                else:
                    k = (p.signo - 1) // SEM_EPOCH
                    v = (p.signo - 1) % SEM_EPOCH + 1
                    wait(e, esems[p.eng][k], (p.eng, k), v)

            for o in ops:
                e = o.eng
                for j, raw in o.deps.items():
                    p = ops[j]
                    if p.dma or o.dma or p.eng != e:
                        wait_op(e, p)
                    elif raw and e in ("act", "dve", "pool"):
                        wait_op(e, p)
                if o.dma:
                    k = dnext[e]
                    dnext[e] = (k + 1) % N_DMA_SEMS
                    prev = dlast[e][k]
                    if prev is not None:
                        wait_op(e, prev)
                    duse[e][k] += 1
                    o.dsem = dsems[e][k]
                    o.dval = 16 * duse[e][k]
                    dlast[e][k] = o
                    ins = o.fn(engobj[e])
                    ins.then_inc(o.dsem, 16)
                else:
                    ins = o.fn(engobj[e])
                    if o.sig:
                        k = (o.signo - 1) // SEM_EPOCH
                        ins.then_inc(esems[e][k], 1)
            for e in dsems:
                for k in range(N_DMA_SEMS):
                    p = dlast[e][k]
                    if p is not None:
                        wait_op(e, p)


C_IDENT, C_MINC0, C_MINC1, C_MSTR0, C_MSTR1, C_NEG1, C_NBIG0, C_NBIG1, C_ONES = range(9)
NBIG = -30000.0


def _consts():
    u = np.arange(128)[:, None]
    t = np.arange(128)[None, :]
    mats = [
        (u == t).astype(np.float32),
        -(u <= t).astype(np.float32),
        -(u >= t).astype(np.float32),
        -(u > t).astype(np.float32),
        -(u < t).astype(np.float32),
        -np.ones((128, 128), np.float32),
        NBIG * (t < u).astype(np.float32),
        NBIG * (t > u).astype(np.float32),
        np.ones((128, 128), np.float32),
    ]
    return np.ascontiguousarray(np.concatenate(mats, axis=1))


S_CC = 0
S_NW = 16
S_MB = 80
S_CW = 176
S_MLNW = 200
S_BIF = 208
S_WIF = 240
S_MISC = 496
NSMALL = 500


def _fm(v):
    v = np.asarray(v, np.float32)
    lead = v.shape[:-1]
    a = v.reshape(lead + (FC, 128))
    a = np.moveaxis(a, -1, 0)
    return a


def _small(c_b, c_ctx, norm_w, mod_b, conv_w, ml_norm_w, ml_b_if, ml_w_if):
    s = np.zeros((128, NSMALL), np.float32)
    cc = np.stack([_fm(c_b), _fm(c_ctx)], axis=-1)
    s[:, S_CC:S_CC + 16] = cc.reshape(128, 16)
    s[:, S_NW:S_NW + 64] = _fm(norm_w).reshape(128, 64)
    mb = np.asarray(mod_b, np.float32).reshape(2, 48, 128)
    s[:, S_MB:S_MB + 96] = np.moveaxis(mb, -1, 0).reshape(128, 96)
    s[:, S_CW:S_CW + 24] = _fm(conv_w[0]).reshape(128, 24)
    s[:, S_MLNW:S_MLNW + 8] = _fm(ml_norm_w[0]).reshape(128, 8)
    s[:, S_BIF:S_BIF + 32] = np.asarray(ml_b_if[0], np.float32).reshape(1, 32)
    wif = np.concatenate([ml_w_if[0, 0], ml_w_if[0, 1]], axis=1)
    wif = wif.reshape(FC, 128, 32).transpose(1, 0, 2)
    s[:, S_WIF:S_WIF + 256] = wif.reshape(128, 256)
    s[:, S_MISC + 0] = 1.0
    s[:, S_MISC + 1] = EPS
    return s


CT = 256
NCX = SEQ // CT
NCH = CT // 128
SKEW = 40


class _BankAlloc:
    def __init__(self):
        self.free = [0, 1, 2, 3]

    def try_acq(self, n):
        if len(self.free) < n:
            return None
        r = self.free[:n]
        self.free = self.free[n:]
        return r

    def rel(self, pairs):
        self.free = self.free + list(pairs)


def build_nc():
    sched = _build(None)
    return _build(sched)


def _build(sched_in):
    dry = sched_in is None
    nc = bass.Bass("TRN2", target_bir_lowering=False)
    dt_in = lambda n, shp: nc.dram_tensor(n, list(shp), F32, kind="ExternalInput").ap()
    x_d = dt_in("x", [SEQ, D])
    ctx_d = dt_in("ctx", [CTX, D])
    small_d = dt_in("small", [128, NSMALL])
    consts_d = dt_in("consts", [128, 9 * 128])
    mod_w_d = dt_in("mod_w", [2, D, 6 * D])
    mlp_w1_d = dt_in("mlp_w1", [2, D, DFF])
    mlp_w2_d = dt_in("mlp_w2", [2, DFF, D])
    conv_w_in_d = dt_in("conv_w_in", [D, 3 * D])
    conv_w_out_d = dt_in("conv_w_out", [D, D])
    ml_w_qkvo_d = dt_in("ml_w_qkvo", [D, 3 * D])
    ml_w_out_d = dt_in("ml_w_out", [D, D])
    out_d = nc.dram_tensor("out", [SEQ, D], F32, kind="ExternalOutput").ap()

    def scratch(n, shp, dt):
        return nc.dram_tensor(n, list(shp), dt).ap()

    wsrc = {
        "win0": conv_w_in_d, "wout0": conv_w_out_d, "w1_0": mlp_w1_d[0], "w2_0": mlp_w2_d[0],
        "qkvo": ml_w_qkvo_d, "mlout": ml_w_out_d, "w1_1": mlp_w1_d[1], "w2_1": mlp_w2_d[1],
    }
    wsc = {n: scratch("sc_" + n, a.shape, BF16) for n, a in wsrc.items()}
    x1_s = scratch("x1_s", [NCX, 128, FC * CT], F32)
    qt_s = scratch("qt_s", [NCX, 64, NH * CT], BF16)
    kt_s = scratch("kt_s", [NCX, 64, NH * CT], BF16)
    ktok_s = scratch("ktok_s", [NCX, 128, NCH * 512], BF16)
    vaug_s = scratch("vaug_s", [NCX, 128, NCH * NH * VS], BF16)
    so_s = scratch("so_s", [NCX, 128, NH * CT], BF16)
    hf_s = scratch("hf_s", [NCX, 128, NCH * D], F32)
    dbg_d = None
    if DEBUG:
        dbg_d = nc.dram_tensor("dbg", [NCX, 128, FC * CT], F32, kind="ExternalOutput").ap()

    with contextlib.ExitStack() as st:
        def sb(name, shape, dt):
            return st.enter_context(nc.sbuf_tensor(name, list(shape), dt))

        P = Prog(nc)
        ps = [st.enter_context(nc.psum_tensor(f"ps{i}", [128, 512], F32)) for i in range(8)]
        banks = _BankAlloc()

        SM = sb("SM", [128, NSMALL], F32)
        CST = sb("CST", [128, 9 * 128], F32)
        ONESB = sb("ONESB", [128, 128], BF16)
        WIFB = sb("WIFB", [128, FC, 32], BF16)
        SIL = sb("SIL", [128, FC, 2], F32)
        MOD = sb("MOD", [128, 2, 2, 48], F32)
        PAR = sb("PAR", [128, 2, 2, 4, 8], F32)
        GT = sb("GT", [128, 2 * NCX + 2, 32], F32)
        WRING = [sb(f"WR{i}", [128, FC, 512], BF16) for i in range(4)]
        LG = sb("LG", [128, 2 * NCX + 2, 32], F32)
        G3S = sb("G3S", [128, 40], F32)
        LFB = sb("LFB", [128, NH, 128], F32)
        DTT = sb("DTT", [128, 2, 128], F32)
        PTT = sb("PTT", [128, 2, 128], BF16)
        KW = sb("KW", [128, NH, 64], BF16)
        T0 = sb("T0", [128, 2, VS], F32)
        T1 = sb("T1", [128, NH, VS], F32)
        RD = sb("RD", [128, 16], F32)
        CST_C = [sb(f"CS{d}", [64, NH, VS], F32) for d in range(2)]
        CST_B = [sb(f"CB{d}", [64, NH, VS], BF16) for d in range(2)]
        HN = sb("HN", [128, 2, 128], F32)
        SSQ = sb("SSQ", [128, 16], F32)

        class Cx:
            def __init__(self, i):
                self.i = i
                self.F0 = sb(f"F0_{i}", [128, FC, CT], F32)
                self.F1 = sb(f"F1_{i}", [128, FC, CT], F32)
                self.F2 = sb(f"F2_{i}", [128, FC, CT], F32)
                self.B0 = sb(f"B0_{i}", [128, FC, CT], BF16)
                self.B1 = sb(f"B1_{i}", [128, FC, CT], BF16)
                self.HID = sb(f"HID_{i}", [128, 32, CT], BF16)
                self.RS = sb(f"RS_{i}", [128, CT], F32)
                self.TMPA = sb(f"TMPA_{i}", [128, 2, CT], F32)
                self.KTOK = sb(f"KTOK_{i}", [128, NCH, 512], BF16)
                self.VAUG = sb(f"VAUG_{i}", [128, NCH, NH, VS], BF16)
                self.SSQ = sb(f"SSQ_{i}", [128, 16], F32)
                self.HN = sb(f"HN_{i}", [128, 2, 128], F32)

            def k(self, name, idx=None):
                return (name + str(self.i), idx)

            def ks(self, name, n):
                return [(name + str(self.i), j) for j in range(n)]

        CXS = [Cx(0), Cx(1)]
        MODBUF = [sb(f"MODBUF{i}", [128, FC, CT], F32) for i in range(2)]

        IDENT = CST[:, C_IDENT * 128:(C_IDENT + 1) * 128]
        MINC = [CST[:, C_MINC0 * 128:(C_MINC0 + 1) * 128], CST[:, C_MINC1 * 128:(C_MINC1 + 1) * 128]]
        MSTR = [CST[:, C_MSTR0 * 128:(C_MSTR0 + 1) * 128], CST[:, C_MSTR1 * 128:(C_MSTR1 + 1) * 128]]
        NEG1 = CST[:, C_NEG1 * 128:(C_NEG1 + 1) * 128]
        NBIGM = [CST[:, C_NBIG0 * 128:(C_NBIG0 + 1) * 128], CST[:, C_NBIG1 * 128:(C_NBIG1 + 1) * 128]]
        ONES32 = CST[:, C_ONES * 128:(C_ONES + 1) * 128]
        ONE_COL = SM[:, S_MISC:S_MISC + 1]
        EPS_COL = SM[:, S_MISC + 1:S_MISC + 2]

        def psk(b):
            return [("ps", b)]

        def acq(n=1):
            while True:
                r = banks.try_acq(n)
                if r is not None:
                    return r
                yield "blocked"

        def reg(pair, mi, ntok):
            b = pair * 2 + mi // 2
            c0 = (mi % 2) * 256
            return b, c0

        P.dma("sp", lambda e: e.dma_start(out=SM[:], in_=small_d), writes=["SM"])
        P.dma("sp", lambda e: e.dma_start(out=CST[:], in_=consts_d), writes=["CST"])
        P.op("act", lambda e: e.activation(out=ONESB[:], in_=ONES32, func=AF.Copy), reads=["CST"], writes=["ONESB"])
        P.op("act", lambda e: e.activation(
            out=WIFB[:], in_=SM[:, S_WIF:S_WIF + 256].rearrange("p (k j) -> p k j", k=FC), func=AF.Copy),
            reads=["SM"], writes=["WIFB"])
        P.op("act", lambda e: e.activation(
            out=SIL[:], in_=SM[:, S_CC:S_CC + 16].rearrange("p (k j) -> p k j", k=FC), func=AF.Silu),
            reads=["SM"], writes=["SIL"])
        for d in range(2):
            P.op("pool", lambda e, d=d: e.memset(CST_C[d][:].rearrange("p a b -> p (a b)"), 0.0),
                 writes=[("CS", d, h) for h in range(NH)])
            P.op("pool", lambda e, d=d: e.memset(CST_B[d][:].rearrange("p a b -> p (a b)"), 0.0),
                 writes=[("CB", d, h) for h in range(NH)])
        for cx in CXS:
            P.op("pool", lambda e, cx=cx: e.memset(cx.VAUG[:].rearrange("p a b c -> p (a b c)"), 1.0),
                 writes=cx.ks("VAUG", NCH))

        PIECE = 1 << 20
        wpieces = {}
        for n, src in wsrc.items():
            R, C = src.shape
            npc = 1
            while R * C // npc > PIECE:
                npc *= 2
            rp = R // npc
            keys = []
            for r0 in range(0, R, rp):
                key = ("wsc", n, r0)
                keys.append(key)
                P.dma("pool", lambda e, n=n, src=src, r0=r0, rp=rp: e.dma_start(
                    out=wsc[n][r0:r0 + rp, :], in_=src[r0:r0 + rp, :]), writes=[key])
            wpieces[n] = keys

        flags = {}

        def mod_layer(l):
            pair = (yield from acq(1))[0]
            pb = pair * 2
            def dma_blk(nb):
                P.dma("sp", lambda e, l=l, nb=nb: e.dma_start(
                    out=MODBUF[nb % 2][:], in_=mod_w_d[l, :, nb * 256:(nb + 1) * 256].rearrange("(k p) n -> p k n", p=128)),
                    writes=[("MODBUF", nb % 2)])
            dma_blk(0)
            for nb in range(24):
                buf = MODBUF[nb % 2]
                bkey = [("MODBUF", nb % 2)]
                if nb + 1 < 24:
                    dma_blk(nb + 1)

                def mm_mod(e, nb=nb, buf=buf):
                    ins = None
                    for mi in range(2):
                        col = (nb * 2 + mi) * 2
                        for kc in range(FC):
                            ins = e.matmul(ps[pb][:, col:col + 2], lhsT=buf[:, kc, mi * 128:(mi + 1) * 128],
                                           rhs=SIL[:, kc, :], start=(kc == 0), stop=(kc == FC - 1))
                    return ins
                P.op("pe", mm_mod, reads=bkey + ["SIL"], writes=psk(pb))
                yield (1.6, 25.0)
            for s in range(2):
                P.op("dve", lambda e, l=l, s=s: e.tensor_tensor(
                    out=MOD[:, l, s, :], in0=ps[pb][:, 0:96].rearrange("p (m s) -> p m s", s=2)[:, :, s],
                    in1=SM[:, S_MB + l * 48:S_MB + (l + 1) * 48], op=ALU.add),
                    reads=psk(pb) + ["SM"], writes=[("MOD", l, s)])
                nw = lambda j, l=l: SM[:, S_NW + (l * 4 + j) * 8:S_NW + (l * 4 + j) * 8 + 8]
                md = lambda j, l=l, s=s: MOD[:, l, s, j * 8:(j + 1) * 8]
                P.op("dve", lambda e, l=l, s=s, nw=nw, md=md: e.scalar_tensor_tensor(
                    out=PAR[:, l, s, 0, :], in0=md(1), scalar=1.0, in1=nw(0), op0=ALU.add, op1=ALU.mult),
                    reads=[("MOD", l, s), "SM"], writes=[("PAR", l, s, 0)])
                P.op("dve", lambda e, l=l, s=s, nw=nw, md=md: e.tensor_tensor(
                    out=PAR[:, l, s, 1, :], in0=md(2), in1=nw(1), op=ALU.mult),
                    reads=[("MOD", l, s), "SM"], writes=[("PAR", l, s, 1)])
                P.op("dve", lambda e, l=l, s=s, nw=nw, md=md: e.scalar_tensor_tensor(
                    out=PAR[:, l, s, 2, :], in0=md(4), scalar=1.0, in1=nw(2), op0=ALU.add, op1=ALU.mult),
                    reads=[("MOD", l, s), "SM"], writes=[("PAR", l, s, 2)])
                P.op("dve", lambda e, l=l, s=s, nw=nw, md=md: e.tensor_tensor(
                    out=PAR[:, l, s, 3, :], in0=md(5), in1=nw(3), op=ALU.mult),
                    reads=[("MOD", l, s), "SM"], writes=[("PAR", l, s, 3)])
            banks.rel([pair])
            flags[("mod", l)] = True

        for _r in mod_layer(0):
            pass

        def par(l, s, which):
            if which == "a1":
                return PAR[:, l, s, 0, :], [("PAR", l, s, 0)]
            if which == "g1":
                return PAR[:, l, s, 1, :], [("PAR", l, s, 1)]
            if which == "a2":
                return PAR[:, l, s, 2, :], [("PAR", l, s, 2)]
            if which == "g2":
                return PAR[:, l, s, 3, :], [("PAR", l, s, 3)]
            if which == "sh1":
                return MOD[:, l, s, 0:8], [("MOD", l, s)]
            if which == "sh2":
                return MOD[:, l, s, 24:32], [("MOD", l, s)]
            raise KeyError(which)

        sched = [] if dry else list(sched_in)
        wstate = {"issued": 0, "used": 0}
        NSLOT = len(WRING)

        def w_issue():
            n = wstate["issued"]
            name, r0, c0 = sched[n]
            slot = n % NSLOT
            P.dma("sp", lambda e, name=name, r0=r0, c0=c0, slot=slot: e.dma_start(
                out=WRING[slot][:], in_=wsc[name][r0:r0 + 1024, c0:c0 + 512].rearrange("(k p) n -> p k n", p=128)),
                reads=wpieces[name], writes=[("WR", slot)])
            wstate["issued"] = n + 1

        def w_next(expect):
            n = wstate["used"]
            if dry:
                sched.append(expect)
            else:
                assert sched[n] == expect, (n, sched[n], expect)
                while wstate["issued"] < min(len(sched), n + NSLOT):
                    w_issue()
            wstate["used"] = n + 1
            slot = n % NSLOT
            return WRING[slot], ("WR", slot)

        def fm_mm(cx, slot, wkey, src, skey, koff, pair, first, last, ncol=4, msize=128, col0=0):
            for mi in range(ncol):
                b, c0 = reg(pair, mi, CT)

                def f(e, mi=mi, b=b, c0=c0):
                    ins = None
                    for kc in range(FC):
                        ins = e.matmul(ps[b][0:msize, c0:c0 + CT],
                                       lhsT=slot[:, kc, col0 + mi * msize:col0 + (mi + 1) * msize],
                                       rhs=src[:, koff + kc, 0:CT],
                                       start=(first and kc == 0 and mi % 2 == 0), stop=(last and kc == FC - 1),
                                       skip_group_check=True)
                    return ins
                P.op("pe", f, reads=[wkey] + [cx.k(skey, koff + kc) for kc in range(FC)], writes=psk(b))

        def rstd_from_sq(cx):
            pair = (yield from acq(1))[0]
            b = pair * 2

            def f(e):
                ins = None
                for fc in range(FC):
                    ins = e.matmul(ps[b][:, 0:CT], lhsT=ONESB[:], rhs=cx.B1[:, fc, :],
                                   start=(fc == 0), stop=(fc == FC - 1))
                return ins
            P.op("pe", f, reads=["ONESB"] + cx.ks("B1", FC), writes=psk(b))
            yield (1.0, 1.0)
            P.op("act", lambda e: e.activation(out=cx.RS[:], in_=ps[b][:, 0:CT], func=AF.Sqrt, bias=EPS_COL, scale=1.0 / D),
                 reads=psk(b) + ["SM"], writes=[cx.k("RS")])
            P.op("dve", lambda e: e.reciprocal(out=cx.RS[:], in_=cx.RS[:]), reads=[cx.k("RS")], writes=[cx.k("RS")])
            banks.rel([pair])

        def norm_mod(cx, l, s, which_a, which_sh):
            a_ap, a_k = par(l, s, which_a)
            sh_ap, sh_k = par(l, s, which_sh)
            for fc in range(FC):
                eng = ("act", "pool", "dve")[fc % 3]
                if eng == "act":
                    P.op("act", lambda e, fc=fc: e.activation(out=cx.B1[:, fc, :], in_=cx.F0[:, fc, :], func=AF.Square),
                         reads=[cx.k("F0", fc)], writes=[cx.k("B1", fc)])
                else:
                    P.op(eng, lambda e, fc=fc: e.tensor_tensor(out=cx.B1[:, fc, :], in0=cx.F0[:, fc, :], in1=cx.F0[:, fc, :],
                                                               op=ALU.mult),
                         reads=[cx.k("F0", fc)], writes=[cx.k("B1", fc)])
            yield (0.0, 8.0)
            yield from rstd_from_sq(cx)
            for fc in range(FC):
                P.op("dve", lambda e, fc=fc: e.tensor_tensor(out=cx.TMPA[:, fc % 2, :], in0=cx.F0[:, fc, :],
                                                             in1=cx.RS[:], op=ALU.mult),
                     reads=[cx.k("F0", fc), cx.k("RS")], writes=[cx.k("TMPA", fc % 2)])
                P.op("act", lambda e, fc=fc: e.activation(out=cx.B0[:, fc, :], in_=cx.TMPA[:, fc % 2, :],
                                                          func=AF.Identity, bias=sh_ap[:, fc:fc + 1],
                                                          scale=a_ap[:, fc:fc + 1]),
                     reads=[cx.k("TMPA", fc % 2)] + a_k + sh_k, writes=[cx.k("B0", fc)])
            yield (0.0, 12.0)

        def evac_branch(cx, pair, fc0):
            for mi in range(4):
                fc = fc0 + mi
                b, c0 = reg(pair, mi, CT)
                if mi % 2 == 0:
                    P.op("dve", lambda e, fc=fc, b=b, c0=c0: e.tensor_copy(out=cx.F1[:, fc, :], in_=ps[b][:, c0:c0 + CT]),
                         reads=psk(b), writes=[cx.k("F1", fc)])
                else:
                    P.op("act", lambda e, fc=fc, b=b, c0=c0: e.activation(out=cx.F1[:, fc, :], in_=ps[b][:, c0:c0 + CT], func=AF.Copy),
                         reads=psk(b), writes=[cx.k("F1", fc)])
                P.op("pool", lambda e, fc=fc: e.tensor_tensor(out=cx.B1[:, fc, :], in0=cx.F1[:, fc, :], in1=cx.F1[:, fc, :],
                                                              op=ALU.mult),
                     reads=[cx.k("F1", fc)], writes=[cx.k("B1", fc)])

        def residual(cx, l, s, which_g):
            g_ap, g_k = par(l, s, which_g)
            yield from rstd_from_sq(cx)
            for fc in range(FC):
                P.op("dve", lambda e, fc=fc: e.scalar_tensor_tensor(
                    out=cx.TMPA[:, fc % 2, :], in0=cx.F1[:, fc, :], scalar=g_ap[:, fc:fc + 1],
                    in1=cx.RS[:], op0=ALU.mult, op1=ALU.mult),
                    reads=[cx.k("F1", fc), cx.k("RS")] + g_k, writes=[cx.k("TMPA", fc % 2)])
                P.op("pool", lambda e, fc=fc: e.tensor_tensor(out=cx.F0[:, fc, :], in0=cx.F0[:, fc, :],
                                                              in1=cx.TMPA[:, fc % 2, :], op=ALU.add),
                     reads=[cx.k("F0", fc), cx.k("TMPA", fc % 2)], writes=[cx.k("F0", fc)])
            yield (0.0, 12.0)

        def mlp(cx, l, s):
            yield from norm_mod(cx, l, s, "a2", "sh2")
            w1n, w2n = f"w1_{l}", f"w2_{l}"
            for j in range(8):
                pair = (yield from acq(1))[0]
                slot, wkey = w_next((w1n, 0, 512 * j))
                fm_mm(cx, slot, wkey, cx.B0, "B0", 0, pair, True, True)
                for mi in range(4):
                    hc = 4 * j + mi
                    b, c0 = reg(pair, mi, CT)
                    P.op("act", lambda e, b=b, c0=c0, hc=hc: e.activation(out=cx.TMPA[:, hc % 2, :], in_=ps[b][:, c0:c0 + CT],
                                                                          func=AF.Relu),
                         reads=psk(b), writes=[cx.k("TMPA", hc % 2)])
                    eng = "dve" if (hc % 2 == 0) else "pool"
                    P.op(eng, lambda e, hc=hc: e.tensor_tensor(out=cx.HID[:, hc, :], in0=cx.TMPA[:, hc % 2, :],
                                                               in1=cx.TMPA[:, hc % 2, :], op=ALU.mult),
                         reads=[cx.k("TMPA", hc % 2)], writes=[cx.k("HID", hc)])
                banks.rel([pair])
                yield (3.7, 1.5)
            for ch in range(2):
                pair = (yield from acq(1))[0]
                for kg in range(4):
                    slot, wkey = w_next((w2n, 1024 * kg, 512 * ch))
                    fm_mm(cx, slot, wkey, cx.HID, "HID", 8 * kg, pair, kg == 0, kg == 3)
                    if kg < 3:
                        yield (3.7, 0.0)
                evac_branch(cx, pair, 4 * ch)
                banks.rel([pair])
                yield (3.7, 4.0)
            yield from residual(cx, l, s, "g2")

        def load_tokens(cx, src_rows):
            xtok = cx.F2[:].rearrange("p a b -> p (a b)").rearrange("p (t f) -> p t f", f=D)
            P.dma("sp", lambda e: e.dma_start(out=xtok, in_=src_rows.rearrange("(t p) f -> p t f", p=128)),
                  writes=cx.ks("F2", FC))
            yield (0.0, 12.0)
            for g in range(2):
                pair = (yield from acq(1))[0]
                for q in range(4):
                    fc = g * 4 + q
                    b, c0 = reg(pair, q, CT)

                    def f(e, fc=fc, b=b, c0=c0):
                        ins = None
                        for tt in range(NCH):
                            ins = e.transpose(out=ps[b][:, c0 + tt * 128:c0 + (tt + 1) * 128],
                                              in_=xtok[:, tt, fc * 128:(fc + 1) * 128], identity=IDENT)
                        return ins
                    P.op("pe", f, reads=cx.ks("F2", FC) + ["CST"], writes=psk(b))
                    if fc % 2 == 0:
                        P.op("act", lambda e, fc=fc, b=b, c0=c0: e.activation(out=cx.F0[:, fc, :], in_=ps[b][:, c0:c0 + CT], func=AF.Copy),
                             reads=psk(b), writes=[cx.k("F0", fc)])
                    else:
                        P.op("dve", lambda e, fc=fc, b=b, c0=c0: e.tensor_copy(out=cx.F0[:, fc, :], in_=ps[b][:, c0:c0 + CT]),
                             reads=psk(b), writes=[cx.k("F0", fc)])
                banks.rel([pair])
                yield (1.8, 1.5)

        def layer0(cx, s, rowlen):
            yield from norm_mod(cx, 0, s, "a1", "sh1")
            BG = lambda fc: cx.HID[:, fc, :]
            CG = lambda fc: cx.HID[:, 8 + fc, :]
            GG = lambda fc: cx.HID[:, 16 + fc, :]
            for j in range(6):
                pair = (yield from acq(1))[0]
                slot, wkey = w_next(("win0", 0, 512 * j))
                fm_mm(cx, slot, wkey, cx.B0, "B0", 0, pair, True, True)
                for mi in range(4):
                    b, c0 = reg(pair, mi, CT)
                    m = 4 * j + mi
                    src = ps[b][:, c0:c0 + CT]
                    if m < 8:
                        P.op("act", lambda e, m=m, src=src: e.activation(out=BG(m), in_=src, func=AF.Copy),
                             reads=psk(b), writes=[cx.k("HID", m)])
                    elif m < 16:
                        P.op("act", lambda e, m=m, src=src: e.activation(out=CG(m - 8), in_=src, func=AF.Copy),
                             reads=psk(b), writes=[cx.k("HID", m)])
                    else:
                        fc = m - 16
                        P.op("dve", lambda e, fc=fc, src=src: e.tensor_tensor(out=cx.F2[:, fc, :], in0=src, in1=CG(fc), op=ALU.mult),
                             reads=psk(b) + [cx.k("HID", 8 + fc)], writes=[cx.k("F2", fc)])
                banks.rel([pair])
                yield (3.7, 2.0)
            cw = lambda k, fc: SM[:, S_CW + k * 8 + fc:S_CW + k * 8 + fc + 1]
            for fc in range(FC):
                yv = cx.TMPA[:, fc % 2, :]
                y3 = yv.rearrange("p (r w) -> p r w", w=rowlen)
                u3 = cx.F2[:, fc, :].rearrange("p (r w) -> p r w", w=rowlen)
                P.op("act", lambda e, fc=fc, yv=yv: e.activation(out=yv, in_=cx.F2[:, fc, :], func=AF.Identity,
                                                                 bias=0.0, scale=cw(1, fc)),
                     reads=[cx.k("F2", fc), "SM"], writes=[cx.k("TMPA", fc % 2)])
                P.op("dve", lambda e, fc=fc, y3=y3, u3=u3: e.scalar_tensor_tensor(
                    out=y3[:, :, 1:rowlen], in0=u3[:, :, 0:rowlen - 1], scalar=cw(0, fc), in1=y3[:, :, 1:rowlen],
                    op0=ALU.mult, op1=ALU.add),
                    reads=[cx.k("F2", fc), cx.k("TMPA", fc % 2), "SM"], writes=[cx.k("TMPA", fc % 2)])
                P.op("dve", lambda e, fc=fc, y3=y3, u3=u3: e.scalar_tensor_tensor(
                    out=y3[:, :, 0:rowlen - 1], in0=u3[:, :, 1:rowlen], scalar=cw(2, fc), in1=y3[:, :, 0:rowlen - 1],
                    op0=ALU.mult, op1=ALU.add),
                    reads=[cx.k("F2", fc), cx.k("TMPA", fc % 2), "SM"], writes=[cx.k("TMPA", fc % 2)])
                P.op("pool", lambda e, fc=fc, yv=yv: e.tensor_tensor(out=GG(fc), in0=yv, in1=BG(fc), op=ALU.mult),
                     reads=[cx.k("TMPA", fc % 2), cx.k("HID", fc)], writes=[cx.k("HID", 16 + fc)])
                if fc % 4 == 3:
                    yield (0.0, 6.0)
            for j in range(2):
                pair = (yield from acq(1))[0]
                slot, wkey = w_next(("wout0", 0, 512 * j))
                fm_mm(cx, slot, wkey, cx.HID, "HID", 16, pair, True, True)
                evac_branch(cx, pair, 4 * j)
                banks.rel([pair])
                yield (3.7, 4.0)
            yield from residual(cx, 0, s, "g1")
            yield from mlp(cx, 0, s)

        def gates_mm(cx, gi0):
            pair = (yield from acq(1))[0]
            bank = pair * 2

            def f(e):
                ins = None
                for c in range(NCH):
                    for kc in range(FC):
                        ins = e.matmul(ps[bank][:, c * 32:(c + 1) * 32], lhsT=cx.B0[:, kc, c * 128:(c + 1) * 128],
                                       rhs=WIFB[:, kc, :], start=(kc == 0), stop=(kc == FC - 1))
                return ins
            P.op("pe", f, reads=["WIFB"] + cx.ks("B0", FC), writes=psk(bank))
            for c in range(NCH):
                P.op("dve", lambda e, c=c: e.tensor_tensor(out=GT[:, gi0 + c, :], in0=ps[bank][:, c * 32:(c + 1) * 32],
                                                           in1=SM[:, S_BIF:S_BIF + 32], op=ALU.add),
                     reads=psk(bank) + ["SM"], writes=[("GT", gi0 + c)])
            banks.rel([pair])
            gks = [("GT", gi0 + c) for c in range(NCH)]
            lks = [("LG", gi0 + c) for c in range(NCH)]
            P.op("act", lambda e: e.activation(out=LG[:, gi0:gi0 + NCH, :], in_=GT[:, gi0:gi0 + NCH, :], func=AF.Exp, scale=-1.0),
                 reads=gks, writes=lks)
            P.op("act", lambda e: e.activation(out=LG[:, gi0:gi0 + NCH, :], in_=LG[:, gi0:gi0 + NCH, :], func=AF.Ln,
                                               bias=ONE_COL, scale=1.0),
                 reads=lks + ["SM"], writes=lks)
            yield (1.0, 3.0)

        def scan_chunk(cx, d, gi, c, with_out, first_dir=True):
            pairs = yield from acq(2)
            bY = [pairs[0] * 2, pairs[0] * 2 + 1]
            bXs = [pairs[1] * 2, pairs[1] * 2 + 1]
            gb = pairs[1] * 2 + 1
            QT = cx.HID[:, 0:8, :]
            KT = cx.HID[:, 8:16, :]
            igs = GT[:, gi, d * 16:d * 16 + 8]
            fps = GT[:, gi, d * 16 + 8:d * 16 + 16]
            gk = [("GT", gi)]
            lfn = LG[:, gi, d * 16 + 8:d * 16 + 16]

            def g3(e):
                e.matmul(ps[gb][:, 0:8], lhsT=MINC[d], rhs=lfn, start=True, stop=True)
                e.matmul(ps[gb][:, 8:16], lhsT=MSTR[d], rhs=lfn, start=True, stop=True)
                return e.matmul(ps[gb][:, 16:24], lhsT=NEG1, rhs=lfn, start=True, stop=True)
            P.op("pe", g3, reads=[("LG", gi), "CST"], writes=psk(gb))
            C1 = G3S[:, 0:8]
            EB = G3S[:, 8:16]
            WKP = G3S[:, 16:24]
            WK = G3S[:, 24:32]
            AT = G3S[:, 32:40]
            if with_out:
                for h in range(NH):
                    if h % 2 == 0:
                        P.op("dve", lambda e, h=h: e.tensor_scalar(out=LFB[:, h, :], in0=ONES32, scalar1=LG[:, gi, d * 16 + 8 + h:d * 16 + 9 + h],
                                                                   scalar2=None, op0=ALU.mult),
                             reads=[("LG", gi), "CST"], writes=[("LFB", h)])
                    else:
                        P.op("act", lambda e, h=h: e.activation(out=LFB[:, h, :], in_=ONES32, func=AF.Identity, bias=0.0,
                                                                scale=LG[:, gi, d * 16 + 8 + h:d * 16 + 9 + h]),
                             reads=[("LG", gi), "CST"], writes=[("LFB", h)])
            yield (0.3, 6.0)
            if with_out:
                P.op("dve", lambda e: e.tensor_tensor(out=C1, in0=igs, in1=ps[gb][:, 0:8], op=ALU.subtract),
                     reads=gk + psk(gb), writes=[("G3S", 0)])
            P.op("dve", lambda e: e.tensor_tensor(out=WKP, in0=igs, in1=ps[gb][:, 8:16], op=ALU.add),
                 reads=gk + psk(gb), writes=[("G3S", 2)])
            if with_out:
                P.op("act", lambda e: e.activation(out=EB, in_=ps[gb][:, 0:8], func=AF.Exp),
                     reads=psk(gb), writes=[("G3S", 1)])
            P.op("act", lambda e: e.activation(out=AT, in_=ps[gb][:, 16:24], func=AF.Exp),
                 reads=psk(gb), writes=[("G3S", 4)])
            P.op("act", lambda e: e.activation(out=WK, in_=WKP, func=AF.Exp), reads=[("G3S", 2)], writes=[("G3S", 3)])
            for h in range(NH):
                if h % 2 == 0:
                    P.op("dve", lambda e, h=h: e.tensor_scalar(out=KW[:, h, :], in0=cx.KTOK[:, c, h * 64:(h + 1) * 64],
                                                               scalar1=G3S[:, 24 + h:25 + h], scalar2=None, op0=ALU.mult),
                         reads=[cx.k("KTOK", c), ("G3S", 3)], writes=[("KW", h)])
                else:
                    P.op("act", lambda e, h=h: e.activation(out=KW[:, h, :], in_=cx.KTOK[:, c, h * 64:(h + 1) * 64],
                                                            func=AF.Identity, bias=0.0, scale=G3S[:, 24 + h:25 + h]),
                         reads=[cx.k("KTOK", c), ("G3S", 3)], writes=[("KW", h)])
            yield (0.0, 10.0)
            cs = slice(c * 128, (c + 1) * 128)
            vk = cx.k("VAUG", c)

            def head_front(h):
                by = bY[h % 2]
                rot = h % 2

                def bbm(e):
                    e.matmul(ps[by][:, 0:128], lhsT=LFB[:, h, :], rhs=MINC[d], start=True, stop=False)
                    e.matmul(ps[by][:, 0:128], lhsT=IDENT, rhs=NBIGM[d], start=False, stop=True)
                    return e.matmul(ps[by][:, 128:256], lhsT=KT[0:64, h, cs], rhs=QT[0:64, h, cs], start=True, stop=True)
                P.op("pe", bbm, reads=[("LFB", h), "CST", cx.k("HID", 8 + h), cx.k("HID", h)], writes=psk(by))
                P.op("act", lambda e: e.activation(out=DTT[:, rot, :], in_=ps[by][:, 0:128], func=AF.Exp,
                                                   bias=G3S[:, h:h + 1], scale=1.0),
                     reads=psk(by) + [("G3S", 0)], writes=[("DTT", rot)])
                P.op("dve", lambda e: e.tensor_tensor(out=PTT[:, rot, :], in0=ps[by][:, 128:256], in1=DTT[:, rot, :], op=ALU.mult),
                     reads=psk(by) + [("DTT", rot)], writes=[("PTT", rot)])

            def head_back(h):
                rot = h % 2
                bX = bXs[h % 2]
                vrhs = cx.VAUG[:, c, h, 0:DV + 1]

                def mm(e):
                    ins = None
                    if with_out:
                        e.matmul(ps[bX][:, 0:DV + 1], lhsT=PTT[:, rot, :], rhs=vrhs, start=True, stop=True)
                        e.matmul(ps[bX][:, 130:130 + DV + 1], lhsT=QT[0:64, h, cs], rhs=CST_B[d][:, h, 0:DV + 1],
                                 start=True, stop=True)
                    return e.matmul(ps[bX][0:64, 260:260 + DV + 1], lhsT=KW[:, h, :], rhs=vrhs, start=True, stop=True)
                rd = [("KW", h), vk]
                if with_out:
                    rd += [("PTT", rot), cx.k("HID", h), ("CB", d, h)]
                P.op("pe", mm, reads=rd, writes=psk(bX))
                if with_out:
                    P.op("act", lambda e: e.activation(out=T0[:, rot, 0:DV + 1], in_=ps[bX][:, 0:DV + 1], func=AF.Copy),
                         reads=psk(bX), writes=[("T0", rot)])
                    P.op("dve", lambda e: e.scalar_tensor_tensor(
                        out=T1[:, h, 0:DV + 1], in0=ps[bX][:, 130:130 + DV + 1], scalar=G3S[:, 8 + h:9 + h],
                        in1=T0[:, rot, 0:DV + 1], op0=ALU.mult, op1=ALU.add),
                        reads=psk(bX) + [("G3S", 1), ("T0", rot)], writes=[("T1", h)])
                P.op("dve", lambda e: e.scalar_tensor_tensor(
                    out=CST_C[d][:, h, 0:DV + 1], in0=CST_C[d][:, h, 0:DV + 1], scalar=G3S[0:64, 32 + h:33 + h],
                    in1=ps[bX][0:64, 260:260 + DV + 1], op0=ALU.mult, op1=ALU.add),
                    reads=[("CS", d, h), ("G3S", 4)] + psk(bX), writes=[("CS", d, h)])
                P.op("act", lambda e: e.activation(out=CST_B[d][:, h, 0:DV + 1], in_=CST_C[d][:, h, 0:DV + 1], func=AF.Copy),
                     reads=[("CS", d, h)], writes=[("CB", d, h)])

            if with_out:
                head_front(0)
                yield (0.7, 6.0)
            for h in range(NH):
                if with_out and h + 1 < NH:
                    head_front(h + 1)
                head_back(h)
                yield (1.0, 4.0)
            if with_out:
                den = T1[:, :, DV]
                P.op("act", lambda e: e.activation(out=RD[:, 0:8], in_=den, func=AF.Abs),
                     reads=[("T1", h) for h in range(NH)], writes=[("RD", 0)])
                P.op("dve", lambda e: e.tensor_scalar_max(out=RD[:, 0:8], in0=RD[:, 0:8], scalar1=1.0),
                     reads=[("RD", 0)], writes=[("RD", 0)])
                P.op("dve", lambda e: e.reciprocal(out=RD[:, 8:16], in_=RD[:, 0:8]), reads=[("RD", 0)], writes=[("RD", 1)])
                hs = cx.F1[:].rearrange("p a b -> p (a b)").rearrange("p (c f) -> p c f", f=D)
                for h in range(NH):
                    dst = hs[:, c, h * DV:(h + 1) * DV]
                    key = cx.k("F1", 4 * c + h // 2)
                    if first_dir:
                        eng = "act" if h % 2 == 0 else "pool"
                        if eng == "act":
                            P.op("act", lambda e, h=h, dst=dst: e.activation(out=dst, in_=T1[:, h, 0:DV], func=AF.Identity,
                                                                             bias=0.0, scale=RD[:, 8 + h:9 + h]),
                                 reads=[("T1", h), ("RD", 1)], writes=[key])
                        else:
                            P.op("dve", lambda e, h=h, dst=dst: e.tensor_scalar(out=dst, in0=T1[:, h, 0:DV],
                                                                                 scalar1=RD[:, 8 + h:9 + h], scalar2=None,
                                                                                 op0=ALU.mult),
                                 reads=[("T1", h), ("RD", 1)], writes=[key])
                    else:
                        P.op("dve", lambda e, h=h, dst=dst: e.scalar_tensor_tensor(
                            out=dst, in0=T1[:, h, 0:DV], scalar=RD[:, 8 + h:9 + h], in1=dst, op0=ALU.mult, op1=ALU.add),
                            reads=[("T1", h), ("RD", 1), key], writes=[key])
            banks.rel(pairs)
            yield (0.0, 4.0)

        def l1_proj(cx, full):
            if full:
                pairs = yield from acq(2)
                slot, wkey = w_next(("qkvo", 0, 0))
                for g in range(2):
                    pair = pairs[g]
                    fm_mm(cx, slot, wkey, cx.B0, "B0", 0, pair, True, True, ncol=4, msize=64, col0=g * 256)
                    for q in range(4):
                        h = g * 4 + q
                        b, c0 = reg(pair, q, CT)
                        P.op("act", lambda e, h=h, b=b, c0=c0: e.activation(out=cx.HID[0:64, h, :], in_=ps[b][0:64, c0:c0 + CT],
                                                                            func=AF.Copy, scale=DQK ** -0.5),
                             reads=psk(b), writes=[cx.k("HID", h)])
                banks.rel(pairs)
                yield (3.7, 2.0)
            pairs = yield from acq(2)
            slot, wkey = w_next(("qkvo", 0, 512))
            if full:
                for g in range(2):
                    pair = pairs[g]
                    fm_mm(cx, slot, wkey, cx.B0, "B0", 0, pair, True, True, ncol=4, msize=64, col0=g * 256)
                    for q in range(4):
                        h = g * 4 + q
                        b, c0 = reg(pair, q, CT)
                        P.op("dve", lambda e, h=h, b=b, c0=c0: e.tensor_copy(out=cx.HID[0:64, 8 + h, :], in_=ps[b][0:64, c0:c0 + CT]),
                             reads=psk(b), writes=[cx.k("HID", 8 + h)])
            pair = pairs[0]
            for c in range(NCH):
                b = pair * 2 + c

                def f(e, c=c, b=b, slot=slot):
                    ins = None
                    for kc in range(FC):
                        ins = e.matmul(ps[b][:, 0:512], lhsT=cx.B0[:, kc, c * 128:(c + 1) * 128], rhs=slot[:, kc, :],
                                       start=(kc == 0), stop=(kc == FC - 1))
                    return ins
                P.op("pe", f, reads=[wkey] + cx.ks("B0", FC), writes=psk(b))
                P.op("act", lambda e, c=c, b=b: e.activation(out=cx.KTOK[:, c, :], in_=ps[b][:, 0:512], func=AF.Copy),
                     reads=psk(b), writes=[cx.k("KTOK", c)])
            banks.rel(pairs)
            yield (5.5, 2.0)
            for j in range(2):
                pair = (yield from acq(1))[0]
                slot, wkey = w_next(("qkvo", 0, 1024 + 512 * j))
                for c in range(NCH):
                    b = pair * 2 + c

                    def f(e, c=c, b=b, slot=slot):
                        ins = None
                        for kc in range(FC):
                            ins = e.matmul(ps[b][:, 0:512], lhsT=cx.B0[:, kc, c * 128:(c + 1) * 128], rhs=slot[:, kc, :],
                                           start=(kc == 0), stop=(kc == FC - 1))
                        return ins
                    P.op("pe", f, reads=[wkey] + cx.ks("B0", FC), writes=psk(b))
                    dst = cx.VAUG[:, c, 4 * j:4 * j + 4, 0:DV]
                    src = ps[b][:, 0:512].rearrange("p (h v) -> p h v", v=DV)
                    if c % 2 == 0:
                        P.op("dve", lambda e, dst=dst, src=src: e.tensor_copy(out=dst, in_=src),
                             reads=psk(b), writes=[cx.k("VAUG", c)])
                    else:
                        P.op("act", lambda e, dst=dst, src=src: e.activation(out=dst, in_=src, func=AF.Copy),
                             reads=psk(b), writes=[cx.k("VAUG", c)])
                banks.rel([pair])
                yield (3.6, 2.0)
            if full:
                for j in range(2):
                    pair = (yield from acq(1))[0]
                    slot, wkey = w_next(("qkvo", 0, 2048 + 512 * j))
                    fm_mm(cx, slot, wkey, cx.B0, "B0", 0, pair, True, True)
                    for mi in range(4):
                        h = 4 * j + mi
                        b, c0 = reg(pair, mi, CT)
                        P.op("act", lambda e, h=h, b=b, c0=c0: e.activation(out=cx.HID[:, 16 + h, :], in_=ps[b][:, c0:c0 + CT], func=AF.Sigmoid),
                             reads=psk(b), writes=[cx.k("HID", 16 + h)])
                    banks.rel([pair])
                    yield (3.7, 2.0)


        def wait_flag(name):
            while not flags.get(name):
                yield "blocked"

        flat3 = lambda t: t[:].rearrange("p a b -> p (a b)")

        def ctx_context(cx):
            yield from load_tokens(cx, ctx_d)
            yield from layer0(cx, 1, CTX)
            yield from wait_flag(("mod", 1))
            yield from norm_mod(cx, 1, 1, "a1", "sh1")
            yield from l1_proj(cx, False)
            yield from gates_mm(cx, 2 * NCX)
            for d in range(2):
                order = [0, 1] if d == 0 else [1, 0]
                for c in order:
                    yield from scan_chunk(cx, d, 2 * NCX + c, c, False)
            flags[("fscan", -1)] = True
            flags[("bscan", NCX)] = True

        def sweep1_context(cx, i):
            yield from load_tokens(cx, x_d[i * CT:(i + 1) * CT, :])
            yield from layer0(cx, 0, 64)
            yield from wait_flag(("mod", 1))
            P.dma("sp", lambda e: e.dma_start(out=x1_s[i], in_=flat3(cx.F0)), reads=cx.ks("F0", FC), writes=[("x1_s", i)])
            if DEBUG:
                P.dma("sp", lambda e: e.dma_start(out=dbg_d[i], in_=flat3(cx.F0)), reads=cx.ks("F0", FC), writes=[("dbg", i)])
            yield from norm_mod(cx, 1, 0, "a1", "sh1")
            yield from l1_proj(cx, True)
            yield from gates_mm(cx, 2 * i)
            P.dma("sp", lambda e: e.dma_start(out=qt_s[i], in_=cx.HID[0:64, 0:8, :].rearrange("p a b -> p (a b)")),
                  reads=[cx.k("HID", h) for h in range(8)], writes=[("qt_s", i)])
            P.dma("sp", lambda e: e.dma_start(out=kt_s[i], in_=cx.HID[0:64, 8:16, :].rearrange("p a b -> p (a b)")),
                  reads=[cx.k("HID", 8 + h) for h in range(8)], writes=[("kt_s", i)])
            P.dma("sp", lambda e: e.dma_start(out=so_s[i], in_=cx.HID[:, 16:24, :].rearrange("p a b -> p (a b)")),
                  reads=[cx.k("HID", 16 + h) for h in range(8)], writes=[("so_s", i)])
            P.dma("sp", lambda e: e.dma_start(out=ktok_s[i], in_=flat3(cx.KTOK)), reads=cx.ks("KTOK", NCH), writes=[("ktok_s", i)])
            P.dma("sp", lambda e: e.dma_start(out=vaug_s[i], in_=cx.VAUG[:].rearrange("p a b c -> p (a b c)")),
                  reads=cx.ks("VAUG", NCH), writes=[("vaug_s", i)])
            yield from wait_flag(("fscan", i - 1))
            for c in range(NCH):
                yield from scan_chunk(cx, 0, 2 * i + c, c, True, first_dir=True)
            flags[("fscan", i)] = True
            P.dma("sp", lambda e: e.dma_start(out=hf_s[i], in_=flat3(cx.F1)), reads=cx.ks("F1", FC), writes=[("hf_s", i)])
            flags[("s1done", i)] = True
            yield (0.0, 0.0)

        def sweep2_context(cx, i):
            yield from wait_flag(("s1done", i))
            P.dma("sp", lambda e: e.dma_start(out=cx.HID[0:64, 0:8, :].rearrange("p a b -> p (a b)"), in_=qt_s[i]),
                  reads=[("qt_s", i)], writes=[cx.k("HID", h) for h in range(8)])
            P.dma("sp", lambda e: e.dma_start(out=cx.HID[0:64, 8:16, :].rearrange("p a b -> p (a b)"), in_=kt_s[i]),
                  reads=[("kt_s", i)], writes=[cx.k("HID", 8 + h) for h in range(8)])
            P.dma("sp", lambda e: e.dma_start(out=flat3(cx.KTOK), in_=ktok_s[i]), reads=[("ktok_s", i)], writes=cx.ks("KTOK", NCH))
            P.dma("sp", lambda e: e.dma_start(out=cx.VAUG[:].rearrange("p a b c -> p (a b c)"), in_=vaug_s[i]),
                  reads=[("vaug_s", i)], writes=cx.ks("VAUG", NCH))
            P.dma("sp", lambda e: e.dma_start(out=flat3(cx.F1), in_=hf_s[i]), reads=[("hf_s", i)], writes=cx.ks("F1", FC))
            P.dma("sp", lambda e: e.dma_start(out=cx.HID[:, 16:24, :].rearrange("p a b -> p (a b)"), in_=so_s[i]),
                  reads=[("so_s", i)], writes=[cx.k("HID", 16 + h) for h in range(8)])
            P.dma("sp", lambda e: e.dma_start(out=flat3(cx.F0), in_=x1_s[i]), reads=[("x1_s", i)], writes=cx.ks("F0", FC))
            yield (0.0, 15.0)
            yield from wait_flag(("bscan", i + 1))
            hs = cx.F1[:].rearrange("p a b -> p (a b)").rearrange("p (c f) -> p c f", f=D)
            for c in reversed(range(NCH)):
                yield from scan_chunk(cx, 1, 2 * i + c, c, True, first_dir=False)
            flags[("bscan", i)] = True
            for c in reversed(range(NCH)):
                hkeys = [cx.k("F1", 4 * c + q) for q in range(4)]
                sq = cx.F2[:].rearrange("p a b -> p (a b)")[:, 0:D]
                sqk = [cx.k("F2", q) for q in range(4)]
                P.op("dve", lambda e, c=c, sq=sq: e.tensor_tensor(out=sq, in0=hs[:, c, :], in1=hs[:, c, :], op=ALU.mult),
                     reads=hkeys, writes=sqk)
                P.op("dve", lambda e, sq=sq: e.tensor_reduce(out=cx.SSQ[:, 0:8], in_=sq.rearrange("p (h v) -> p h v", v=DV),
                                                             axis=AX.X, op=ALU.add),
                     reads=sqk, writes=[cx.k("SSQ", 0)])
                P.op("act", lambda e: e.activation(out=cx.SSQ[:, 8:16], in_=cx.SSQ[:, 0:8], func=AF.Sqrt, bias=EPS_COL, scale=1.0 / DV),
                     reads=[cx.k("SSQ", 0), "SM"], writes=[cx.k("SSQ", 1)])
                P.op("dve", lambda e: e.reciprocal(out=cx.SSQ[:, 8:16], in_=cx.SSQ[:, 8:16]), reads=[cx.k("SSQ", 1)], writes=[cx.k("SSQ", 1)])
                yield (0.0, 12.0)
                pair = (yield from acq(1))[0]
                for h in range(NH):
                    rot = h % 2
                    b = pair * 2 + rot
                    P.op("act", lambda e, c=c, h=h, rot=rot: e.activation(out=cx.HN[:, rot, :], in_=hs[:, c, h * DV:(h + 1) * DV],
                                                                          func=AF.Identity, bias=0.0, scale=cx.SSQ[:, 8 + h:9 + h]),
                         reads=hkeys + [cx.k("SSQ", 1)], writes=[cx.k("HN", rot)])
                    P.op("pe", lambda e, rot=rot, b=b: e.transpose(out=ps[b][:, 0:128], in_=cx.HN[:, rot, :], identity=IDENT),
                         reads=[cx.k("HN", rot), "CST"], writes=psk(b))
                    P.op("dve", lambda e, c=c, h=h, b=b: e.scalar_tensor_tensor(
                        out=cx.HID[:, 24 + h, c * 128:(c + 1) * 128], in0=ps[b][:, 0:128],
                        scalar=SM[:, S_MLNW + h:S_MLNW + h + 1], in1=cx.HID[:, 16 + h, c * 128:(c + 1) * 128],
                        op0=ALU.mult, op1=ALU.mult),
                        reads=psk(b) + ["SM", cx.k("HID", 16 + h)], writes=[cx.k("HID", 24 + h)])
                    if h % 2 == 1:
                        yield (0.5, 5.0)
                banks.rel([pair])
            for j in range(2):
                pair = (yield from acq(1))[0]
                slot, wkey = w_next(("mlout", 0, 512 * j))
                fm_mm(cx, slot, wkey, cx.HID, "HID", 24, pair, True, True)
                evac_branch(cx, pair, 4 * j)
                banks.rel([pair])
                yield (3.7, 4.0)
            yield from residual(cx, 1, 0, "g1")
            yield from mlp(cx, 1, 0)
            xt = cx.F2[:].rearrange("p a b -> p (a b)").rearrange("p (t f) -> p t f", f=D)
            for tt in range(NCH):
                pair = (yield from acq(1))[0]
                for half in range(2):
                    b = pair * 2 + half

                    def f(e, tt=tt, half=half, b=b):
                        ins = None
                        for q in range(4):
                            fc = half * 4 + q
                            ins = e.transpose(out=ps[b][:, q * 128:(q + 1) * 128], in_=cx.F0[:, fc, tt * 128:(tt + 1) * 128],
                                              identity=IDENT)
                        return ins
                    P.op("pe", f, reads=[cx.k("F0", half * 4 + q) for q in range(4)] + ["CST"], writes=psk(b))
                    wk = [cx.k("F2", tt * 4 + half * 2), cx.k("F2", tt * 4 + half * 2 + 1)]
                    if half == 0:
                        P.op("act", lambda e, tt=tt, half=half, b=b: e.activation(out=xt[:, tt, half * 512:(half + 1) * 512],
                                                                                  in_=ps[b][:, 0:512], func=AF.Copy),
                             reads=psk(b), writes=wk)
                    else:
                        P.op("dve", lambda e, tt=tt, half=half, b=b: e.tensor_copy(out=xt[:, tt, half * 512:(half + 1) * 512],
                                                                                   in_=ps[b][:, 0:512]),
                             reads=psk(b), writes=wk)
                banks.rel([pair])
                yield (1.8, 2.0)
            P.dma("sp", lambda e: e.dma_start(out=out_d[i * CT:(i + 1) * CT, :].rearrange("(t p) f -> p t f", p=128), in_=xt),
                  reads=cx.ks("F2", FC), writes=[("out", i)])
            yield (0.0, 0.0)

        jobs = [("ctx", None)] + [("s1", i) for i in range(NCX_RUN)] + [("s2", i) for i in reversed(range(NCX_RUN))]
        if STOP == "ctx":
            jobs = jobs[:1]
        elif STOP == "sweep1":
            jobs = jobs[:1 + NCX_RUN]

        def make(job, cx):
            kind, i = job
            if kind == "ctx":
                return ctx_context(cx)
            if kind == "s1":
                return sweep1_context(cx, i)
            return sweep2_context(cx, i)

        if NCX_RUN < NCX:
            for i in range(NCX_RUN, NCX):
                flags[("s1done", i)] = True
            flags[("bscan", NCX_RUN)] = True
        active = [[mod_layer(1), None, 0.0]]
        free_cx = [CXS[0], CXS[1]]
        pending = list(jobs)

        pe_clock = 0.0
        guard = 0
        while active or pending:
            guard += 1
            assert guard < 2000000, "scheduler livelock"
            while pending and free_cx:
                cx_ = free_cx.pop(0)
                active.append([make(pending.pop(0), cx_), cx_, pe_clock])
            order = sorted(active, key=lambda a_: max(a_[2], pe_clock))
            progressed = False
            for ent in order:
                try:
                    r = next(ent[0])
                except StopIteration:
                    active.remove(ent)
                    if ent[1] is not None:
                        free_cx.append(ent[1])
                    progressed = True
                    break
                if r == "blocked":
                    continue
                pe_us, lat_us = r if r is not None else (0.0, 0.0)
                start = max(pe_clock, ent[2])
                pe_clock = start + pe_us
                ent[2] = pe_clock + lat_us
                progressed = True
                break
            assert progressed, "all pipeline contexts blocked"
        assert not pending
        assert dry or wstate["used"] == len(sched), (wstate, len(sched))
        if dry:
            return sched
        P.emit()
    return nc


_NC_CACHE = {}


def kernel(x, c, ctx, c_ctx, norm_w, mod_w, mod_b, mlp_w1, mlp_w2, conv_w_in, conv_w, conv_w_out,
           ml_w_qkvo, ml_w_if, ml_b_if, ml_norm_w, ml_w_out):
    f32 = lambda a: np.ascontiguousarray(np.asarray(a, dtype=np.float32))
    x = f32(x); c = f32(c); ctx = f32(ctx); c_ctx = f32(c_ctx)
    if "nc" not in _NC_CACHE:
        _NC_CACHE["nc"] = build_nc()
    nc = _NC_CACHE["nc"]
    consts = _consts()
    shared = {
        "consts": consts, "mod_w": f32(mod_w), "mlp_w1": f32(mlp_w1), "mlp_w2": f32(mlp_w2),
        "conv_w_in": f32(conv_w_in[0]), "conv_w_out": f32(conv_w_out[0]),
        "ml_w_qkvo": f32(ml_w_qkvo[0]), "ml_w_out": f32(ml_w_out[0]),
    }
    in_maps = []
    for b in range(NCORES):
        m = dict(shared)
        m["x"] = x[b]
        m["ctx"] = ctx[b]
        m["small"] = _small(c[b], c_ctx, f32(norm_w), f32(mod_b), f32(conv_w), f32(ml_norm_w), f32(ml_b_if), f32(ml_w_if))
        in_maps.append(m)
    res = run_bass_kernel_spmd(nc, in_maps, core_ids=list(range(NCORES)))
    _NC_CACHE["last"] = res
    return np.stack([np.asarray(r["out"], dtype=np.float32) for r in res.results], axis=0)
```
